# Optimizing a Trainium2 kernel written in Bass

```python
import jax, jax.numpy as jnp
from jax import lax
import numpy as np

D_MODEL = 2048
BATCH = 4
SEQ = 2048
DEPTH = 4

MIX_A = D_MODEL // 2
RET_HEAD_DIM = 256
RET_HEADS = MIX_A // RET_HEAD_DIM
RET_CHUNK = 128
RET_COLS = 4 * MIX_A
ROPE_BASE = 10000.0
MIX_B = D_MODEL - MIX_A
RWKV_HEAD_DIM = 64
RWKV_HEADS = MIX_B // RWKV_HEAD_DIM
RWKV_LORA_W = 64
RWKV_LORA_A = 64
RWKV_LORA_G = 160
RWKV_LORA_V = 32
RWKV_COLS = 3 * MIX_B + RWKV_LORA_W + RWKV_LORA_A + RWKV_LORA_G
RWKV_SPLITS = (MIX_B, 2 * MIX_B, 3 * MIX_B, 3 * MIX_B + RWKV_LORA_W, 3 * MIX_B + RWKV_LORA_W + RWKV_LORA_A)
RWKV_GN_EPS = 64e-5
EVEN_PROJ = RET_COLS + RWKV_COLS
GLA_HEADS = 4
GLA_DK = D_MODEL // 2
GLA_DV = D_MODEL
GLA_HEAD_DK = GLA_DK // GLA_HEADS
GLA_HEAD_DV = GLA_DV // GLA_HEADS
GLA_LORA = 16
GLA_GATE_NORM = 16.0
GLA_CHUNK = 64
ODD_PROJ = 2 * GLA_DK + 2 * GLA_DV + GLA_LORA
GLA_SPLITS = (GLA_DK, 2 * GLA_DK, 2 * GLA_DK + GLA_DV, 2 * GLA_DK + 2 * GLA_DV)
MOE_GROUPS = 4
MOE_PER_GROUP = 8
N_EXPERTS = MOE_GROUPS * MOE_PER_GROUP
MOE_TOPK = 2
EXPERT_FF = D_MODEL // 4
MOE_BLOCK = 128
N_EVEN = (DEPTH + 1) // 2
N_ODD = DEPTH // 2
DEEPNORM_ALPHA = (2 * DEPTH) ** 0.25
DEEPNORM_BETA = (8 * DEPTH) ** -0.25
LN_EPS = 1e-5

kernel_name = "hybrid_retnet_rwkv7_gla_hmoe_deepnorm"

f32 = jnp.float32


def layer_norm(x, g, b):
    xf = x.astype(f32)
    mu = jnp.mean(xf, -1, keepdims=True)
    var = jnp.mean(jnp.square(xf - mu), -1, keepdims=True)
    return ((xf - mu) * lax.rsqrt(var + LN_EPS) * g + b).astype(x.dtype)


def head_layer_norm(y, g, b, eps):
    H, d = y.shape[-2:]
    yf = y.astype(f32)
    mu = jnp.mean(yf, -1, keepdims=True)
    var = jnp.mean(jnp.square(yf - mu), -1, keepdims=True)
    out = (yf - mu) * lax.rsqrt(var + eps) * g.reshape(H, d) + b.reshape(H, d)
    return out.reshape(*y.shape[:-2], H * d)


def head_rms_norm(y, g, eps):
    H, d = y.shape[-2:]
    yf = y.astype(f32)
    out = yf * lax.rsqrt(jnp.mean(jnp.square(yf), -1, keepdims=True) + eps) * g.reshape(H, d)
    return out.reshape(*y.shape[:-2], H * d)


def rotary(x, positions):
    half = x.shape[-1] // 2
    inv = ROPE_BASE ** (-jnp.arange(half, dtype=f32) / half)
    ang = positions.astype(f32)[..., None] * inv
    cos, sin = jnp.cos(ang)[:, :, None, :], jnp.sin(ang)[:, :, None, :]
    x1, x2 = x[..., :half], x[..., half:]
    return jnp.concatenate([x1 * cos - x2 * sin, x1 * sin + x2 * cos], -1).astype(x.dtype)


def retention(q, k, v):
    B_, S_, H, d = q.shape
    C = RET_CHUNK
    N = S_ // C
    log_g = jnp.log1p(-jnp.exp2(-5.0 - jnp.arange(H, dtype=f32)))
    idx = jnp.arange(C, dtype=f32)
    diff = idx[:, None] - idx[None, :]
    inner_decay = jnp.where(diff >= 0, jnp.exp(log_g[:, None, None] * jnp.maximum(diff, 0.0)), 0.0)
    qc = q.reshape(B_, N, C, H, d)
    kc = k.reshape(B_, N, C, H, d)
    vc = v.reshape(B_, N, C, H, d)
    scores = jnp.einsum('bnihd,bnjhd->bnhij', qc, kc) * inner_decay
    o_inner = jnp.einsum('bnhij,bnjhe->bnihe', scores, vc)
    k_w = jnp.exp(log_g[None, :] * (C - 1 - idx)[:, None])
    kv = jnp.einsum('bnjhd,jh,bnjhe->nbhde', kc, k_w, vc).astype(f32)
    g_chunk = jnp.exp(log_g * C)

    def step(state, kv_n):
        return state * g_chunk[None, :, None, None] + kv_n, state

    _, states = lax.scan(step, jnp.zeros((B_, H, d, d), f32), kv)
    q_w = jnp.exp(log_g[None, :] * (idx + 1.0)[:, None])
    o_cross = jnp.einsum('bnihd,nbhde,ih->bnihe', qc, states, q_w)
    return (o_inner + o_cross).reshape(B_, S_, H, d)


def wkv7_scan(r, w, k, v, a, b):
    B_, _, H, N = r.shape
    xs = tuple(jnp.moveaxis(t.astype(f32), 1, 0) for t in (r, w, k, v, a, b))

    def step(state, inp):
        r_t, w_t, k_t, v_t, a_t, b_t = inp
        sa = jnp.einsum('bhvk,bhk->bhv', state, a_t)
        state = state * w_t[:, :, None, :] + sa[..., None] * b_t[:, :, None, :] + v_t[..., None] * k_t[:, :, None, :]
        return state, jnp.einsum('bhvk,bhk->bhv', state, r_t)

    _, y = lax.scan(step, jnp.zeros((B_, H, N, N), f32), xs)
    return jnp.moveaxis(y, 0, 1)


def rwkv7_time_mix(pb, v_first, vres, mu, w0, w2, a0, a2, g2, k_k, k_a, r_k, lnx_g, lnx_b):
    B_, S_, _ = pb.shape
    prev = jnp.pad(pb[:, :-1], ((0, 0), (1, 0), (0, 0)))
    pb = pb + (prev - pb) * mu
    r, k, v, wd, ad, gd = jnp.split(pb, RWKV_SPLITS, axis=-1)
    w_log = -jax.nn.softplus(-(w0 + jnp.tanh(wd) @ w2).astype(f32)) - 0.5
    decay = jnp.exp(-jnp.exp(w_log))
    a = jax.nn.sigmoid((a0 + ad @ a2).astype(f32))
    gate = jax.nn.sigmoid(gd) @ g2
    if vres is None:
        v_first = v
    else:
        v0, v1, v2 = vres
        v = v + (v_first - v) * jax.nn.sigmoid(v0 + (v @ v1) @ v2)
    heads = lambda t: t.reshape(B_, S_, RWKV_HEADS, RWKV_HEAD_DIM)
    kk = heads((k * k_k).astype(f32))
    kk = kk / jnp.maximum(jnp.sqrt(jnp.sum(jnp.square(kk), -1, keepdims=True)), 1e-12)
    k = k * (1.0 + (a - 1.0) * k_a)
    rh, kh, vh, ah = heads(r), heads(k), heads(v), heads(a)
    y = wkv7_scan(rh, heads(decay), kh, vh, -kk, kk * ah)
    y = head_layer_norm(y, lnx_g, lnx_b, RWKV_GN_EPS)
    bonus = (jnp.sum(rh * kh * r_k, -1, keepdims=True) * vh).reshape(B_, S_, MIX_B)
    return (y + bonus) * gate, v_first


def even_mixer(h, positions, v_first, vres, win, wo, ret_gn_g, ret_gn_b,
               mu, w0, w2, a0, a2, g2, k_k, k_a, r_k, lnx_g, lnx_b):
    B_, S_, _ = h.shape
    p = h @ win
    q, k, v, g = jnp.split(p[..., :RET_COLS], 4, axis=-1)
    heads = lambda t: t.reshape(B_, S_, RET_HEADS, RET_HEAD_DIM)
    q = rotary(heads(q), positions)
    k = rotary(heads(k), positions) * RET_HEAD_DIM ** -0.5
    ya = retention(q, k, heads(v))
    out_a = jax.nn.silu(g.astype(f32)) * head_layer_norm(ya, ret_gn_g, ret_gn_b, LN_EPS)
    out_b, v_first = rwkv7_time_mix(p[..., RET_COLS:], v_first, vres, mu, w0, w2, a0, a2, g2,
                                    k_k, k_a, r_k, lnx_g, lnx_b)
    y = jnp.concatenate([out_a, out_b.astype(f32)], -1).astype(h.dtype) @ wo
    return y, v_first


def gla_chunked(q, k, v, log_a):
    B_, S_, H, dk = q.shape
    dv = v.shape[-1]
    C = GLA_CHUNK
    N = S_ // C
    to_chunks = lambda t: t.astype(f32).reshape(B_, N, C, H, t.shape[-1]).transpose(1, 0, 3, 2, 4)
    causal = jnp.tril(jnp.ones((C, C), bool))

    def step(state, inp):
        qc, kc, vc, lac = inp
        bcum = jnp.cumsum(lac, axis=2)
        o_inter = jnp.einsum('bhid,bhde->bhie', qc * jnp.exp(bcum), state)
        rel = jnp.where(causal[None, None, :, :, None], bcum[:, :, :, None, :] - bcum[:, :, None, :, :], -jnp.inf)
        attn = jnp.einsum('bhid,bhjd,bhijd->bhij', qc, kc, jnp.exp(rel))
        o_intra = jnp.einsum('bhij,bhje->bhie', attn, vc)
        b_last = bcum[:, :, -1:, :]
        state = state * jnp.exp(b_last)[:, :, 0, :, None] + jnp.einsum('bhjd,bhje->bhde', kc * jnp.exp(b_last - bcum), vc)
        return state, o_inter + o_intra

    _, o = lax.scan(step, jnp.zeros((B_, H, dk, dv), f32),
                    (to_chunks(q), to_chunks(k), to_chunks(v), to_chunks(log_a)))
    return o.transpose(1, 0, 3, 2, 4).reshape(B_, S_, H, dv)


def gla_mixer(h, win, wo, a2, ab, norm_g):
    B_, S_, _ = h.shape
    q, k, v, r, ad = jnp.split(h @ win, GLA_SPLITS, axis=-1)
    log_a = jax.nn.log_sigmoid((ad @ a2 + ab).astype(f32)) / GLA_GATE_NORM
    hk = lambda t: t.reshape(B_, S_, GLA_HEADS, GLA_HEAD_DK)
    o = gla_chunked(hk(q) * GLA_HEAD_DK ** -0.5, hk(k), v.reshape(B_, S_, GLA_HEADS, GLA_HEAD_DV), hk(log_a))
    o = head_rms_norm(o, norm_g, LN_EPS)
    return (jax.nn.silu(r.astype(f32)) * o).astype(h.dtype) @ wo


def hier_moe(h, wg, bg, we, be, w1, w3, w2):
    B_, S_, D = h.shape
    T = B_ * S_
    xt = h.reshape(T, D)
    g_prob = jax.nn.softmax((xt @ wg + bg).astype(f32), -1)
    p_top, g_top = lax.top_k(g_prob, 1)
    e_logits = (xt @ we + be).astype(f32).reshape(T, MOE_GROUPS, MOE_PER_GROUP)
    e_sel = jnp.take_along_axis(e_logits, g_top[:, :, None], axis=1)[:, 0]
    e_val, e_top = lax.top_k(e_sel, MOE_TOPK)
    gate = p_top * jax.nn.softmax(e_val, -1)
    expert = g_top * MOE_PER_GROUP + e_top
    A = T * MOE_TOPK
    e_flat = expert.reshape(A)
    tok_flat = jnp.repeat(jnp.arange(T, dtype=jnp.int32), MOE_TOPK)
    order = jnp.argsort(e_flat)
    e_s, tok_s, w_s = e_flat[order], tok_flat[order], gate.reshape(A)[order]
    counts = jnp.bincount(e_flat, length=N_EXPERTS)
    padded = (counts + MOE_BLOCK - 1) // MOE_BLOCK * MOE_BLOCK
    pad_end = jnp.cumsum(padded)
    pad_start = pad_end - padded
    start = jnp.cumsum(counts) - counts
    slot = pad_start[e_s] + jnp.arange(A, dtype=jnp.int32) - start[e_s]
    NB = -(-A // MOE_BLOCK) + N_EXPERTS
    buf_tok = jnp.full((NB * MOE_BLOCK,), T, jnp.int32).at[slot].set(tok_s)
    blk_expert = jnp.minimum(jnp.searchsorted(pad_end, jnp.arange(NB, dtype=jnp.int32) * MOE_BLOCK, side='right'),
                             N_EXPERTS - 1)
    x_pad = jnp.concatenate([xt, jnp.zeros((1, D), xt.dtype)], 0)

    def expert_block(args):
        toks, e = args
        xb = x_pad[toks]
        return (jax.nn.silu(xb @ w1[e]) * (xb @ w3[e])) @ w2[e]

    y_buf = lax.map(expert_block, (buf_tok.reshape(NB, MOE_BLOCK), blk_expert)).reshape(NB * MOE_BLOCK, D)
    y = jax.ops.segment_sum(y_buf[slot] * w_s[:, None].astype(y_buf.dtype), tok_s, num_segments=T)
    return y.reshape(B_, S_, D)


def setup_inputs(seed: int = 0) -> dict:
    key = jax.random.key(seed)
    ks = iter(jax.random.split(key, 48))

    def nrm(shape, scale):
        return scale * jax.random.normal(next(ks), shape, f32)

    def gain(shape):
        return 1.0 + nrm(shape, 0.02)

    D = D_MODEL
    x = nrm((BATCH, SEQ, D), 1.0)
    c = nrm((BATCH, D), 1.0)
    positions = jnp.arange(SEQ, dtype=jnp.int32)[None, :] + jax.random.randint(next(ks), (BATCH, 1), 0, 4096, jnp.int32)
    ada_w = nrm((DEPTH, D, 6 * D), 0.1 * D ** -0.5)
    ada_b = nrm((DEPTH, 6 * D), 0.01)
    ln_g = gain((DEPTH, 2, D))
    ln_b = nrm((DEPTH, 2, D), 0.02)
    ev_win = nrm((N_EVEN, D, EVEN_PROJ), D ** -0.5)
    ev_wo = nrm((N_EVEN, D, D), DEEPNORM_BETA * D ** -0.5)
    ret_gn_g = gain((N_EVEN, MIX_A))
    ret_gn_b = nrm((N_EVEN, MIX_A), 0.02)
    rw_mu = jax.random.uniform(next(ks), (N_EVEN, RWKV_COLS), f32)
    ratio = jnp.arange(MIX_B, dtype=f32) / (MIX_B - 1)
    expo = 0.85 + (jnp.arange(N_EVEN, dtype=f32) / max(N_EVEN - 1, 1)) ** 0.5
    rw_w0 = -6.5 + 5.0 * ratio[None, :] ** expo[:, None] + nrm((N_EVEN, MIX_B), 0.1)
    rw_w2 = nrm((N_EVEN, RWKV_LORA_W, MIX_B), 0.5 * RWKV_LORA_W ** -0.5)
    rw_a0 = nrm((N_EVEN, MIX_B), 0.1)
    rw_a2 = nrm((N_EVEN, RWKV_LORA_A, MIX_B), RWKV_LORA_A ** -0.5)
    rw_g2 = nrm((N_EVEN, RWKV_LORA_G, MIX_B), RWKV_LORA_G ** -0.5)
    rw_kk = 0.85 + nrm((N_EVEN, MIX_B), 0.05)
    rw_ka = 1.0 + nrm((N_EVEN, MIX_B), 0.05)
    rw_rk = nrm((N_EVEN, RWKV_HEADS, RWKV_HEAD_DIM), 0.1)
    rw_lnx_g = gain((N_EVEN, MIX_B))
    rw_lnx_b = nrm((N_EVEN, MIX_B), 0.02)
    rw_v0 = nrm((N_EVEN - 1, MIX_B), 0.1)
    rw_v1 = nrm((N_EVEN - 1, MIX_B, RWKV_LORA_V), MIX_B ** -0.5)
    rw_v2 = nrm((N_EVEN - 1, RWKV_LORA_V, MIX_B), RWKV_LORA_V ** -0.5)
    od_win = nrm((N_ODD, D, ODD_PROJ), D ** -0.5)
    od_wo = nrm((N_ODD, GLA_DV, D), DEEPNORM_BETA * GLA_DV ** -0.5)
    gla_a2 = nrm((N_ODD, GLA_LORA, GLA_DK), GLA_LORA ** -0.5)
    gla_ab = nrm((N_ODD, GLA_DK), 0.5)
    gla_norm_g = gain((N_ODD, GLA_DV))
    moe_wg = nrm((DEPTH, D, MOE_GROUPS), D ** -0.5)
    moe_bg = nrm((DEPTH, MOE_GROUPS), 0.01)
    moe_we = nrm((DEPTH, D, N_EXPERTS), D ** -0.5)
    moe_be = nrm((DEPTH, N_EXPERTS), 0.01)
    moe_w1 = nrm((DEPTH, N_EXPERTS, D, EXPERT_FF), D ** -0.5)
    moe_w3 = nrm((DEPTH, N_EXPERTS, D, EXPERT_FF), D ** -0.5)
    moe_w2 = nrm((DEPTH, N_EXPERTS, EXPERT_FF, D), DEEPNORM_BETA * EXPERT_FF ** -0.5)
    return {"x": x, "c": c, "positions": positions, "ada_w": ada_w, "ada_b": ada_b,
            "ln_g": ln_g, "ln_b": ln_b, "ev_win": ev_win, "ev_wo": ev_wo,
            "ret_gn_g": ret_gn_g, "ret_gn_b": ret_gn_b, "rw_mu": rw_mu, "rw_w0": rw_w0,
            "rw_w2": rw_w2, "rw_a0": rw_a0, "rw_a2": rw_a2, "rw_g2": rw_g2, "rw_kk": rw_kk,
            "rw_ka": rw_ka, "rw_rk": rw_rk, "rw_lnx_g": rw_lnx_g, "rw_lnx_b": rw_lnx_b,
            "rw_v0": rw_v0, "rw_v1": rw_v1, "rw_v2": rw_v2, "od_win": od_win, "od_wo": od_wo,
            "gla_a2": gla_a2, "gla_ab": gla_ab, "gla_norm_g": gla_norm_g,
            "moe_wg": moe_wg, "moe_bg": moe_bg, "moe_we": moe_we, "moe_be": moe_be,
            "moe_w1": moe_w1, "moe_w3": moe_w3, "moe_w2": moe_w2}


def reference(x, c, positions, ada_w, ada_b, ln_g, ln_b, ev_win, ev_wo, ret_gn_g, ret_gn_b,
              rw_mu, rw_w0, rw_w2, rw_a0, rw_a2, rw_g2, rw_kk, rw_ka, rw_rk, rw_lnx_g, rw_lnx_b,
              rw_v0, rw_v1, rw_v2, od_win, od_wo, gla_a2, gla_ab, gla_norm_g,
              moe_wg, moe_bg, moe_we, moe_be, moe_w1, moe_w3, moe_w2):
    c_act = jax.nn.silu(c)
    v_first = None
    for layer in range(DEPTH):
        mod = (c_act @ ada_w[layer] + ada_b[layer])[:, None, :]
        sh_m, sc_m, gt_m, sh_f, sc_f, gt_f = jnp.split(mod, 6, axis=-1)
        h = x * (1.0 + sc_m) + sh_m
        j = layer // 2
        if layer % 2 == 0:
            vres = None if j == 0 else (rw_v0[j - 1], rw_v1[j - 1], rw_v2[j - 1])
            y, v_first = even_mixer(h, positions, v_first, vres, ev_win[j], ev_wo[j], ret_gn_g[j], ret_gn_b[j],
                                    rw_mu[j], rw_w0[j], rw_w2[j], rw_a0[j], rw_a2[j], rw_g2[j],
                                    rw_kk[j], rw_ka[j], rw_rk[j], rw_lnx_g[j], rw_lnx_b[j])
        else:
            y = gla_mixer(h, od_win[j], od_wo[j], gla_a2[j], gla_ab[j], gla_norm_g[j])
        x = layer_norm(DEEPNORM_ALPHA * x + (1.0 + gt_m) * y, ln_g[layer, 0], ln_b[layer, 0])
        h = x * (1.0 + sc_f) + sh_f
        y = hier_moe(h, moe_wg[layer], moe_bg[layer], moe_we[layer], moe_be[layer],
                     moe_w1[layer], moe_w3[layer], moe_w2[layer])
        x = layer_norm(DEEPNORM_ALPHA * x + (1.0 + gt_f) * y, ln_g[layer, 1], ln_b[layer, 1])
    return x
```

```python
import contextlib
import numpy as np
import concourse.bass as bass
import concourse.mybir as mybir

F32 = mybir.dt.float32
I32 = mybir.dt.int32
ALU = mybir.AluOpType
AF = mybir.ActivationFunctionType
AX = mybir.AxisListType

ENGS = ("pe", "act", "dve", "pool", "sp")
EPOCH = 30000
NDMASEM = 24


class Buf:
    __slots__ = ("name", "w", "r")

    def __init__(self, name):
        self.name = name
        self.w = None
        self.r = []


class FW:
    def __init__(self):
        self.nc = bass.Bass("TRN2", target_bir_lowering=False)
        self.stack = contextlib.ExitStack()
        self.prog = {e: [] for e in ENGS}
        self.cnt = {e: 0 for e in ENGS}
        self.epoch_sem = {}
        self.known = {e: {} for e in ENGS}
        self.sems = {}
        self.nsem = 0
        for e in ENGS:
            self.epoch_sem[e] = self._newsem()
        self.dma_sems = [self._newsem() for _ in range(NDMASEM)]
        self.dma_cnt = [0] * NDMASEM
        self.dma_rr = 0
        self.final_tokens = []
        self.nbuf = 0

    def _newsem(self):
        s = self.stack.enter_context(self.nc.semaphore(f"s{self.nsem}"))
        self.nsem += 1
        self.sems[id(s)] = s
        return s

    def sbuf(self, shape, dtype=F32, name=None):
        self.nbuf += 1
        name = name or f"sb{self.nbuf}"
        t = self.stack.enter_context(self.nc.sbuf_tensor(name, list(shape), dtype))
        return t

    def psum(self, shape, dtype=F32, name=None):
        self.nbuf += 1
        name = name or f"ps{self.nbuf}"
        t = self.stack.enter_context(self.nc.psum_tensor(name, list(shape), dtype))
        return t

    def dram(self, name, shape, dtype=F32, kind="Internal"):
        return self.nc.dram_tensor(name, list(shape), dtype, kind=kind)

    def _need(self, eng, tokens):
        best = {}
        for tk in tokens:
            if tk is None:
                continue
            s, v = tk
            if best.get(id(s), 0) < v:
                best[id(s)] = v
        for sid, v in best.items():
            if self.known[eng].get(sid, 0) >= v:
                continue
            self.known[eng][sid] = v
            self.prog[eng].append(("wait", self.sems[sid], v))

    def _deps(self, reads, writes):
        toks = []
        for b in reads:
            toks.append(b.w)
        for b in writes:
            toks.append(b.w)
            toks.extend(b.r)
        return toks

    def op(self, eng, fn, reads=(), writes=()):
        self._need(eng, self._deps(reads, writes))
        if self.cnt[eng] >= EPOCH:
            self.epoch_sem[eng] = self._newsem()
            self.cnt[eng] = 0
        self.cnt[eng] += 1
        tok = (self.epoch_sem[eng], self.cnt[eng])
        self.prog[eng].append(("op", fn, tok[0], 1))
        for b in reads:
            b.r.append(tok)
        for b in writes:
            b.w = tok
            b.r = []
        return tok

    def dma(self, eng, fn, reads=(), writes=(), inc=16):
        slot = self.dma_rr
        self.dma_rr = (self.dma_rr + 1) % NDMASEM
        s = self.dma_sems[slot]
        prev = self.dma_cnt[slot]
        toks = self._deps(reads, writes)
        if prev > 0:
            toks.append((s, prev))
        self._need(eng, toks)
        self.dma_cnt[slot] = prev + inc
        tok = (s, prev + inc)
        self.prog[eng].append(("op", fn, s, inc))
        for b in reads:
            b.r.append(tok)
        for b in writes:
            b.w = tok
            b.r = []
        return tok

    def barrier(self):
        toks = []
        for e in ENGS:
            if self.cnt[e] > 0:
                toks.append((self.epoch_sem[e], self.cnt[e]))
        for i in range(NDMASEM):
            if self.dma_cnt[i] > 0:
                toks.append((self.dma_sems[i], self.dma_cnt[i]))
        for e in ENGS:
            self._need(e, list(toks))

    def finish(self, final_bufs):
        toks = []
        for b in final_bufs:
            toks.append(b.w)
        self._need("sp", toks)
        nc = self.nc
        with nc.Block() as block:
            def run(engname):
                def body(e):
                    for item in self.prog[engname]:
                        if item[0] == "wait":
                            e.wait_ge(item[1], item[2])
                        else:
                            ins = item[1](e)
                            ins.then_inc(item[2], item[3])
                return body
            block.tensor(run("pe"))
            block.scalar(run("act"))
            block.vector(run("dve"))
            block.gpsimd(run("pool"))
            block.sync(run("sp"))
        self.stack.close()
        return nc


from concourse.bass_utils import run_bass_kernel_spmd

D = 2048
DEPTH = 4
NCORE = 8
TOK = 1024
ALPHA = (2 * DEPTH) ** 0.25
NEG = -1.0e30


def _tile_rows(v):
    return np.ascontiguousarray(np.broadcast_to(np.asarray(v, np.float32)[None, :], (128, v.shape[-1])))


def _kmajor(w):
    K, N = w.shape
    return np.ascontiguousarray(w.reshape(K // 128, 128, N).transpose(1, 0, 2))


def build_ada():
    fw = FW(); nc = fw.nc
    cT = nc.dram_tensor("cT", [128, 16, 4], F32, kind="ExternalInput")
    adaw = nc.dram_tensor("adaw", [DEPTH, 128, 16, 1536], F32, kind="ExternalInput")
    adab = nc.dram_tensor("adab", [DEPTH, 4, 1536], F32, kind="ExternalInput")
    mod = nc.dram_tensor("mod", [DEPTH, 4, 1536], F32, kind="ExternalOutput")
    B_mod = Buf("mod")
    ct = fw.sbuf([128, 16, 4]); B_ct = Buf("ct")
    ca = fw.sbuf([128, 16, 4]); B_ca = Buf("ca")
    wts = [fw.sbuf([128, 16, 512], name=f"w{i}") for i in range(2)]; B_w = [Buf("w0"), Buf("w1")]
    bt = fw.sbuf([4, DEPTH, 1536]); B_bt = Buf("bt")
    ots = [fw.sbuf([4, 512], name=f"o{i}") for i in range(2)]; B_o = [Buf("o0"), Buf("o1")]
    pss = [fw.psum([128, 512], name=f"p{i}") for i in range(2)]; B_p = [Buf("p0"), Buf("p1")]
    fw.dma("sp", lambda e: e.dma_start(out=ct[:], in_=cT[:, :, :]), writes=[B_ct])
    for l in range(DEPTH):
        fw.dma("sp", lambda e, l=l: e.dma_start(out=bt[:, l, :], in_=adab[l, :, :]), writes=[B_bt])
    fw.op("act", lambda e: e.activation(out=ca[:], in_=ct[:], func=AF.Silu), reads=[B_ct], writes=[B_ca])
    it = 0
    for l in range(DEPTH):
        for n in range(3):
            wt, Bw = wts[it % 2], B_w[it % 2]
            ps, Bp = pss[it % 2], B_p[it % 2]
            ot, Bo = ots[it % 2], B_o[it % 2]
            fw.dma("sp" if it % 2 == 0 else "act", lambda e, l=l, n=n, wt=wt: e.dma_start(out=wt[:], in_=adaw[l, :, :, n * 512:(n + 1) * 512]), writes=[Bw])
            for k in range(16):
                fw.op("pe", lambda e, k=k, wt=wt, ps=ps: e.matmul(ps[0:4, :], lhsT=ca[:, k, :], rhs=wt[:, k, :], start=(k == 0), stop=(k == 15)),
                      reads=[B_ca, Bw], writes=[Bp] if k in (0, 15) else [])
            fw.op("dve", lambda e, l=l, n=n, ps=ps, ot=ot: e.tensor_tensor(out=ot[:], in0=ps[0:4, :], in1=bt[:, l, n * 512:(n + 1) * 512], op=ALU.add),
                  reads=[Bp, B_bt], writes=[Bo])
            fw.dma("sp", lambda e, l=l, n=n, ot=ot: e.dma_start(out=mod[l, :, n * 512:(n + 1) * 512], in_=ot[:]), reads=[Bo], writes=[B_mod])
            it += 1
    return fw.finish([B_mod])


def build_post(mode, nparts=1, want_h=True):
    fw = FW(); nc = fw.nc
    NT = TOK // 128
    x = nc.dram_tensor("x", [TOK, D], F32, kind="ExternalInput")
    rows = nc.dram_tensor("rows", [5, D], F32, kind="ExternalInput")
    finals = []
    if want_h:
        ho = nc.dram_tensor("ho", [TOK, D], F32, kind="ExternalOutput"); B_ho = Buf("ho")
        finals.append(B_ho)
    if mode != "mod":
        xo = nc.dram_tensor("xo", [TOK, D], F32, kind="ExternalOutput"); B_xo = Buf("xo")
        finals.append(B_xo)
    if mode == "wo":
        fT = nc.dram_tensor("fT", [128, 16, TOK], F32, kind="ExternalInput")
        wo = nc.dram_tensor("wo", [128, 16, D], F32, kind="ExternalInput")
        wot = fw.sbuf([128, 16, D]); B_wo = Buf("wo")
        fw.dma("sp", lambda e: e.dma_start(out=wot[:, 0:8, :], in_=wo[:, 0:8, :]), writes=[B_wo])
        fw.dma("act", lambda e: e.dma_start(out=wot[:, 8:16, :], in_=wo[:, 8:16, :]), writes=[B_wo])
        ft = fw.sbuf([128, 16, 128]); B_ft = Buf("ft")
        pss = [fw.psum([128, 512], name=f"p{i}") for i in range(4)]; B_p = [Buf(f"p{i}") for i in range(4)]
    if mode == "y":
        yp = nc.dram_tensor("yp", [nparts, TOK, D], F32, kind="ExternalInput")
        ypt = fw.sbuf([128, D]); B_ypt = Buf("ypt")
    rt = fw.sbuf([128, 5, D]); B_rt = Buf("rt")
    for j in range(5):
        fw.dma("pool", lambda e, j=j: e.dma_start(out=rt[:, j, :], in_=rows[j:j + 1, :].partition_broadcast(128)), writes=[B_rt])
    fw.op("pool", lambda e: e.tensor_scalar_add(out=rt[:, 0, :], in0=rt[:, 0, :], scalar1=1.0), reads=[B_rt], writes=[B_rt])
    fw.op("pool", lambda e: e.tensor_scalar_add(out=rt[:, 3, :], in0=rt[:, 3, :], scalar1=1.0), reads=[B_rt], writes=[B_rt])
    xt = fw.sbuf([128, D]); B_xt = Buf("xt")
    yt = fw.sbuf([128, D]); B_yt = Buf("yt")
    ht = fw.sbuf([128, D]); B_ht = Buf("ht")
    st = fw.sbuf([128, 4, 6]); B_st = Buf("st")
    mv = fw.sbuf([128, 2]); B_mv = Buf("mv")
    rs = fw.sbuf([128, 1]); B_rs = Buf("rs")
    for t in range(NT):
        ts_ = slice(t * 128, (t + 1) * 128)
        fw.dma("sp", lambda e, ts_=ts_: e.dma_start(out=xt[:], in_=x[ts_, :]), writes=[B_xt])
        if mode == "mod":
            src, B_src = xt, B_xt
        else:
            if mode == "wo":
                fw.dma("act", lambda e, ts_=ts_: e.dma_start(out=ft[:], in_=fT[:, :, ts_]), writes=[B_ft])
                for c in range(4):
                    for k in range(16):
                        fw.op("pe", lambda e, c=c, k=k: e.matmul(pss[c][:, :], lhsT=ft[:, k, :], rhs=wot[:, k, c * 512:(c + 1) * 512], start=(k == 0), stop=(k == 15)),
                              reads=[B_ft, B_wo], writes=[B_p[c]] if k in (0, 15) else [])
                    fw.op("dve", lambda e, c=c: e.tensor_tensor(out=yt[:, c * 512:(c + 1) * 512], in0=pss[c][:, :], in1=rt[:, 0, c * 512:(c + 1) * 512], op=ALU.mult),
                          reads=[B_p[c], B_rt], writes=[B_yt])
            else:
                fw.dma("act", lambda e, ts_=ts_: e.dma_start(out=yt[:], in_=yp[0, ts_, :]), writes=[B_yt])
                for p in range(1, nparts):
                    fw.dma("act", lambda e, ts_=ts_, p=p: e.dma_start(out=ypt[:], in_=yp[p, ts_, :]), writes=[B_ypt])
                    fw.op("pool", lambda e: e.tensor_tensor(out=yt[:], in0=yt[:], in1=ypt[:], op=ALU.add), reads=[B_yt, B_ypt], writes=[B_yt])
                fw.op("dve", lambda e: e.tensor_tensor(out=yt[:], in0=yt[:], in1=rt[:, 0, :], op=ALU.mult), reads=[B_yt, B_rt], writes=[B_yt])
            fw.op("dve", lambda e: e.scalar_tensor_tensor(out=yt[:], in0=xt[:], scalar=ALPHA, in1=yt[:], op0=ALU.mult, op1=ALU.add),
                  reads=[B_xt, B_yt], writes=[B_yt])
            for c in range(4):
                fw.op("dve", lambda e, c=c: e.bn_stats(out=st[:, c, :], in_=yt[:, c * 512:(c + 1) * 512]), reads=[B_yt], writes=[B_st])
            fw.op("dve", lambda e: e.bn_aggr(out=mv[:], in_=st[:].rearrange("p a b -> p (a b)")), reads=[B_st], writes=[B_mv])
            fw.op("dve", lambda e: e.tensor_scalar_add(out=rs[:], in0=mv[:, 1:2], scalar1=1e-5), reads=[B_mv], writes=[B_rs])
            fw.op("act", lambda e: e.activation(out=rs[:], in_=rs[:], func=AF.Sqrt), reads=[B_rs], writes=[B_rs])
            fw.op("dve", lambda e: e.reciprocal(out=rs[:], in_=rs[:]), reads=[B_rs], writes=[B_rs])
            fw.op("dve", lambda e: e.tensor_scalar(out=yt[:], in0=yt[:], scalar1=mv[:, 0:1], scalar2=rs[:, 0:1], op0=ALU.subtract, op1=ALU.mult),
                  reads=[B_yt, B_mv, B_rs], writes=[B_yt])
            fw.op("pool", lambda e: e.tensor_tensor(out=yt[:], in0=yt[:], in1=rt[:, 1, :], op=ALU.mult), reads=[B_yt, B_rt], writes=[B_yt])
            fw.op("pool", lambda e: e.tensor_tensor(out=yt[:], in0=yt[:], in1=rt[:, 2, :], op=ALU.add), reads=[B_yt, B_rt], writes=[B_yt])
            fw.dma("sp", lambda e, ts_=ts_: e.dma_start(out=xo[ts_, :], in_=yt[:]), reads=[B_yt], writes=[B_xo])
            src, B_src = yt, B_yt
        if want_h:
            fw.op("dve", lambda e, src=src: e.tensor_tensor(out=ht[:], in0=src[:], in1=rt[:, 3, :], op=ALU.mult), reads=[B_src, B_rt], writes=[B_ht])
            fw.op("dve", lambda e: e.tensor_tensor(out=ht[:], in0=ht[:], in1=rt[:, 4, :], op=ALU.add), reads=[B_ht, B_rt], writes=[B_ht])
            fw.dma("sp", lambda e, ts_=ts_: e.dma_start(out=ho[ts_, :], in_=ht[:]), reads=[B_ht], writes=[B_ho])
    return fw.finish(finals)


def build_route():
    fw = FW(); nc = fw.nc
    NT = TOK // 128
    hT = nc.dram_tensor("hT", [128, 16, TOK], F32, kind="ExternalInput")
    wge = nc.dram_tensor("wge", [128, 16, 36], F32, kind="ExternalInput")
    bge = nc.dram_tensor("bge", [128, 36], F32, kind="ExternalInput")
    G = nc.dram_tensor("G", [TOK, 32], F32, kind="ExternalOutput"); B_G = Buf("G")
    MG = nc.dram_tensor("MG", [TOK, 4], F32, kind="ExternalOutput"); B_MG = Buf("MG")
    wt = fw.sbuf([128, 16, 36]); B_wt = Buf("wt")
    bt = fw.sbuf([128, 36]); B_bt = Buf("bt")
    fw.dma("sp", lambda e: e.dma_start(out=wt[:], in_=wge[:, :, :]), writes=[B_wt])
    fw.dma("sp", lambda e: e.dma_start(out=bt[:], in_=bge[:, :]), writes=[B_bt])
    ht = fw.sbuf([128, 16, 128]); B_ht = Buf("ht")
    ps = fw.psum([128, 512]); B_ps = Buf("ps")
    def sb(shape, name):
        return fw.sbuf(shape, name=name), Buf(name)
    lg, B_lg = sb([128, 36], "lg")
    mx, B_mx = sb([128, 1], "mx"); nmx, B_nmx = sb([128, 1], "nmx")
    mg, B_mg = sb([128, 4], "mg"); eg, B_eg = sb([128, 4], "eg")
    sm, B_sm = sb([128, 1], "sm"); pt, B_pt = sb([128, 1], "pt")
    pen, B_pen = sb([128, 4], "pen")
    lem, B_lem = sb([128, 32], "lem"); lem2, B_lem2 = sb([128, 32], "lem2")
    m1, B_m1 = sb([128, 1], "m1"); m2, B_m2 = sb([128, 1], "m2")
    k1, B_k1 = sb([128, 32], "k1"); k2, B_k2 = sb([128, 32], "k2")
    dd, B_dd = sb([128, 1], "dd"); ed, B_ed = sb([128, 1], "ed")
    w1, B_w1 = sb([128, 1], "w1"); w2, B_w2 = sb([128, 1], "w2")
    gt, B_gt = sb([128, 32], "gt")
    for t in range(NT):
        ts_ = slice(t * 128, (t + 1) * 128)
        fw.dma("sp", lambda e, ts_=ts_: e.dma_start(out=ht[:], in_=hT[:, :, ts_]), writes=[B_ht])
        for k in range(16):
            fw.op("pe", lambda e, k=k: e.matmul(ps[:, 0:36], lhsT=ht[:, k, :], rhs=wt[:, k, :], start=(k == 0), stop=(k == 15)),
                  reads=[B_ht, B_wt], writes=[B_ps] if k in (0, 15) else [])
        fw.op("dve", lambda e: e.tensor_tensor(out=lg[:], in0=ps[:, 0:36], in1=bt[:], op=ALU.add), reads=[B_ps, B_bt], writes=[B_lg])
        fw.op("dve", lambda e: e.reduce_max(out=mx[:], in_=lg[:, 0:4], axis=AX.X), reads=[B_lg], writes=[B_mx])
        fw.op("dve", lambda e: e.tensor_scalar(out=mg[:], in0=lg[:, 0:4], scalar1=mx[:, 0:1], scalar2=None, op0=ALU.is_equal), reads=[B_lg, B_mx], writes=[B_mg])
        fw.op("dve", lambda e: e.tensor_scalar_mul(out=nmx[:], in0=mx[:], scalar1=-1.0), reads=[B_mx], writes=[B_nmx])
        fw.op("act", lambda e: e.activation(out=eg[:], in_=lg[:, 0:4], func=AF.Exp, bias=nmx[:, 0:1], scale=1.0), reads=[B_lg, B_nmx], writes=[B_eg])
        fw.op("dve", lambda e: e.reduce_sum(out=sm[:], in_=eg[:], axis=AX.X), reads=[B_eg], writes=[B_sm])
        fw.op("dve", lambda e: e.reciprocal(out=pt[:], in_=sm[:]), reads=[B_sm], writes=[B_pt])
        fw.op("dve", lambda e: e.tensor_scalar(out=pen[:], in0=mg[:], scalar1=-1.0, scalar2=-NEG, op0=ALU.add, op1=ALU.mult), reads=[B_mg], writes=[B_pen])
        fw.op("dve", lambda e: e.tensor_tensor(out=lem[:].rearrange("p (g e) -> p g e", g=4), in0=lg[:, 4:36].rearrange("p (g e) -> p g e", g=4),
                                               in1=pen[:].unsqueeze(2).to_broadcast([128, 4, 8]), op=ALU.add), reads=[B_lg, B_pen], writes=[B_lem])
        fw.op("dve", lambda e: e.reduce_max(out=m1[:], in_=lem[:], axis=AX.X), reads=[B_lem], writes=[B_m1])
        fw.op("dve", lambda e: e.tensor_scalar(out=k1[:], in0=lem[:], scalar1=m1[:, 0:1], scalar2=None, op0=ALU.is_equal), reads=[B_lem, B_m1], writes=[B_k1])
        fw.op("dve", lambda e: e.scalar_tensor_tensor(out=lem2[:], in0=k1[:], scalar=NEG, in1=lem[:], op0=ALU.mult, op1=ALU.add), reads=[B_k1, B_lem], writes=[B_lem2])
        fw.op("dve", lambda e: e.reduce_max(out=m2[:], in_=lem2[:], axis=AX.X), reads=[B_lem2], writes=[B_m2])
        fw.op("dve", lambda e: e.tensor_scalar(out=k2[:], in0=lem2[:], scalar1=m2[:, 0:1], scalar2=None, op0=ALU.is_equal), reads=[B_lem2, B_m2], writes=[B_k2])
        fw.op("dve", lambda e: e.tensor_tensor(out=dd[:], in0=m2[:], in1=m1[:], op=ALU.subtract), reads=[B_m1, B_m2], writes=[B_dd])
        fw.op("act", lambda e: e.activation(out=ed[:], in_=dd[:], func=AF.Exp), reads=[B_dd], writes=[B_ed])
        fw.op("dve", lambda e: e.tensor_scalar_add(out=w1[:], in0=ed[:], scalar1=1.0), reads=[B_ed], writes=[B_w1])
        fw.op("dve", lambda e: e.reciprocal(out=w1[:], in_=w1[:]), reads=[B_w1], writes=[B_w1])
        fw.op("dve", lambda e: e.tensor_tensor(out=w2[:], in0=ed[:], in1=w1[:], op=ALU.mult), reads=[B_ed, B_w1], writes=[B_w2])
        fw.op("dve", lambda e: e.tensor_tensor(out=w1[:], in0=w1[:], in1=pt[:], op=ALU.mult), reads=[B_w1, B_pt], writes=[B_w1])
        fw.op("dve", lambda e: e.tensor_tensor(out=w2[:], in0=w2[:], in1=pt[:], op=ALU.mult), reads=[B_w2, B_pt], writes=[B_w2])
        fw.op("dve", lambda e: e.tensor_scalar(out=gt[:], in0=k1[:], scalar1=w1[:, 0:1], scalar2=None, op0=ALU.mult), reads=[B_k1, B_w1], writes=[B_gt])
        fw.op("dve", lambda e: e.scalar_tensor_tensor(out=gt[:], in0=k2[:], scalar=w2[:, 0:1], in1=gt[:], op0=ALU.mult, op1=ALU.add), reads=[B_k2, B_w2, B_gt], writes=[B_gt])
        fw.dma("sp", lambda e, ts_=ts_: e.dma_start(out=G[ts_, :], in_=gt[:]), reads=[B_gt], writes=[B_G])
        fw.dma("sp", lambda e, ts_=ts_: e.dma_start(out=MG[ts_, :], in_=mg[:]), reads=[B_mg], writes=[B_MG])
    return fw.finish([B_G, B_MG])


def build_moe(ntok):
    fw = FW(); nc = fw.nc
    NE = 4
    TB = 512
    NB = ntok // TB
    hT = nc.dram_tensor("hT", [128, 16, ntok], F32, kind="ExternalInput")
    gb = nc.dram_tensor("gb", [NE, ntok], F32, kind="ExternalInput")
    w1 = nc.dram_tensor("w1", [NE, 128, 16, 512], F32, kind="ExternalInput")
    w3 = nc.dram_tensor("w3", [NE, 128, 16, 512], F32, kind="ExternalInput")
    w2 = nc.dram_tensor("w2", [NE, 128, 4, D], F32, kind="ExternalInput")
    y = nc.dram_tensor("y", [ntok, D], F32, kind="ExternalOutput"); B_y = Buf("y")
    def sb(shape, name):
        return fw.sbuf(shape, name=name), Buf(name)
    w1t, B_w1 = sb([128, 16, 512], "w1t"); w3t, B_w3 = sb([128, 16, 512], "w3t"); w2t, B_w2 = sb([128, 4, D], "w2t")
    ht, B_ht = sb([128, 16, TB], "ht")
    gt, B_gt = sb([128, TB], "gt")
    a1, B_a1 = sb([128, TB], "a1")
    at, B_at = sb([128, 4, TB], "at")
    ya, B_ya = sb([128, 4, D], "ya")
    pA = [fw.psum([128, 512], name=f"pa{i}") for i in range(2)]; B_pA = [Buf("pa0"), Buf("pa1")]
    pB = [fw.psum([128, 512], name=f"pb{i}") for i in range(2)]; B_pB = [Buf("pb0"), Buf("pb1")]
    pY = [fw.psum([128, 512], name=f"py{i}") for i in range(4)]; B_pY = [Buf(f"py{i}") for i in range(4)]
    for b in range(NB):
        bs = slice(b * TB, (b + 1) * TB)
        fw.dma("sp", lambda e, bs=bs: e.dma_start(out=ht[:, 0:8, :], in_=hT[:, 0:8, bs]), writes=[B_ht])
        fw.dma("act", lambda e, bs=bs: e.dma_start(out=ht[:, 8:16, :], in_=hT[:, 8:16, bs]), writes=[B_ht])
        for ex in range(NE):
            fw.dma("sp", lambda e, ex=ex: e.dma_start(out=w1t[:], in_=w1[ex, :, :, :]), writes=[B_w1])
            fw.dma("act", lambda e, ex=ex: e.dma_start(out=w3t[:], in_=w3[ex, :, :, :]), writes=[B_w3])
            fw.dma("pool", lambda e, ex=ex: e.dma_start(out=w2t[:], in_=w2[ex, :, :, :]), writes=[B_w2])
            fw.dma("pool", lambda e, ex=ex, bs=bs: e.dma_start(out=gt[:], in_=gb[ex:ex + 1, bs].partition_broadcast(128)), writes=[B_gt])
            for f in range(4):
                pa, Bpa = pA[f % 2], B_pA[f % 2]
                pb, Bpb = pB[f % 2], B_pB[f % 2]
                for k in range(16):
                    fw.op("pe", lambda e, k=k, f=f, pa=pa: e.matmul(pa[:, :], lhsT=w1t[:, k, f * 128:(f + 1) * 128], rhs=ht[:, k, :], start=(k == 0), stop=(k == 15)),
                          reads=[B_w1, B_ht], writes=[Bpa] if k in (0, 15) else [])
                for k in range(16):
                    fw.op("pe", lambda e, k=k, f=f, pb=pb: e.matmul(pb[:, :], lhsT=w3t[:, k, f * 128:(f + 1) * 128], rhs=ht[:, k, :], start=(k == 0), stop=(k == 15)),
                          reads=[B_w3, B_ht], writes=[Bpb] if k in (0, 15) else [])
                fw.op("act", lambda e, pa=pa: e.activation(out=a1[:], in_=pa[:, :], func=AF.Silu), reads=[Bpa], writes=[B_a1])
                fw.op("dve", lambda e, pb=pb, f=f: e.tensor_tensor(out=at[:, f, :], in0=pb[:, :], in1=a1[:], op=ALU.mult), reads=[Bpb, B_a1], writes=[B_at])
                fw.op("pool", lambda e, f=f: e.tensor_tensor(out=at[:, f, :], in0=at[:, f, :], in1=gt[:], op=ALU.mult), reads=[B_at, B_gt], writes=[B_at])
            for tt in range(4):
                for c in range(4):
                    for f in range(4):
                        fw.op("pe", lambda e, tt=tt, c=c, f=f: e.matmul(pY[c][:, :], lhsT=at[:, f, tt * 128:(tt + 1) * 128], rhs=w2t[:, f, c * 512:(c + 1) * 512], start=(f == 0), stop=(f == 3)),
                              reads=[B_at, B_w2], writes=[B_pY[c]] if f in (0, 3) else [])
                    if ex == 0:
                        fw.op("act", lambda e, tt=tt, c=c: e.copy(out=ya[:, tt, c * 512:(c + 1) * 512], in_=pY[c][:, :]), reads=[B_pY[c]], writes=[B_ya])
                    else:
                        fw.op("dve", lambda e, tt=tt, c=c: e.tensor_tensor(out=ya[:, tt, c * 512:(c + 1) * 512], in0=pY[c][:, :], in1=ya[:, tt, c * 512:(c + 1) * 512], op=ALU.add),
                              reads=[B_pY[c], B_ya], writes=[B_ya])
        for tt in range(4):
            fw.dma("sp", lambda e, tt=tt, b=b: e.dma_start(out=y[b * TB + tt * 128: b * TB + (tt + 1) * 128, :], in_=ya[:, tt, :]), reads=[B_ya], writes=[B_y])
    return fw.finish([B_y])


T_SEQ = 2048


def build_gla():
    fw = FW(); nc = fw.nc
    T = T_SEQ
    hT = nc.dram_tensor("hT", [128, 16, T], F32, kind="ExternalInput")
    Wfm = nc.dram_tensor("Wfm", [2, 128, 16, 512], F32, kind="ExternalInput")
    Wtm = nc.dram_tensor("Wtm", [5, 128, 16, 512], F32, kind="ExternalInput")
    Wad = nc.dram_tensor("Wad", [128, 16, 16], F32, kind="ExternalInput")
    a2 = nc.dram_tensor("a2", [16, 512], F32, kind="ExternalInput")
    rowsd = nc.dram_tensor("rowsd", [2, 1024], F32, kind="ExternalInput")
    tri = nc.dram_tensor("tri", [128, 128], F32, kind="ExternalInput")
    feat = nc.dram_tensor("feat", [T, 1024], F32, kind="ExternalOutput"); B_feat = Buf("feat")
    PT = nc.dram_tensor("PT", [1024, T], F32); B_PT = Buf("PT")
    PM = nc.dram_tensor("PM", [T, 2560], F32); B_PM = Buf("PM")

    def sb(shape, name):
        return fw.sbuf(shape, name=name), Buf(name)
    hTb, B_hTb = sb([128, 16, 512], "hTb")
    wb = [sb([128, 16, 512], f"wb{i}") for i in range(2)]
    wad, B_wad = sb([128, 16, 16], "wad")
    adT, B_adT = sb([16, T], "adT")
    a2t, B_a2t = sb([16, 512], "a2t")
    abr, B_abr = sb([128, 512], "abr")
    ngr, B_ngr = sb([128, 1024], "ngr")
    trit, B_tri = sb([128, 128], "trit")
    ev = [sb([128, 512], f"ev{i}") for i in range(2)]
    pp = [(fw.psum([128, 512], name=f"pp{i}"), Buf(f"pp{i}")) for i in range(8)]
    fw.dma("pool", lambda e: e.dma_start(out=wad[:], in_=Wad[:, :, :]), writes=[B_wad])
    fw.dma("pool", lambda e: e.dma_start(out=a2t[:], in_=a2[:, :]), writes=[B_a2t])
    fw.dma("pool", lambda e: e.dma_start(out=abr[:], in_=rowsd[0:1, 0:512].partition_broadcast(128)), writes=[B_abr])
    fw.dma("pool", lambda e: e.dma_start(out=ngr[:], in_=rowsd[1:2, :].partition_broadcast(128)), writes=[B_ngr])
    fw.dma("pool", lambda e: e.dma_start(out=trit[:], in_=tri[:, :]), writes=[B_tri])
    it = 0
    for tb in range(T // 512):
        tbs = slice(tb * 512, (tb + 1) * 512)
        fw.dma("sp", lambda e, tbs=tbs: e.dma_start(out=hTb[:, 0:8, :], in_=hT[:, 0:8, tbs]), writes=[B_hTb])
        fw.dma("act", lambda e, tbs=tbs: e.dma_start(out=hTb[:, 8:16, :], in_=hT[:, 8:16, tbs]), writes=[B_hTb])
        p, Bp = pp[it % 8]; it += 1
        for kc in range(16):
            fw.op("pe", lambda e, kc=kc, p=p: e.matmul(p[0:16, :], lhsT=wad[:, kc, :], rhs=hTb[:, kc, :], start=(kc == 0), stop=(kc == 15)),
                  reads=[B_wad, B_hTb], writes=[Bp] if kc in (0, 15) else [])
        fw.op("act", lambda e, p=p, tbs=tbs: e.copy(out=adT[:, tbs], in_=p[0:16, :]), reads=[Bp], writes=[B_adT])
        for blk in range(7):
            w, Bw = wb[blk % 2]
            src = Wfm[blk] if blk < 2 else Wtm[blk - 2]
            fw.dma("sp", lambda e, w=w, src=src: e.dma_start(out=w[:, 0:8, :], in_=src[:, 0:8, :]), writes=[Bw])
            fw.dma("act", lambda e, w=w, src=src: e.dma_start(out=w[:, 8:16, :], in_=src[:, 8:16, :]), writes=[Bw])
            for sub in range(4):
                p, Bp = pp[it % 8]
                evt, Bev = ev[it % 2]
                it += 1
                if blk < 2:
                    for kc in range(16):
                        fw.op("pe", lambda e, kc=kc, p=p, w=w, sub=sub: e.matmul(p[:, :], lhsT=w[:, kc, sub * 128:(sub + 1) * 128], rhs=hTb[:, kc, :], start=(kc == 0), stop=(kc == 15)),
                              reads=[Bw, B_hTb], writes=[Bp] if kc in (0, 15) else [])
                    sc = 0.0625 if blk == 0 else 1.0
                    fw.op("act", lambda e, p=p, evt=evt, sc=sc: e.mul(out=evt[:], in_=p[:, :], mul=sc), reads=[Bp], writes=[Bev])
                    r0 = blk * 512 + sub * 128
                    fw.dma("pool", lambda e, evt=evt, r0=r0, tbs=tbs: e.dma_start(out=PT[r0:r0 + 128, tbs], in_=evt[:]), reads=[Bev], writes=[B_PT])
                else:
                    for kc in range(16):
                        fw.op("pe", lambda e, kc=kc, p=p, w=w, sub=sub: e.matmul(p[:, :], lhsT=hTb[:, kc, sub * 128:(sub + 1) * 128], rhs=w[:, kc, :], start=(kc == 0), stop=(kc == 15)),
                              reads=[Bw, B_hTb], writes=[Bp] if kc in (0, 15) else [])
                    fw.op("act", lambda e, p=p, evt=evt: e.copy(out=evt[:], in_=p[:, :]), reads=[Bp], writes=[Bev])
                    t0 = tb * 512 + sub * 128
                    c0 = (blk - 2) * 512
                    fw.dma("pool", lambda e, evt=evt, t0=t0, c0=c0: e.dma_start(out=PM[t0:t0 + 128, c0:c0 + 512], in_=evt[:]), reads=[Bev], writes=[B_PM])
    S = [[sb([128, 512], f"S{h}{c}") for c in range(2)] for h in range(2)]
    for h in range(2):
        for c in range(2):
            fw.op("pool", lambda e, t=S[h][c][0]: e.memset(t[:], 0.0), writes=[S[h][c][1]])
    qT, B_qT = sb([128, 4, 128], "qT"); kT, B_kT = sb([128, 4, 128], "kT")
    km, B_km = sb([128, 512], "km"); vm, B_vm = sb([128, 1024], "vm"); rm, B_rm = sb([128, 1024], "rm")
    la, B_la = sb([128, 512], "la")
    eL, B_eL = sb([128, 512], "eL")
    eP, B_eP = sb([128, 4, 128], "eP"); eN, B_eN = sb([128, 4, 128], "eN")
    aT, B_aT = sb([128, 128], "aT")
    o, B_o = sb([128, 512], "o")
    st, B_st = sb([128, 6], "st"); mv, B_mv = sb([128, 2], "mv"); rs, B_rs = sb([128, 1], "rs")
    fo, B_fo = sb([128, 1024], "fo")
    pz, B_pz = pp[0]; pL, B_pL = pp[1]; pLT, B_pLT = pp[2]; pS, B_pS = pp[3]; pO, B_pO = pp[4]; pU, B_pU = pp[5]
    for n in range(T // 128):
        ts_ = slice(n * 128, (n + 1) * 128)
        fw.dma("sp", lambda e, ts_=ts_: e.dma_start(out=qT[:], in_=PT[0:512, ts_].rearrange("(c p) t -> p c t", p=128)), reads=[B_PT], writes=[B_qT])
        fw.dma("sp", lambda e, ts_=ts_: e.dma_start(out=kT[:], in_=PT[512:1024, ts_].rearrange("(c p) t -> p c t", p=128)), reads=[B_PT], writes=[B_kT])
        fw.dma("act", lambda e, ts_=ts_: e.dma_start(out=km[:], in_=PM[ts_, 0:512]), reads=[B_PM], writes=[B_km])
        fw.dma("act", lambda e, ts_=ts_: e.dma_start(out=vm[:], in_=PM[ts_, 512:1536]), reads=[B_PM], writes=[B_vm])
        fw.dma("act", lambda e, ts_=ts_: e.dma_start(out=rm[:], in_=PM[ts_, 1536:2560]), reads=[B_PM], writes=[B_rm])
        fw.op("pe", lambda e, ts_=ts_: e.matmul(pz[:, :], lhsT=adT[:, ts_], rhs=a2t[:, :], start=True, stop=True), reads=[B_adT, B_a2t], writes=[B_pz])
        fw.op("dve", lambda e: e.tensor_tensor(out=la[:], in0=pz[:, :], in1=abr[:], op=ALU.add), reads=[B_pz, B_abr], writes=[B_la])
        fw.op("act", lambda e: e.activation(out=la[:], in_=la[:], func=AF.Exp, scale=-1.0), reads=[B_la], writes=[B_la])
        fw.op("dve", lambda e: e.tensor_scalar_add(out=la[:], in0=la[:], scalar1=1.0), reads=[B_la], writes=[B_la])
        fw.op("act", lambda e: e.activation(out=la[:], in_=la[:], func=AF.Ln), reads=[B_la], writes=[B_la])
        fw.op("dve", lambda e: e.tensor_scalar_mul(out=la[:], in0=la[:], scalar1=1.0 / 16.0), reads=[B_la], writes=[B_la])
        fw.op("pe", lambda e: e.matmul(pL[:, :], lhsT=trit[:, :], rhs=la[:, :], start=True, stop=True), reads=[B_tri, B_la], writes=[B_pL])
        fw.op("act", lambda e: e.activation(out=eL[:], in_=pL[:, :], func=AF.Exp), reads=[B_pL], writes=[B_eL])
        fw.op("dve", lambda e: e.tensor_tensor(out=km[:], in0=km[:], in1=eL[:], op=ALU.mult), reads=[B_km, B_eL], writes=[B_km])
        for cc in range(4):
            fw.op("pe", lambda e, cc=cc: e.matmul(pLT[:, cc * 128:(cc + 1) * 128], lhsT=la[:, cc * 128:(cc + 1) * 128], rhs=trit[:, :], start=True, stop=True),
                  reads=[B_la, B_tri], writes=[B_pLT])
        fw.op("act", lambda e: e.activation(out=eN[:].rearrange("p c t -> p (c t)"), in_=pLT[:, :], func=AF.Exp), reads=[B_pLT], writes=[B_eN])
        fw.op("act", lambda e: e.activation(out=eP[:].rearrange("p c t -> p (c t)"), in_=pLT[:, :], func=AF.Exp, scale=-1.0), reads=[B_pLT], writes=[B_eP])
        fw.op("dve", lambda e: e.tensor_tensor(out=qT[:], in0=qT[:], in1=eP[:], op=ALU.mult), reads=[B_qT, B_eP], writes=[B_qT])
        fw.op("pool", lambda e: e.tensor_tensor(out=kT[:], in0=kT[:], in1=eN[:], op=ALU.mult), reads=[B_kT, B_eN], writes=[B_kT])
        fw.op("act", lambda e: e.activation(out=rm[:], in_=rm[:], func=AF.Silu), reads=[B_rm], writes=[B_rm])
        for h in range(2):
            for c in range(2):
                cc = 2 * h + c
                fw.op("pe", lambda e, cc=cc, c=c: e.matmul(pS[:, 0:128], lhsT=kT[:, cc, :], rhs=qT[:, cc, :], start=(c == 0), stop=(c == 1)),
                      reads=[B_kT, B_qT], writes=[B_pS] if True else [])
            fw.op("dve", lambda e: e.tensor_tensor(out=aT[:], in0=pS[:, 0:128], in1=trit[:], op=ALU.mult), reads=[B_pS, B_tri], writes=[B_aT])
            vs = slice(h * 512, (h + 1) * 512)
            fw.op("pe", lambda e, vs=vs: e.matmul(pO[:, :], lhsT=aT[:, :], rhs=vm[:, vs], start=True, stop=False), reads=[B_aT, B_vm], writes=[B_pO])
            for c in range(2):
                cc = 2 * h + c
                fw.op("pe", lambda e, cc=cc, c=c, h=h: e.matmul(pO[:, :], lhsT=qT[:, cc, :], rhs=S[h][c][0][:, :], start=False, stop=(c == 1)),
                      reads=[B_qT, S[h][c][1]], writes=[B_pO])
            fw.op("act", lambda e: e.copy(out=o[:], in_=pO[:, :]), reads=[B_pO], writes=[B_o])
            for c in range(2):
                cc = 2 * h + c
                St, BS = S[h][c]
                fw.op("pe", lambda e, cc=cc, vs=vs: e.matmul(pU[:, :], lhsT=km[:, cc * 128:(cc + 1) * 128], rhs=vm[:, vs], start=True, stop=True), reads=[B_km, B_vm], writes=[B_pU])
                fw.op("dve", lambda e, St=St: e.tensor_tensor(out=St[:], in0=pU[:, :], in1=St[:], op=ALU.add), reads=[B_pU, BS], writes=[BS])
                fw.op("dve", lambda e, St=St, cc=cc: e.tensor_scalar(out=St[:], in0=St[:], scalar1=eP[:, cc, 127:128], scalar2=None, op0=ALU.mult), reads=[BS, B_eP], writes=[BS])
            fw.op("dve", lambda e: e.bn_stats(out=st[:], in_=o[:]), reads=[B_o], writes=[B_st])
            fw.op("dve", lambda e: e.bn_aggr(out=mv[:], in_=st[:]), reads=[B_st], writes=[B_mv])
            fw.op("dve", lambda e: e.tensor_tensor(out=rs[:], in0=mv[:, 0:1], in1=mv[:, 0:1], op=ALU.mult), reads=[B_mv], writes=[B_rs])
            fw.op("dve", lambda e: e.tensor_tensor(out=rs[:], in0=rs[:], in1=mv[:, 1:2], op=ALU.add), reads=[B_rs, B_mv], writes=[B_rs])
            fw.op("dve", lambda e: e.tensor_scalar_add(out=rs[:], in0=rs[:], scalar1=1e-5), reads=[B_rs], writes=[B_rs])
            fw.op("act", lambda e: e.activation(out=rs[:], in_=rs[:], func=AF.Sqrt), reads=[B_rs], writes=[B_rs])
            fw.op("dve", lambda e: e.reciprocal(out=rs[:], in_=rs[:]), reads=[B_rs], writes=[B_rs])
            fw.op("dve", lambda e, vs=vs: e.scalar_tensor_tensor(out=fo[:, vs], in0=o[:], scalar=rs[:, 0:1], in1=ngr[:, vs], op0=ALU.mult, op1=ALU.mult),
                  reads=[B_o, B_rs, B_ngr], writes=[B_fo])
            fw.op("pool", lambda e, vs=vs: e.tensor_tensor(out=fo[:, vs], in0=fo[:, vs], in1=rm[:, vs], op=ALU.mult), reads=[B_fo, B_rm], writes=[B_fo])
        fw.dma("sp", lambda e, ts_=ts_: e.dma_start(out=feat[ts_, :], in_=fo[:]), reads=[B_fo], writes=[B_feat])
    return fw.finish([B_feat])


def gla_inputs(hT_b, g, win, a2, ab, norm_g):
    f = np.float32
    win = np.asarray(win, f)
    q = win[:, 0:1024][:, g * 512:(g + 1) * 512]
    k = win[:, 1024:2048][:, g * 512:(g + 1) * 512]
    v = win[:, 2048:4096][:, g * 1024:(g + 1) * 1024]
    r = win[:, 4096:6144][:, g * 1024:(g + 1) * 1024]
    ad = win[:, 6144:6160]
    Wfm = np.stack([_kmajor(q), _kmajor(k)])
    Wtm = np.stack([_kmajor(k), _kmajor(v[:, :512]), _kmajor(v[:, 512:]), _kmajor(r[:, :512]), _kmajor(r[:, 512:])])
    rowsd = np.zeros((2, 1024), f)
    rowsd[0, :512] = np.asarray(ab, f)[g * 512:(g + 1) * 512]
    rowsd[1] = np.asarray(norm_g, f)[g * 1024:(g + 1) * 1024]
    tri = np.triu(np.ones((128, 128), f))
    return {"hT": hT_b, "Wfm": Wfm, "Wtm": Wtm, "Wad": _kmajor(ad), "a2": np.ascontiguousarray(np.asarray(a2, f)[:, g * 512:(g + 1) * 512]),
            "rowsd": rowsd, "tri": tri}


import math
TWO_PI = 2.0 * math.pi
RW_C = 64
W_SCALE = -math.exp(-0.5)


def build_even(layer2, do_ret=True, do_rw=True):
    fw = FW(); nc = fw.nc
    T = T_SEQ
    NFM = 5 if layer2 else 3
    NTM = 5
    hT = nc.dram_tensor("hT", [128, 16, T], F32, kind="ExternalInput")
    Wfm = nc.dram_tensor("Wfm", [NFM, 128, 16, 512], F32, kind="ExternalInput")
    Wtm = nc.dram_tensor("Wtm", [NTM, 128, 16, 512], F32, kind="ExternalInput")
    mufm = nc.dram_tensor("mufm", [NFM, 512], F32, kind="ExternalInput")
    mutm = nc.dram_tensor("mutm", [NTM, 512], F32, kind="ExternalInput")
    posi = nc.dram_tensor("posi", [1, T], I32, kind="ExternalInput")
    cst = nc.dram_tensor("cst", [128, 8, 128], F32, kind="ExternalInput")
    cst2 = nc.dram_tensor("cst2", [128, 8], F32, kind="ExternalInput")
    rowsd = nc.dram_tensor("rowsd", [12, 512], F32, kind="ExternalInput")
    lw2 = nc.dram_tensor("lw2", [64, 512], F32, kind="ExternalInput")
    la2 = nc.dram_tensor("la2", [64, 512], F32, kind="ExternalInput")
    lg2 = nc.dram_tensor("lg2", [160, 512], F32, kind="ExternalInput")
    if layer2:
        lv1 = nc.dram_tensor("lv1", [128, 8, 32], F32, kind="ExternalInput")
        lv2 = nc.dram_tensor("lv2", [32, 512], F32, kind="ExternalInput")
        vfirst = nc.dram_tensor("vfirst", [T, 512], F32, kind="ExternalInput")
    feat = nc.dram_tensor("feat", [T, 1024], F32, kind="ExternalOutput"); B_feat = Buf("feat")
    finals = [B_feat]
    if not layer2:
        vout = nc.dram_tensor("vout", [T, 512], F32, kind="ExternalOutput"); B_vout = Buf("vout")
        finals.append(B_vout)
    PT = nc.dram_tensor("PT", [NFM * 512, T], F32); B_PT = Buf("PT")
    PM = nc.dram_tensor("PM", [T, NTM * 512], F32); B_PM = Buf("PM")

    ARENA = 50000
    arena = fw.sbuf([128, ARENA], name="arena")
    off = [0]
    def sb(shape, name, dtype=F32):
        n = 1
        for d_ in shape[1:]:
            n *= d_
        assert off[0] + n <= ARENA, (name, off[0], n)
        ap = arena[0:shape[0], off[0]:off[0] + n]
        if len(shape) == 3:
            ap = ap.rearrange("p (a b) -> p a b", a=shape[1])
        if dtype is not F32:
            ap = ap.bitcast(dtype)
        off[0] += n
        return ap, Buf(name)
    def psb(shape, name, dtype=F32):
        return fw.sbuf(shape, name=name, dtype=dtype), Buf(name)
    TBK = 256
    hTb, B_hTb = sb([128, 16, TBK], "hTb")
    hTp, B_hTp = sb([128, 16, TBK], "hTp")
    w1, B_w1 = sb([128, 16, 512], "w1")
    w2, B_w2 = sb([128, 16, 512], "w2")
    mur, B_mur = sb([128, 512], "mur")
    cs, B_cs = psb([128, 8, 128], "cs")
    cs2, B_cs2 = psb([128, 8], "cs2")
    ev = [sb([128, 512], f"ev{i}") for i in range(2)]
    pp = [(fw.psum([128, 512], name=f"pp{i}"), Buf(f"pp{i}")) for i in range(8)]
    fw.dma("pool", lambda e: e.dma_start(out=cs[:], in_=cst[:, :, :]), writes=[B_cs])
    fw.dma("pool", lambda e: e.dma_start(out=cs2[:], in_=cst2[:, :]), writes=[B_cs2])
    TRI, IDN, GP0, GN0, GP1, GN1, TBD, SBD = range(8)
    it = 0
    blocks = [("fm", i) for i in range(NFM)] + [("tm", i) for i in range(NTM)]
    def mixed(kind, i):
        return (kind == "fm" and i >= 2) or (kind == "tm" and i >= 2)
    for tb in range(T // TBK):
        t0 = tb * TBK
        fw.dma("sp", lambda e, t0=t0: e.dma_start(out=hTb[:, 0:8, :], in_=hT[:, 0:8, t0:t0 + TBK]), writes=[B_hTb])
        fw.dma("act", lambda e, t0=t0: e.dma_start(out=hTb[:, 8:16, :], in_=hT[:, 8:16, t0:t0 + TBK]), writes=[B_hTb])
        if tb == 0:
            fw.op("pool", lambda e: e.memset(hTp[:, :, 0:1], 0.0), writes=[B_hTp])
            fw.dma("sp", lambda e: e.dma_start(out=hTp[:, :, 1:TBK], in_=hT[:, :, 0:TBK - 1]), writes=[B_hTp])
        else:
            fw.dma("sp", lambda e, t0=t0: e.dma_start(out=hTp[:, 0:8, :], in_=hT[:, 0:8, t0 - 1:t0 + TBK - 1]), writes=[B_hTp])
            fw.dma("act", lambda e, t0=t0: e.dma_start(out=hTp[:, 8:16, :], in_=hT[:, 8:16, t0 - 1:t0 + TBK - 1]), writes=[B_hTp])
        for (kind, bi) in blocks:
            src = Wfm[bi] if kind == "fm" else Wtm[bi]
            mx = mixed(kind, bi)
            fw.dma("sp", lambda e, src=src: e.dma_start(out=w1[:, 0:8, :], in_=src[:, 0:8, :]), writes=[B_w1])
            fw.dma("act", lambda e, src=src: e.dma_start(out=w1[:, 8:16, :], in_=src[:, 8:16, :]), writes=[B_w1])
            if mx:
                mus = mufm if kind == "fm" else mutm
                fw.dma("pool", lambda e, mus=mus, bi=bi: e.dma_start(out=mur[:], in_=mus[bi:bi + 1, :].partition_broadcast(128)), writes=[B_mur])
                fw.op("dve", lambda e: e.tensor_tensor(out=w2[:], in0=w1[:], in1=mur[:].unsqueeze(1).to_broadcast([128, 16, 512]), op=ALU.mult), reads=[B_w1, B_mur], writes=[B_w2])
                fw.op("pool", lambda e: e.tensor_tensor(out=w1[:], in0=w1[:], in1=w2[:], op=ALU.subtract), reads=[B_w1, B_w2], writes=[B_w1])
            nsub = 4 if kind == "fm" else TBK // 128
            if kind == "fm" and bi == 2:
                nsub = 3
            for sub in range(nsub):
                p, Bp = pp[it % 8]
                evt, Bev = ev[it % 2]
                it += 1
                groups = [(w1, B_w1, hTb, B_hTb)] + ([(w2, B_w2, hTp, B_hTp)] if mx else [])
                nmm = 16 * len(groups)
                j = 0
                for (w, Bw, hh, Bh) in groups:
                    for kc in range(16):
                        first, last = (j == 0), (j == nmm - 1)
                        if kind == "fm":
                            fw.op("pe", lambda e, kc=kc, p=p, w=w, hh=hh, sub=sub, first=first, last=last: e.matmul(p[:, 0:TBK], lhsT=w[:, kc, sub * 128:(sub + 1) * 128], rhs=hh[:, kc, :], start=first, stop=last),
                                  reads=[Bw, Bh], writes=[Bp] if (first or last) else [])
                        else:
                            fw.op("pe", lambda e, kc=kc, p=p, w=w, hh=hh, sub=sub, first=first, last=last: e.matmul(p[:, :], lhsT=hh[:, kc, sub * 128:(sub + 1) * 128], rhs=w[:, kc, :], start=first, stop=last),
                                  reads=[Bw, Bh], writes=[Bp] if (first or last) else [])
                        j += 1
                if kind == "fm":
                    fw.op("act", lambda e, p=p, evt=evt: e.copy(out=evt[:, 0:TBK], in_=p[:, 0:TBK]), reads=[Bp], writes=[Bev])
                    r0 = bi * 512 + sub * 128
                    fw.dma("pool", lambda e, evt=evt, r0=r0, t0=t0: e.dma_start(out=PT[r0:r0 + 128, t0:t0 + TBK], in_=evt[:, 0:TBK]), reads=[Bev], writes=[B_PT])
                else:
                    fw.op("act", lambda e, p=p, evt=evt: e.copy(out=evt[:], in_=p[:, :]), reads=[Bp], writes=[Bev])
                    tt = t0 + sub * 128
                    fw.dma("pool", lambda e, evt=evt, tt=tt, bi=bi: e.dma_start(out=PM[tt:tt + 128, bi * 512:(bi + 1) * 512], in_=evt[:]), reads=[Bev], writes=[B_PM])
    fw.barrier()
    off[0] = 0
    fo, B_fo = sb([128, 1024], "fo")
    rw_, B_rw = sb([128, 12, 512], "rowsb")
    for j in range(10 if layer2 else 9):
        fw.dma("pool", lambda e, j=j: e.dma_start(out=rw_[:, j, :], in_=rowsd[j:j + 1, :].partition_broadcast(128)), writes=[B_rw])
    st, B_st = sb([128, 6], "st"); mv, B_mv = sb([128, 2], "mv"); rs, B_rs = sb([128, 1], "rs")
    if do_ret:
        pi_, B_pi = sb([128, T], "posint", I32)
        ang, B_ang = sb([128, T], "ang")
        cosT, B_cos = sb([128, T], "cosT"); sinT, B_sin = sb([128, T], "sinT")
        fw.dma("sp", lambda e: e.dma_start(out=pi_[:], in_=posi[0:1, :].partition_broadcast(128)), writes=[B_pi])
        fw.op("dve", lambda e: e.tensor_copy(out=ang[:], in_=pi_[:]), reads=[B_pi], writes=[B_ang])
        fw.op("dve", lambda e: e.tensor_scalar(out=ang[:], in0=ang[:], scalar1=cs2[:, 0:1], scalar2=None, op0=ALU.mult), reads=[B_ang, B_cs2], writes=[B_ang])
        ki, B_ki = sb([128, T], "ki", I32)
        C1, C2 = 6.28125, TWO_PI - 6.28125
        fw.op("dve", lambda e: e.tensor_scalar(out=sinT[:], in0=ang[:], scalar1=1.0 / TWO_PI, scalar2=0.5, op0=ALU.mult, op1=ALU.add), reads=[B_ang], writes=[B_sin])
        fw.op("dve", lambda e: e.tensor_copy(out=ki[:], in_=sinT[:]), reads=[B_sin], writes=[B_ki])
        fw.op("dve", lambda e: e.tensor_copy(out=cosT[:], in_=ki[:]), reads=[B_ki], writes=[B_cos])
        fw.op("dve", lambda e: e.scalar_tensor_tensor(out=sinT[:], in0=cosT[:], scalar=-C1, in1=ang[:], op0=ALU.mult, op1=ALU.add), reads=[B_cos, B_ang], writes=[B_sin])
        fw.op("dve", lambda e: e.scalar_tensor_tensor(out=sinT[:], in0=cosT[:], scalar=-C2, in1=sinT[:], op0=ALU.mult, op1=ALU.add), reads=[B_cos, B_sin], writes=[B_sin])
        def wrap(tt, Bt, tmp, Btmp):
            fw.op("dve", lambda e: e.tensor_single_scalar(out=tmp[:], in_=tt[:], scalar=math.pi, op=ALU.is_gt), reads=[Bt], writes=[Btmp])
            fw.op("dve", lambda e: e.scalar_tensor_tensor(out=tt[:], in0=tmp[:], scalar=-TWO_PI, in1=tt[:], op0=ALU.mult, op1=ALU.add), reads=[Btmp, Bt], writes=[Bt])
            fw.op("dve", lambda e: e.tensor_single_scalar(out=tmp[:], in_=tt[:], scalar=-math.pi, op=ALU.is_lt), reads=[Bt], writes=[Btmp])
            fw.op("dve", lambda e: e.scalar_tensor_tensor(out=tt[:], in0=tmp[:], scalar=TWO_PI, in1=tt[:], op0=ALU.mult, op1=ALU.add), reads=[Btmp, Bt], writes=[Bt])
        wrap(sinT, B_sin, ang, B_ang)
        fw.op("dve", lambda e: e.tensor_scalar_add(out=cosT[:], in0=sinT[:], scalar1=0.5 * math.pi), reads=[B_sin], writes=[B_cos])
        wrap(cosT, B_cos, ang, B_ang)
        fw.op("act", lambda e: e.activation(out=sinT[:], in_=sinT[:], func=AF.Sin), reads=[B_sin], writes=[B_sin])
        fw.op("act", lambda e: e.activation(out=cosT[:], in_=cosT[:], func=AF.Sin), reads=[B_cos], writes=[B_cos])
        S = [[sb([128, 256], f"S{h}{c}") for c in range(2)] for h in range(2)]
        for h in range(2):
            for c in range(2):
                fw.op("pool", lambda e, t=S[h][c][0]: e.memset(t[:], 0.0), writes=[S[h][c][1]])
        qT, B_qT = sb([128, 4, 128], "qT"); kT, B_kT = sb([128, 4, 128], "kT")
        qR, B_qR = sb([128, 4, 128], "qR"); kR, B_kR = sb([128, 4, 128], "kR")
        tA, B_tA = sb([128, 128], "tA"); tB, B_tB = sb([128, 128], "tB")
        km, B_km = sb([128, 512], "km"); vm, B_vm = sb([128, 512], "vm"); gm, B_gm = sb([128, 512], "gm")
        aT, B_aT = sb([128, 128], "aT"); o, B_o = sb([128, 256], "o")
        pS, B_pS = pp[0]; pO, B_pO = pp[1]; pU, B_pU = pp[2]; pK, B_pK = pp[3]
        for n in range(T // 128):
            ts_ = slice(n * 128, (n + 1) * 128)
            fw.dma("sp", lambda e, ts_=ts_: e.dma_start(out=qT[:], in_=PT[0:512, ts_].rearrange("(c p) t -> p c t", p=128)), reads=[B_PT], writes=[B_qT])
            fw.dma("sp", lambda e, ts_=ts_: e.dma_start(out=kT[:], in_=PT[512:1024, ts_].rearrange("(c p) t -> p c t", p=128)), reads=[B_PT], writes=[B_kT])
            fw.dma("act", lambda e, ts_=ts_: e.dma_start(out=vm[:], in_=PM[ts_, 0:512]), reads=[B_PM], writes=[B_vm])
            fw.dma("act", lambda e, ts_=ts_: e.dma_start(out=gm[:], in_=PM[ts_, 512:1024]), reads=[B_PM], writes=[B_gm])
            fw.op("act", lambda e: e.activation(out=gm[:], in_=gm[:], func=AF.Silu), reads=[B_gm], writes=[B_gm])
            for h in range(2):
                for (src, Bsrc, dst, Bdst, tab) in ((qT, B_qT, qR, B_qR, GP0 if h == 0 else GP1), (kT, B_kT, kR, B_kR, GN0 if h == 0 else GN1)):
                    c1, c2 = 2 * h, 2 * h + 1
                    eng1, eng2 = ("dve", "pool") if src is qT else ("pool", "dve")
                    fw.op(eng1, lambda e, src=src, c1=c1, ts_=ts_: e.tensor_tensor(out=tA[:], in0=src[:, c1, :], in1=cosT[:, ts_], op=ALU.mult), reads=[Bsrc, B_cos], writes=[B_tA])
                    fw.op(eng1, lambda e, src=src, c2=c2, ts_=ts_: e.tensor_tensor(out=tB[:], in0=src[:, c2, :], in1=sinT[:, ts_], op=ALU.mult), reads=[Bsrc, B_sin], writes=[B_tB])
                    fw.op(eng1, lambda e: e.tensor_tensor(out=tA[:], in0=tA[:], in1=tB[:], op=ALU.subtract), reads=[B_tA, B_tB], writes=[B_tA])
                    fw.op(eng1, lambda e, dst=dst, c1=c1, tab=tab: e.tensor_tensor(out=dst[:, c1, :], in0=tA[:], in1=cs[:, tab, :], op=ALU.mult), reads=[B_tA, B_cs], writes=[Bdst])
                    fw.op(eng1, lambda e, src=src, c1=c1, ts_=ts_: e.tensor_tensor(out=tA[:], in0=src[:, c1, :], in1=sinT[:, ts_], op=ALU.mult), reads=[Bsrc, B_sin], writes=[B_tA])
                    fw.op(eng1, lambda e, src=src, c2=c2, ts_=ts_: e.tensor_tensor(out=tB[:], in0=src[:, c2, :], in1=cosT[:, ts_], op=ALU.mult), reads=[Bsrc, B_cos], writes=[B_tB])
                    fw.op(eng1, lambda e: e.tensor_tensor(out=tA[:], in0=tA[:], in1=tB[:], op=ALU.add), reads=[B_tA, B_tB], writes=[B_tA])
                    fw.op(eng1, lambda e, dst=dst, c2=c2, tab=tab: e.tensor_tensor(out=dst[:, c2, :], in0=tA[:], in1=cs[:, tab, :], op=ALU.mult), reads=[B_tA, B_cs], writes=[Bdst])
            for cc in range(4):
                fw.op("pe", lambda e, cc=cc: e.transpose(out=pK[:, cc * 128:(cc + 1) * 128], in_=kR[:, cc, :], identity=cs[:, IDN, :]), reads=[B_kR, B_cs], writes=[B_pK])
            fw.op("act", lambda e: e.copy(out=km[:], in_=pK[:, :]), reads=[B_pK], writes=[B_km])
            for h in range(2):
                for c in range(2):
                    cc = 2 * h + c
                    fw.op("pe", lambda e, cc=cc, c=c: e.matmul(pS[:, 0:128], lhsT=kR[:, cc, :], rhs=qR[:, cc, :], start=(c == 0), stop=(c == 1)), reads=[B_kR, B_qR], writes=[B_pS])
                fw.op("dve", lambda e: e.tensor_tensor(out=aT[:], in0=pS[:, 0:128], in1=cs[:, TRI, :], op=ALU.mult), reads=[B_pS, B_cs], writes=[B_aT])
                vs = slice(h * 256, (h + 1) * 256)
                fw.op("pe", lambda e, vs=vs: e.matmul(pO[:, 0:256], lhsT=aT[:, :], rhs=vm[:, vs], start=True, stop=False), reads=[B_aT, B_vm], writes=[B_pO])
                for c in range(2):
                    cc = 2 * h + c
                    fw.op("pe", lambda e, cc=cc, c=c, h=h: e.matmul(pO[:, 0:256], lhsT=qR[:, cc, :], rhs=S[h][c][0][:, :], start=False, stop=(c == 1)), reads=[B_qR, S[h][c][1]], writes=[B_pO])
                fw.op("act", lambda e: e.copy(out=o[:], in_=pO[:, 0:256]), reads=[B_pO], writes=[B_o])
                for c in range(2):
                    cc = 2 * h + c
                    St, BS = S[h][c]
                    fw.op("pe", lambda e, cc=cc, vs=vs: e.matmul(pU[:, 0:256], lhsT=km[:, cc * 128:(cc + 1) * 128], rhs=vm[:, vs], start=True, stop=True), reads=[B_km, B_vm], writes=[B_pU])
                    fw.op("dve", lambda e, St=St: e.tensor_tensor(out=St[:], in0=pU[:, 0:256], in1=St[:], op=ALU.add), reads=[B_pU, BS], writes=[BS])
                    fw.op("dve", lambda e, St=St, h=h: e.tensor_scalar(out=St[:], in0=St[:], scalar1=cs2[:, 1 + h:2 + h], scalar2=None, op0=ALU.mult), reads=[BS, B_cs2], writes=[BS])
                fw.op("dve", lambda e: e.bn_stats(out=st[:], in_=o[:]), reads=[B_o], writes=[B_st])
                fw.op("dve", lambda e: e.bn_aggr(out=mv[:], in_=st[:]), reads=[B_st], writes=[B_mv])
                fw.op("dve", lambda e: e.tensor_scalar_add(out=rs[:], in0=mv[:, 1:2], scalar1=1e-5), reads=[B_mv], writes=[B_rs])
                fw.op("act", lambda e: e.activation(out=rs[:], in_=rs[:], func=AF.Sqrt), reads=[B_rs], writes=[B_rs])
                fw.op("dve", lambda e: e.reciprocal(out=rs[:], in_=rs[:]), reads=[B_rs], writes=[B_rs])
                fw.op("dve", lambda e: e.tensor_scalar(out=o[:], in0=o[:], scalar1=mv[:, 0:1], scalar2=rs[:, 0:1], op0=ALU.subtract, op1=ALU.mult), reads=[B_o, B_mv, B_rs], writes=[B_o])
                fw.op("pool", lambda e, vs=vs: e.tensor_tensor(out=o[:], in0=o[:], in1=rw_[:, 0, vs], op=ALU.mult), reads=[B_o, B_rw], writes=[B_o])
                fw.op("pool", lambda e, vs=vs: e.tensor_tensor(out=o[:], in0=o[:], in1=rw_[:, 1, vs], op=ALU.add), reads=[B_o, B_rw], writes=[B_o])
                fw.op("dve", lambda e, vs=vs: e.tensor_tensor(out=fo[:, vs], in0=o[:], in1=gm[:, vs], op=ALU.mult), reads=[B_o, B_gm], writes=[B_fo])
            fw.dma("sp", lambda e, ts_=ts_: e.dma_start(out=feat[ts_, 0:512], in_=fo[:, 0:512]), reads=[B_fo], writes=[B_feat])
    if do_rw:
        C = RW_C
        SLO, SUP = 6, 7
        lw2t, B_lw2 = sb([64, 512], "lw2t"); la2t, B_la2 = sb([64, 512], "la2t")
        lg2a, B_lg2a = sb([128, 512], "lg2a"); lg2b, B_lg2b = sb([32, 512], "lg2b")
        fw.dma("pool", lambda e: e.dma_start(out=lw2t[:], in_=lw2[:, :]), writes=[B_lw2])
        fw.dma("pool", lambda e: e.dma_start(out=la2t[:], in_=la2[:, :]), writes=[B_la2])
        fw.dma("pool", lambda e: e.dma_start(out=lg2a[:], in_=lg2[0:128, :]), writes=[B_lg2a])
        fw.dma("pool", lambda e: e.dma_start(out=lg2b[:], in_=lg2[128:160, :]), writes=[B_lg2b])
        if layer2:
            lv1t, B_lv1 = sb([128, 8, 32], "lv1t"); lv2t, B_lv2 = sb([32, 512], "lv2t")
            fw.dma("pool", lambda e: e.dma_start(out=lv1t[:], in_=lv1[:, :, :]), writes=[B_lv1])
            fw.dma("pool", lambda e: e.dma_start(out=lv2t[:], in_=lv2[:, :]), writes=[B_lv2])
        H = [sb([64, 64], f"H{hh}") for hh in range(8)]
        for hh in range(8):
            fw.op("pool", lambda e, t=H[hh][0]: e.memset(t[:], 0.0), writes=[H[hh][1]])
        def t64(name, w=512):
            return sb([64, w], name)
        rt, B_rt = t64("rt"); kt, B_kt = t64("kt"); vt, B_vt = t64("vt")
        wdT, B_wdT = sb([64, 64], "wdT"); adT, B_adT = sb([64, 64], "adT"); gda, B_gda = sb([128, 64], "gda"); gdb, B_gdb = sb([32, 64], "gdb")
        lw, B_lw = t64("lw"); asg, B_asg = t64("asg"); gate, B_gate = t64("gate")
        kkx, B_kkx = t64("kkx"); sq, B_sq = t64("sq"); ss, B_ss = sb([64, 8], "ss")
        kmod, B_kmod = t64("kmod"); bv, B_bv = t64("bv"); tmp, B_tmp = t64("tmp")
        eLp, B_eLp = t64("eLp"); eLn, B_eLn = t64("eLn"); eLx, B_eLx = t64("eLx")
        Rt, B_Rt = t64("Rt"); Kt, B_Kt = t64("Kt"); Bt, B_Bt = t64("Bt"); At, B_At = t64("At")
        RT, B_RT = sb([64, 8, 64], "RTT"); KT, B_KT = sb([64, 8, 64], "KTT"); BT, B_BT = sb([64, 8, 64], "BTT"); AT, B_AT = sb([64, 8, 64], "ATT")
        dec, B_dec = sb([64, 512], "dec")
        ones64, B_ones = sb([64, 64], "ones64")
        fw.op("pool", lambda e: e.memset(ones64[:], 1.0), writes=[B_ones])
        bon, B_bon = t64("bon"); rks, B_rks = sb([64, 8], "rks")
        yt, B_yt = t64("yt")
        if layer2:
            vTa, B_vTa = sb([128, 8, 64], "vTa"); vf, B_vf = t64("vf"); sT, B_sT = sb([32, 64], "sT"); sg, B_sg = t64("sg")
        def ph(name):
            return [sb([64, 64], f"{name}{hh}") for hh in range(8)]
        Na, NTa, Nb, NTb, Pm, Pak, Prb, Prk, R0, U = [ph(n) for n in ("Na", "NTa", "Nb", "NTb", "Pm", "Pak", "Prb", "Prk", "R0", "U")]
        def regs(i):
            b_ = Buf(f"pp{i}bank")
            return [b_] * 8
        PB = [regs(i) for i in range(8)]
        def pall(i):
            return PB[i]
        hsl = lambda hh: slice(hh * 64, (hh + 1) * 64)
        for j in range(T // C):
            t0 = j * C
            tsl = slice(t0, t0 + C)
            fw.dma("sp", lambda e, tsl=tsl: e.dma_start(out=rt[:], in_=PM[tsl, 1024:1536]), reads=[B_PM], writes=[B_rt])
            fw.dma("act", lambda e, tsl=tsl: e.dma_start(out=kt[:], in_=PM[tsl, 1536:2048]), reads=[B_PM], writes=[B_kt])
            fw.dma("sp", lambda e, tsl=tsl: e.dma_start(out=vt[:], in_=PM[tsl, 2048:2560]), reads=[B_PM], writes=[B_vt])
            fw.dma("act", lambda e, tsl=tsl: e.dma_start(out=wdT[:], in_=PT[1024:1088, tsl]), reads=[B_PT], writes=[B_wdT])
            fw.dma("act", lambda e, tsl=tsl: e.dma_start(out=adT[:], in_=PT[1088:1152, tsl]), reads=[B_PT], writes=[B_adT])
            fw.dma("sp", lambda e, tsl=tsl: e.dma_start(out=gda[:], in_=PT[1152:1280, tsl]), reads=[B_PT], writes=[B_gda])
            fw.dma("sp", lambda e, tsl=tsl: e.dma_start(out=gdb[:], in_=PT[1280:1312, tsl]), reads=[B_PT], writes=[B_gdb])
            if not layer2:
                fw.dma("pool", lambda e, tsl=tsl: e.dma_start(out=vout[tsl, :], in_=vt[:]), reads=[B_vt], writes=[B_vout])
            p0, p1, p2, p3, p4, p5, p6, p7 = [pp[i][0] for i in range(8)]
            fw.op("act", lambda e: e.activation(out=wdT[:], in_=wdT[:], func=AF.Tanh), reads=[B_wdT], writes=[B_wdT])
            fw.op("pe", lambda e: e.matmul(p0[0:64, :], lhsT=wdT[:, :], rhs=lw2t[:, :], start=True, stop=True), reads=[B_wdT, B_lw2], writes=pall(0))
            fw.op("dve", lambda e: e.tensor_tensor(out=lw[:], in0=p0[0:64, :], in1=rw_[0:64, 2, :], op=ALU.add), reads=pall(0) + [B_rw], writes=[B_lw])
            fw.op("act", lambda e: e.activation(out=lw[:], in_=lw[:], func=AF.Sigmoid), reads=[B_lw], writes=[B_lw])
            fw.op("dve", lambda e: e.tensor_scalar_mul(out=lw[:], in0=lw[:], scalar1=W_SCALE), reads=[B_lw], writes=[B_lw])
            fw.op("pe", lambda e: e.matmul(p1[0:64, :], lhsT=adT[:, :], rhs=la2t[:, :], start=True, stop=True), reads=[B_adT, B_la2], writes=pall(1))
            fw.op("dve", lambda e: e.tensor_tensor(out=asg[:], in0=p1[0:64, :], in1=rw_[0:64, 3, :], op=ALU.add), reads=pall(1) + [B_rw], writes=[B_asg])
            fw.op("act", lambda e: e.activation(out=asg[:], in_=asg[:], func=AF.Sigmoid), reads=[B_asg], writes=[B_asg])
            fw.op("act", lambda e: e.activation(out=gda[:], in_=gda[:], func=AF.Sigmoid), reads=[B_gda], writes=[B_gda])
            fw.op("act", lambda e: e.activation(out=gdb[:], in_=gdb[:], func=AF.Sigmoid), reads=[B_gdb], writes=[B_gdb])
            fw.op("pe", lambda e: e.matmul(p2[0:64, :], lhsT=gda[:, :], rhs=lg2a[:, :], start=True, stop=False), reads=[B_gda, B_lg2a], writes=pall(2))
            fw.op("pe", lambda e: e.matmul(p2[0:64, :], lhsT=gdb[:, :], rhs=lg2b[:, :], start=False, stop=True), reads=[B_gdb, B_lg2b], writes=pall(2))
            fw.op("act", lambda e: e.copy(out=gate[:], in_=p2[0:64, :]), reads=pall(2), writes=[B_gate])
            if layer2:
                fw.dma("sp", lambda e, tsl=tsl: e.dma_start(out=vTa[:], in_=PT[1536:2560, tsl].rearrange("(c p) t -> p c t", p=128)), reads=[B_PT], writes=[B_vTa])
                fw.dma("act", lambda e, tsl=tsl: e.dma_start(out=vf[:], in_=vfirst[tsl, :]), writes=[B_vf])
                for cc in range(8):
                    fw.op("pe", lambda e, cc=cc: e.matmul(p3[0:32, 0:64], lhsT=lv1t[:, cc, :], rhs=vTa[:, cc, :], start=(cc == 0), stop=(cc == 7)), reads=[B_lv1, B_vTa], writes=pall(3) if cc in (0, 7) else [])
                fw.op("act", lambda e: e.copy(out=sT[:], in_=p3[0:32, 0:64]), reads=pall(3), writes=[B_sT])
                fw.op("pe", lambda e: e.matmul(p3[0:64, :], lhsT=sT[:, :], rhs=lv2t[:, :], start=True, stop=True), reads=[B_sT, B_lv2], writes=pall(3))
                fw.op("dve", lambda e: e.tensor_tensor(out=sg[:], in0=p3[0:64, :], in1=rw_[0:64, 9, :], op=ALU.add), reads=pall(3) + [B_rw], writes=[B_sg])
                fw.op("act", lambda e: e.activation(out=sg[:], in_=sg[:], func=AF.Sigmoid), reads=[B_sg], writes=[B_sg])
                fw.op("dve", lambda e: e.tensor_tensor(out=vf[:], in0=vf[:], in1=vt[:], op=ALU.subtract), reads=[B_vf, B_vt], writes=[B_vf])
                fw.op("dve", lambda e: e.tensor_tensor(out=vf[:], in0=vf[:], in1=sg[:], op=ALU.mult), reads=[B_vf, B_sg], writes=[B_vf])
                fw.op("dve", lambda e: e.tensor_tensor(out=vt[:], in0=vt[:], in1=vf[:], op=ALU.add), reads=[B_vf, B_vt], writes=[B_vt])
            fw.op("pool", lambda e: e.tensor_tensor(out=kkx[:], in0=kt[:], in1=rw_[0:64, 4, :], op=ALU.mult), reads=[B_kt, B_rw], writes=[B_kkx])
            fw.op("pool", lambda e: e.tensor_tensor(out=sq[:], in0=kkx[:], in1=kkx[:], op=ALU.mult), reads=[B_kkx], writes=[B_sq])
            fw.op("dve", lambda e: e.tensor_reduce(out=ss[:], in_=sq[:].rearrange("p (h d) -> p h d", h=8), axis=AX.X, op=ALU.add), reads=[B_sq], writes=[B_ss])
            fw.op("act", lambda e: e.activation(out=ss[:], in_=ss[:], func=AF.Sqrt), reads=[B_ss], writes=[B_ss])
            fw.op("dve", lambda e: e.tensor_scalar_max(out=ss[:], in0=ss[:], scalar1=1e-12), reads=[B_ss], writes=[B_ss])
            fw.op("dve", lambda e: e.reciprocal(out=ss[:], in_=ss[:]), reads=[B_ss], writes=[B_ss])
            fw.op("dve", lambda e: e.tensor_tensor(out=kkx[:].rearrange("p (h d) -> p h d", h=8), in0=kkx[:].rearrange("p (h d) -> p h d", h=8),
                                                   in1=ss[:].unsqueeze(2).to_broadcast([64, 8, 64]), op=ALU.mult), reads=[B_kkx, B_ss], writes=[B_kkx])
            fw.op("pool", lambda e: e.tensor_tensor(out=tmp[:], in0=asg[:], in1=rw_[0:64, 5, :], op=ALU.mult), reads=[B_asg, B_rw], writes=[B_tmp])
            fw.op("pool", lambda e: e.tensor_tensor(out=tmp[:], in0=tmp[:], in1=rw_[0:64, 5, :], op=ALU.subtract), reads=[B_tmp, B_rw], writes=[B_tmp])
            fw.op("pool", lambda e: e.tensor_scalar_add(out=tmp[:], in0=tmp[:], scalar1=1.0), reads=[B_tmp], writes=[B_tmp])
            fw.op("pool", lambda e: e.tensor_tensor(out=kmod[:], in0=kt[:], in1=tmp[:], op=ALU.mult), reads=[B_kt, B_tmp], writes=[B_kmod])
            fw.op("dve", lambda e: e.tensor_tensor(out=bv[:], in0=kkx[:], in1=asg[:], op=ALU.mult), reads=[B_kkx, B_asg], writes=[B_bv])
            fw.op("pool", lambda e: e.tensor_tensor(out=tmp[:], in0=rt[:], in1=kmod[:], op=ALU.mult), reads=[B_rt, B_kmod], writes=[B_tmp])
            fw.op("pool", lambda e: e.tensor_tensor(out=tmp[:], in0=tmp[:], in1=rw_[0:64, 6, :], op=ALU.mult), reads=[B_tmp, B_rw], writes=[B_tmp])
            fw.op("dve", lambda e: e.tensor_reduce(out=rks[:], in_=tmp[:].rearrange("p (h d) -> p h d", h=8), axis=AX.X, op=ALU.add), reads=[B_tmp], writes=[B_rks])
            fw.op("dve", lambda e: e.tensor_tensor(out=bon[:].rearrange("p (h d) -> p h d", h=8), in0=vt[:].rearrange("p (h d) -> p h d", h=8),
                                                   in1=rks[:].unsqueeze(2).to_broadcast([64, 8, 64]), op=ALU.mult), reads=[B_vt, B_rks], writes=[B_bon])
            fw.op("pe", lambda e: e.matmul(p4[0:64, :], lhsT=cs[0:64, TRI, 0:64], rhs=lw[:, :], start=True, stop=True), reads=[B_cs, B_lw], writes=pall(4))
            fw.op("act", lambda e: e.activation(out=eLp[:], in_=p4[0:64, :], func=AF.Exp), reads=pall(4), writes=[B_eLp])
            fw.op("act", lambda e: e.activation(out=eLn[:], in_=p4[0:64, :], func=AF.Exp, scale=-1.0), reads=pall(4), writes=[B_eLn])
            fw.op("dve", lambda e: e.tensor_tensor(out=eLx[:], in0=p4[0:64, :], in1=lw[:], op=ALU.subtract), reads=pall(4) + [B_lw], writes=[B_eLx])
            fw.op("act", lambda e: e.activation(out=eLx[:], in_=eLx[:], func=AF.Exp), reads=[B_eLx], writes=[B_eLx])
            fw.op("dve", lambda e: e.tensor_tensor(out=Rt[:], in0=rt[:], in1=eLp[:], op=ALU.mult), reads=[B_rt, B_eLp], writes=[B_Rt])
            fw.op("pool", lambda e: e.tensor_tensor(out=Kt[:], in0=kmod[:], in1=eLn[:], op=ALU.mult), reads=[B_kmod, B_eLn], writes=[B_Kt])
            fw.op("dve", lambda e: e.tensor_tensor(out=Bt[:], in0=bv[:], in1=eLn[:], op=ALU.mult), reads=[B_bv, B_eLn], writes=[B_Bt])
            fw.op("dve", lambda e: e.scalar_tensor_tensor(out=At[:], in0=kkx[:], scalar=-1.0, in1=eLx[:], op0=ALU.mult, op1=ALU.mult), reads=[B_kkx, B_eLx], writes=[B_At])
            for hh in range(8):
                fw.op("pe", lambda e, hh=hh: e.matmul(p5[0:64, hsl(hh)], lhsT=lw[:, hsl(hh)], rhs=ones64[:, :], start=True, stop=True), reads=[B_lw, B_ones], writes=[PB[5][hh]])
            fw.op("act", lambda e: e.activation(out=dec[:], in_=p5[0:64, :], func=AF.Exp), reads=pall(5), writes=[B_dec])
            for (src, Bsrc, dst, Bdst, bank) in ((Rt, B_Rt, RT, B_RT, 0), (Kt, B_Kt, KT, B_KT, 1), (Bt, B_Bt, BT, B_BT, 2), (At, B_At, AT, B_AT, 3)):
                pb_ = pp[bank][0]
                for hh in range(8):
                    fw.op("pe", lambda e, hh=hh, src=src, pb_=pb_: e.transpose(out=pb_[0:64, hsl(hh)], in_=src[:, hsl(hh)], identity=cs[0:64, IDN, 0:64]), reads=[Bsrc, B_cs], writes=[PB[bank][hh]])
                fw.op("act" if bank % 2 == 0 else "dve", (lambda e, dst=dst, pb_=pb_: e.copy(out=dst[:].rearrange("p h t -> p (h t)"), in_=pb_[0:64, :])) if bank % 2 == 0 else
                      (lambda e, dst=dst, pb_=pb_: e.tensor_copy(out=dst[:].rearrange("p h t -> p (h t)"), in_=pb_[0:64, :])), reads=pall(bank), writes=[Bdst])
            def score(bank, lhs, Blhs, rhs, Brhs, dsts, mask, eng):
                pb_ = pp[bank][0]
                for hh in range(8):
                    fw.op("pe", lambda e, hh=hh, pb_=pb_: e.matmul(pb_[0:64, hsl(hh)], lhsT=lhs[:, hh, :], rhs=rhs[:, hh, :], start=True, stop=True), reads=[Blhs, Brhs], writes=[PB[bank][hh]])
                for hh in range(8):
                    d, Bd = dsts[hh]
                    fw.op(eng, lambda e, hh=hh, d=d, pb_=pb_: e.tensor_tensor(out=d[:], in0=pb_[0:64, hsl(hh)], in1=cs[0:64, mask, 0:64], op=ALU.mult), reads=[PB[bank][hh], B_cs], writes=[Bd])
            score(4, BT, B_BT, AT, B_AT, Na, SUP, "dve")
            score(5, AT, B_AT, BT, B_BT, NTa, SLO, "dve")
            score(6, KT, B_KT, AT, B_AT, Pak, SUP, "dve")
            score(7, BT, B_BT, RT, B_RT, Prb, TRI, "dve")
            score(0, KT, B_KT, RT, B_RT, Prk, TRI, "dve")
            for hh in range(8):
                fw.op("pool", lambda e, hh=hh: e.tensor_tensor(out=Pm[hh][0][:], in0=Na[hh][0][:], in1=cs[0:64, IDN, 0:64], op=ALU.add), reads=[Na[hh][1], B_cs], writes=[Pm[hh][1]])
            cur, curT, nxt, nxtT = Na, NTa, Nb, NTb
            for lvl in range(1, 6):
                last = (lvl == 5)
                if not last:
                    for hh in range(8):
                        fw.op("pe", lambda e, hh=hh, cur=cur, curT=curT: e.matmul(p1[0:64, hsl(hh)], lhsT=curT[hh][0][:, :], rhs=cur[hh][0][:, :], start=True, stop=True), reads=[curT[hh][1], cur[hh][1]], writes=[PB[1][hh]])
                for hh in range(8):
                    fw.op("pe", lambda e, hh=hh, cur=cur, curT=curT: e.matmul(p2[0:64, hsl(hh)], lhsT=cur[hh][0][:, :], rhs=curT[hh][0][:, :], start=True, stop=True), reads=[curT[hh][1], cur[hh][1]], writes=[PB[2][hh]])
                for hh in range(8):
                    if not last:
                        fw.op("act", lambda e, hh=hh, nxt=nxt: e.copy(out=nxt[hh][0][:], in_=p1[0:64, hsl(hh)]), reads=[PB[1][hh]], writes=[nxt[hh][1]])
                    fw.op("dve", lambda e, hh=hh, nxtT=nxtT: e.tensor_copy(out=nxtT[hh][0][:], in_=p2[0:64, hsl(hh)]), reads=[PB[2][hh]], writes=[nxtT[hh][1]])
                for hh in range(8):
                    fw.op("pe", lambda e, hh=hh, nxtT=nxtT: e.matmul(p3[0:64, hsl(hh)], lhsT=nxtT[hh][0][:, :], rhs=Pm[hh][0][:, :], start=True, stop=True), reads=[nxtT[hh][1], Pm[hh][1]], writes=[PB[3][hh]])
                for hh in range(8):
                    fw.op("dve", lambda e, hh=hh: e.tensor_tensor(out=Pm[hh][0][:], in0=p3[0:64, hsl(hh)], in1=Pm[hh][0][:], op=ALU.add), reads=[PB[3][hh], Pm[hh][1]], writes=[Pm[hh][1]])
                cur, curT, nxt, nxtT = nxt, nxtT, cur, curT
            for hh in range(8):
                fw.op("pe", lambda e, hh=hh: e.matmul(p4[0:64, hsl(hh)], lhsT=AT[:, hh, :], rhs=H[hh][0][:, :], start=True, stop=False), reads=[B_AT, H[hh][1]], writes=[PB[4][hh]])
                fw.op("pe", lambda e, hh=hh: e.matmul(p4[0:64, hsl(hh)], lhsT=Pak[hh][0][:, :], rhs=vt[:, hsl(hh)], start=False, stop=True), reads=[Pak[hh][1], B_vt], writes=[PB[4][hh]])
            for hh in range(8):
                fw.op("act", lambda e, hh=hh: e.copy(out=R0[hh][0][:], in_=p4[0:64, hsl(hh)]), reads=[PB[4][hh]], writes=[R0[hh][1]])
            for hh in range(8):
                fw.op("pe", lambda e, hh=hh: e.matmul(p5[0:64, hsl(hh)], lhsT=Pm[hh][0][:, :], rhs=R0[hh][0][:, :], start=True, stop=True), reads=[Pm[hh][1], R0[hh][1]], writes=[PB[5][hh]])
            for hh in range(8):
                fw.op("act", lambda e, hh=hh: e.copy(out=U[hh][0][:], in_=p5[0:64, hsl(hh)]), reads=[PB[5][hh]], writes=[U[hh][1]])
            for hh in range(8):
                fw.op("pe", lambda e, hh=hh: e.matmul(p6[0:64, hsl(hh)], lhsT=RT[:, hh, :], rhs=H[hh][0][:, :], start=True, stop=False), reads=[B_RT, H[hh][1]], writes=[PB[6][hh]])
                fw.op("pe", lambda e, hh=hh: e.matmul(p6[0:64, hsl(hh)], lhsT=Prk[hh][0][:, :], rhs=vt[:, hsl(hh)], start=False, stop=False), reads=[Prk[hh][1], B_vt], writes=[PB[6][hh]])
                fw.op("pe", lambda e, hh=hh: e.matmul(p6[0:64, hsl(hh)], lhsT=Prb[hh][0][:, :], rhs=U[hh][0][:, :], start=False, stop=True), reads=[Prb[hh][1], U[hh][1]], writes=[PB[6][hh]])
            fw.op("act", lambda e: e.copy(out=yt[:], in_=p6[0:64, :]), reads=pall(6), writes=[B_yt])
            for hh in range(8):
                fw.op("pe", lambda e, hh=hh: e.matmul(p7[0:64, hsl(hh)], lhsT=Kt[:, hsl(hh)], rhs=vt[:, hsl(hh)], start=True, stop=False), reads=[B_Kt, B_vt], writes=[PB[7][hh]])
                fw.op("pe", lambda e, hh=hh: e.matmul(p7[0:64, hsl(hh)], lhsT=Bt[:, hsl(hh)], rhs=U[hh][0][:, :], start=False, stop=True), reads=[B_Bt, U[hh][1]], writes=[PB[7][hh]])
            for hh in range(8):
                fw.op("dve", lambda e, hh=hh: e.tensor_tensor(out=H[hh][0][:], in0=p7[0:64, hsl(hh)], in1=H[hh][0][:], op=ALU.add), reads=[PB[7][hh], H[hh][1]], writes=[H[hh][1]])
                fw.op("pool", lambda e, hh=hh: e.tensor_scalar(out=H[hh][0][:], in0=H[hh][0][:], scalar1=dec[:, hh * 64:hh * 64 + 1], scalar2=None, op0=ALU.mult), reads=[H[hh][1], B_dec], writes=[H[hh][1]])
            y3 = yt[:].rearrange("p (h d) -> p h d", h=8)
            fw.op("dve", lambda e: e.tensor_reduce(out=ss[:], in_=yt[:].rearrange("p (h d) -> p h d", h=8), axis=AX.X, op=ALU.add), reads=[B_yt], writes=[B_ss])
            fw.op("dve", lambda e: e.tensor_scalar_mul(out=ss[:], in0=ss[:], scalar1=1.0 / 64.0), reads=[B_ss], writes=[B_ss])
            fw.op("dve", lambda e: e.tensor_tensor(out=yt[:].rearrange("p (h d) -> p h d", h=8), in0=yt[:].rearrange("p (h d) -> p h d", h=8),
                                                   in1=ss[:].unsqueeze(2).to_broadcast([64, 8, 64]), op=ALU.subtract), reads=[B_yt, B_ss], writes=[B_yt])
            fw.op("pool", lambda e: e.tensor_tensor(out=sq[:], in0=yt[:], in1=yt[:], op=ALU.mult), reads=[B_yt], writes=[B_sq])
            fw.op("dve", lambda e: e.tensor_reduce(out=rks[:], in_=sq[:].rearrange("p (h d) -> p h d", h=8), axis=AX.X, op=ALU.add), reads=[B_sq], writes=[B_rks])
            fw.op("dve", lambda e: e.tensor_scalar(out=rks[:], in0=rks[:], scalar1=1.0 / 64.0, scalar2=64e-5, op0=ALU.mult, op1=ALU.add), reads=[B_rks], writes=[B_rks])
            fw.op("act", lambda e: e.activation(out=rks[:], in_=rks[:], func=AF.Sqrt), reads=[B_rks], writes=[B_rks])
            fw.op("dve", lambda e: e.reciprocal(out=rks[:], in_=rks[:]), reads=[B_rks], writes=[B_rks])
            fw.op("dve", lambda e: e.tensor_tensor(out=yt[:].rearrange("p (h d) -> p h d", h=8), in0=yt[:].rearrange("p (h d) -> p h d", h=8),
                                                   in1=rks[:].unsqueeze(2).to_broadcast([64, 8, 64]), op=ALU.mult), reads=[B_yt, B_rks], writes=[B_yt])
            fw.op("pool", lambda e: e.tensor_tensor(out=yt[:], in0=yt[:], in1=rw_[0:64, 7, :], op=ALU.mult), reads=[B_yt, B_rw], writes=[B_yt])
            fw.op("pool", lambda e: e.tensor_tensor(out=yt[:], in0=yt[:], in1=rw_[0:64, 8, :], op=ALU.add), reads=[B_yt, B_rw], writes=[B_yt])
            fw.op("dve", lambda e: e.tensor_tensor(out=yt[:], in0=yt[:], in1=bon[:], op=ALU.add), reads=[B_yt, B_bon], writes=[B_yt])
            fw.op("dve", lambda e: e.tensor_tensor(out=yt[:], in0=yt[:], in1=gate[:], op=ALU.mult), reads=[B_yt, B_gate], writes=[B_yt])
            fw.dma("sp", lambda e, tsl=tsl: e.dma_start(out=feat[tsl, 512:1024], in_=yt[:]), reads=[B_yt], writes=[B_feat])
    return fw.finish(finals)


def even_consts(g):
    f = np.float32
    c = np.zeros((128, 8, 128), f)
    c[:, 0] = np.triu(np.ones((128, 128), f))
    c[:, 1] = np.eye(128, dtype=f)
    t = np.arange(128, dtype=np.float64)
    for hh in range(2):
        gam = 1.0 - 2.0 ** (-5.0 - (2 * g + hh))
        c[:, 2 + 2 * hh] = (gam ** (t + 1.0))[None, :]
        c[:, 3 + 2 * hh] = (gam ** (-(t + 1.0)) / 16.0)[None, :]
    s = np.arange(128)
    c[:, 6] = (s[None, :] < s[:, None]).astype(f)
    c[:, 7] = (s[:, None] < s[None, :]).astype(f)
    c2 = np.zeros((128, 8), f)
    c2[:, 0] = (10000.0 ** (-np.arange(128, dtype=np.float32) / np.float32(128))).astype(f)
    for hh in range(2):
        gam = 1.0 - 2.0 ** (-5.0 - (2 * g + hh))
        c2[:, 1 + hh] = gam ** 128.0
    c2[:64, 3] = 1.0
    c2[64:, 4] = 1.0
    return c, c2


def even_inputs(hT_b, g, pos_b, win, layer2, p, vfirst=None):
    f = np.float32
    win = np.asarray(win, f)
    rq, rk, rv, rg = [win[:, i * 1024:(i + 1) * 1024][:, g * 512:(g + 1) * 512] for i in range(4)]
    wb = win[:, 4096:]
    mu = np.asarray(p["mu"], f)
    wr, wk, wv = [wb[:, i * 1024:(i + 1) * 1024][:, g * 512:(g + 1) * 512] for i in range(3)]
    mr, mk, mvv = [mu[i * 1024:(i + 1) * 1024][g * 512:(g + 1) * 512] for i in range(3)]
    lora = np.zeros((2048, 512), f); lora[:, :288] = wb[:, 3072:3360]
    mlo = np.zeros(512, f); mlo[:288] = mu[3072:3360]
    fm = [_kmajor(rq), _kmajor(rk), _kmajor(lora)]
    mfm = [np.zeros(512, f), np.zeros(512, f), mlo]
    if layer2:
        fm += [_kmajor(wb[:, 2048:2560]), _kmajor(wb[:, 2560:3072])]
        mfm += [mu[2048:2560], mu[2560:3072]]
    tm = [_kmajor(rv), _kmajor(rg), _kmajor(wr), _kmajor(wk), _kmajor(wv)]
    mtm = [np.zeros(512, f), np.zeros(512, f), mr, mk, mvv]
    c, c2 = even_consts(g)
    rows = np.zeros((12, 512), f)
    sl = slice(g * 512, (g + 1) * 512)
    rows[0] = np.asarray(p["ret_gn_g"], f)[sl]; rows[1] = np.asarray(p["ret_gn_b"], f)[sl]
    rows[2] = np.asarray(p["w0"], f)[sl]; rows[3] = np.asarray(p["a0"], f)[sl]
    rows[4] = np.asarray(p["kk"], f)[sl]; rows[5] = np.asarray(p["ka"], f)[sl]
    rows[6] = np.asarray(p["rk"], f).reshape(-1)[sl]; rows[7] = np.asarray(p["lnx_g"], f)[sl]; rows[8] = np.asarray(p["lnx_b"], f)[sl]
    d = {"hT": hT_b, "Wfm": np.stack(fm), "Wtm": np.stack(tm), "mufm": np.stack(mfm).astype(f), "mutm": np.stack(mtm).astype(f),
         "posi": np.ascontiguousarray(np.asarray(pos_b, np.int32)[None, :]), "cst": c, "cst2": c2,
         "lw2": np.ascontiguousarray(np.asarray(p["w2"], f)[:, sl]), "la2": np.ascontiguousarray(np.asarray(p["a2"], f)[:, sl]),
         "lg2": np.ascontiguousarray(np.asarray(p["g2"], f)[:, sl])}
    if layer2:
        rows[9] = np.asarray(p["v0"], f)[sl]
        d["lv1"] = _kmajor(np.asarray(p["v1"], f))
        d["lv2"] = np.ascontiguousarray(np.asarray(p["v2"], f)[:, sl])
        d["vfirst"] = vfirst
    d["rowsd"] = rows
    return d


_CACHE = {}
def _get(name, fn, *a):
    key = (name,) + a
    if key not in _CACHE:
        _CACHE[key] = fn(*a)
    return _CACHE[key]


def _run(nc, in_maps):
    res = run_bass_kernel_spmd(nc, in_maps, core_ids=list(range(NCORE)))
    return res.results


def kernel(x, c, positions, ada_w, ada_b, ln_g, ln_b, ev_win, ev_wo, ret_gn_g, ret_gn_b,
           rw_mu, rw_w0, rw_w2, rw_a0, rw_a2, rw_g2, rw_kk, rw_ka, rw_rk, rw_lnx_g, rw_lnx_b,
           rw_v0, rw_v1, rw_v2, od_win, od_wo, gla_a2, gla_ab, gla_norm_g,
           moe_wg, moe_bg, moe_we, moe_be, moe_w1, moe_w3, moe_w2):
    f = np.float32
    x = np.asarray(x, f); c = np.asarray(c, f)
    positions = np.asarray(positions)
    vfirst = None
    T = x.shape[0] * x.shape[1]
    xs = x.reshape(NCORE, TOK, D)
    cT = _kmajor(np.ascontiguousarray(c.T))
    ada_w = np.asarray(ada_w, f); ada_b = np.asarray(ada_b, f)
    ins = []
    for i in range(NCORE):
        cs = slice(i * 1536, (i + 1) * 1536)
        aw = np.stack([_kmajor(ada_w[l][:, cs]) for l in range(DEPTH)])
        ab = np.ascontiguousarray(np.broadcast_to(ada_b[:, None, cs], (DEPTH, 4, 1536)))
        ins.append({"cT": cT, "adaw": aw, "adab": ab})
    r = _run(_get("ada", build_ada), ins)
    mod = np.concatenate([r[i]["mod"] for i in range(NCORE)], axis=-1)
    def seg(l, b, j):
        return mod[l, b, j * D:(j + 1) * D]
    zero = np.zeros(D, f)
    def rows_for(b, gt, g, bb, sc, sh):
        return np.ascontiguousarray(np.stack([np.asarray(v, np.float32) for v in (gt, g, bb, sc, sh)]))
    ln_g = np.asarray(ln_g, f); ln_b = np.asarray(ln_b, f)
    ins = [{"x": xs[i], "rows": rows_for(i // 2, zero, zero, zero, seg(0, i // 2, 1), seg(0, i // 2, 0))} for i in range(NCORE)]
    r = _run(_get("post_mod", build_post, "mod"), ins)
    h = [r[i]["ho"] for i in range(NCORE)]
    xcur = [xs[i] for i in range(NCORE)]
    for l in range(DEPTH):
        j = l // 2
        wo = np.asarray(ev_wo[j] if l % 2 == 0 else od_wo[j], f)
        hTb = [_kmajor(np.ascontiguousarray(np.concatenate([h[2 * b], h[2 * b + 1]], axis=0).T)) for b in range(4)]
        if l % 2 == 0:
            layer2 = (j == 1)
            p = {"mu": rw_mu[j], "ret_gn_g": ret_gn_g[j], "ret_gn_b": ret_gn_b[j], "w0": rw_w0[j], "w2": rw_w2[j], "a0": rw_a0[j],
                 "a2": rw_a2[j], "g2": rw_g2[j], "kk": rw_kk[j], "ka": rw_ka[j], "rk": rw_rk[j], "lnx_g": rw_lnx_g[j], "lnx_b": rw_lnx_b[j]}
            if layer2:
                p.update({"v0": rw_v0[j - 1], "v1": rw_v1[j - 1], "v2": rw_v2[j - 1]})
            ins = [even_inputs(hTb[i // 2], i % 2, positions[i // 2], ev_win[j], layer2, p, vfirst[i] if layer2 else None) for i in range(NCORE)]
            r = _run(_get("even", build_even, layer2), ins)
            if not layer2:
                vfirst = [r[i]["vout"] for i in range(NCORE)]
            featb = []
            for b in range(4):
                fb = np.empty((T_SEQ, D), f)
                for g in range(2):
                    fc = r[2 * b + g]["feat"]
                    fb[:, g * 512:(g + 1) * 512] = fc[:, 0:512]
                    fb[:, 1024 + g * 512:1024 + (g + 1) * 512] = fc[:, 512:1024]
                featb.append(fb)
        else:
            ins = [gla_inputs(hTb[i // 2], i % 2, od_win[j], gla_a2[j], gla_ab[j], gla_norm_g[j]) for i in range(NCORE)]
            r = _run(_get("gla", build_gla), ins)
            featb = [np.concatenate([r[2 * b]["feat"], r[2 * b + 1]["feat"]], axis=1) for b in range(4)]
        feat = [featb[i // 2][(i % 2) * TOK:(i % 2 + 1) * TOK] for i in range(NCORE)]
        wo_l = _kmajor(wo)
        ins = []
        for i in range(NCORE):
            b = i // 2
            ins.append({"x": xcur[i], "fT": _kmajor(np.ascontiguousarray(feat[i].T)), "wo": wo_l,
                        "rows": rows_for(b, seg(l, b, 2), ln_g[l, 0], ln_b[l, 0], seg(l, b, 4), seg(l, b, 3))})
        r = _run(_get("post_wo", build_post, "wo"), ins)
        xcur = [r[i]["xo"] for i in range(NCORE)]
        h2 = [r[i]["ho"] for i in range(NCORE)]
        wge = _kmajor(np.concatenate([np.asarray(moe_wg[l], f), np.asarray(moe_we[l], f)], axis=1))
        bge = _tile_rows(np.concatenate([np.asarray(moe_bg[l], f), np.asarray(moe_be[l], f)]))
        h2T = [_kmajor(np.ascontiguousarray(h2[i].T)) for i in range(NCORE)]
        r = _run(_get("route", build_route), [{"hT": h2T[i], "wge": wge, "bge": bge} for i in range(NCORE)])
        G = np.concatenate([r[i]["G"] for i in range(NCORE)], axis=0)
        MG = np.concatenate([r[i]["MG"] for i in range(NCORE)], axis=0)
        h2_all = np.concatenate(h2, axis=0)
        idx = [np.nonzero(MG[:, g])[0] for g in range(4)]
        cap = max(512, -(-max(len(ix) for ix in idx) // 512) * 512)
        ins = []
        for i in range(NCORE):
            g = i // 2
            hb = np.zeros((cap, D), f); hb[:len(idx[g])] = h2_all[idx[g]]
            gbr = np.zeros((4, cap), f); gbr[:, :len(idx[g])] = G[idx[g], 4 * i:4 * i + 4].T
            ins.append({"hT": _kmajor(np.ascontiguousarray(hb.T)), "gb": gbr,
                        "w1": np.stack([_kmajor(np.asarray(moe_w1[l, e], f)) for e in range(4 * i, 4 * i + 4)]),
                        "w3": np.stack([_kmajor(np.asarray(moe_w3[l, e], f)) for e in range(4 * i, 4 * i + 4)]),
                        "w2": np.stack([_kmajor(np.asarray(moe_w2[l, e], f)) for e in range(4 * i, 4 * i + 4)])})
        r = _run(_get("moe", build_moe, cap), ins)
        ypart = np.zeros((2, T, D), f)
        for i in range(NCORE):
            g = i // 2
            ypart[i % 2, idx[g]] = r[i]["y"][:len(idx[g])]
        ins = []
        for i in range(NCORE):
            b = i // 2
            yp = np.ascontiguousarray(ypart[:, i * TOK:(i + 1) * TOK])
            if l + 1 < DEPTH:
                sc, sh = seg(l + 1, b, 1), seg(l + 1, b, 0)
            else:
                sc, sh = zero, zero
            ins.append({"x": xcur[i], "yp": yp, "rows": rows_for(b, seg(l, b, 5), ln_g[l, 1], ln_b[l, 1], sc, sh)})
        lastl = (l + 1 == DEPTH)
        r = _run(_get("post_y", build_post, "y", 2, not lastl), ins)
        xcur = [r[i]["xo"] for i in range(NCORE)]
        if not lastl:
            h = [r[i]["ho"] for i in range(NCORE)]
    return np.concatenate(xcur, axis=0).reshape(x.shape).astype(np.float32)
```

```python
import contextlib
import numpy as np
import concourse.bass as bass
import concourse.mybir as mybir

F32 = mybir.dt.float32
I32 = mybir.dt.int32
ALU = mybir.AluOpType
AF = mybir.ActivationFunctionType
AX = mybir.AxisListType

ENGS = ("pe", "act", "dve", "pool", "sp")
EPOCH = 30000
NDMASEM = 24


class Buf:
    __slots__ = ("name", "w", "r")

    def __init__(self, name):
        self.name = name
        self.w = None
        self.r = []


class FW:
    def __init__(self):
        self.nc = bass.Bass("TRN2", target_bir_lowering=False)
        self.stack = contextlib.ExitStack()
        self.prog = {e: [] for e in ENGS}
        self.cnt = {e: 0 for e in ENGS}
        self.epoch_sem = {}
        self.known = {e: {} for e in ENGS}
        self.sems = {}
        self.nsem = 0
        for e in ENGS:
            self.epoch_sem[e] = self._newsem()
        self.dma_sems = [self._newsem() for _ in range(NDMASEM)]
        self.dma_cnt = [0] * NDMASEM
        self.dma_rr = 0
        self.final_tokens = []
        self.nbuf = 0

    def _newsem(self):
        s = self.stack.enter_context(self.nc.semaphore(f"s{self.nsem}"))
        self.nsem += 1
        self.sems[id(s)] = s
        return s

    def sbuf(self, shape, dtype=F32, name=None):
        self.nbuf += 1
        name = name or f"sb{self.nbuf}"
        t = self.stack.enter_context(self.nc.sbuf_tensor(name, list(shape), dtype))
        return t

    def psum(self, shape, dtype=F32, name=None):
        self.nbuf += 1
        name = name or f"ps{self.nbuf}"
        t = self.stack.enter_context(self.nc.psum_tensor(name, list(shape), dtype))
        return t

    def dram(self, name, shape, dtype=F32, kind="Internal"):
        return self.nc.dram_tensor(name, list(shape), dtype, kind=kind)

    def _need(self, eng, tokens):
        best = {}
        for tk in tokens:
            if tk is None:
                continue
            s, v = tk
            if best.get(id(s), 0) < v:
                best[id(s)] = v
        for sid, v in best.items():
            if self.known[eng].get(sid, 0) >= v:
                continue
            self.known[eng][sid] = v
            self.prog[eng].append(("wait", self.sems[sid], v))

    def _deps(self, reads, writes):
        toks = []
        for b in reads:
            toks.append(b.w)
        for b in writes:
            toks.append(b.w)
            toks.extend(b.r)
        return toks

    def op(self, eng, fn, reads=(), writes=()):
        self._need(eng, self._deps(reads, writes))
        if self.cnt[eng] >= EPOCH:
            self.epoch_sem[eng] = self._newsem()
            self.cnt[eng] = 0
        self.cnt[eng] += 1
        tok = (self.epoch_sem[eng], self.cnt[eng])
        self.prog[eng].append(("op", fn, tok[0], 1))
        for b in reads:
            b.r.append(tok)
        for b in writes:
            b.w = tok
            b.r = []
        return tok

    def dma(self, eng, fn, reads=(), writes=(), inc=16):
        slot = self.dma_rr
        self.dma_rr = (self.dma_rr + 1) % NDMASEM
        s = self.dma_sems[slot]
        prev = self.dma_cnt[slot]
        toks = self._deps(reads, writes)
        if prev > 0:
            toks.append((s, prev))
        self._need(eng, toks)
        self.dma_cnt[slot] = prev + inc
        tok = (s, prev + inc)
        self.prog[eng].append(("op", fn, s, inc))
        for b in reads:
            b.r.append(tok)
        for b in writes:
            b.w = tok
            b.r = []
        return tok

    def barrier(self):
        toks = []
        for e in ENGS:
            if self.cnt[e] > 0:
                toks.append((self.epoch_sem[e], self.cnt[e]))
        for i in range(NDMASEM):
            if self.dma_cnt[i] > 0:
                toks.append((self.dma_sems[i], self.dma_cnt[i]))
        for e in ENGS:
            self._need(e, list(toks))

    def finish(self, final_bufs):
        toks = []
        for b in final_bufs:
            toks.append(b.w)
        self._need("sp", toks)
        nc = self.nc
        with nc.Block() as block:
            def run(engname):
                def body(e):
                    for item in self.prog[engname]:
                        if item[0] == "wait":
                            e.wait_ge(item[1], item[2])
                        else:
                            ins = item[1](e)
                            ins.then_inc(item[2], item[3])
                return body
            block.tensor(run("pe"))
            block.scalar(run("act"))
            block.vector(run("dve"))
            block.gpsimd(run("pool"))
            block.sync(run("sp"))
        self.stack.close()
        return nc


from concourse.bass_utils import run_bass_kernel_spmd

D = 2048
DEPTH = 4
NCORE = 8
TOK = 1024
ALPHA = (2 * DEPTH) ** 0.25
NEG = -1.0e30


def _tile_rows(v):
    return np.ascontiguousarray(np.broadcast_to(np.asarray(v, np.float32)[None, :], (128, v.shape[-1])))


def _kmajor(w):
    K, N = w.shape
    return np.ascontiguousarray(w.reshape(K // 128, 128, N).transpose(1, 0, 2))


def build_ada():
    fw = FW(); nc = fw.nc
    cT = nc.dram_tensor("cT", [128, 16, 4], F32, kind="ExternalInput")
    adaw = nc.dram_tensor("adaw", [DEPTH, 128, 16, 1536], F32, kind="ExternalInput")
    adab = nc.dram_tensor("adab", [DEPTH, 4, 1536], F32, kind="ExternalInput")
    mod = nc.dram_tensor("mod", [DEPTH, 4, 1536], F32, kind="ExternalOutput")
    B_mod = Buf("mod")
    ct = fw.sbuf([128, 16, 4]); B_ct = Buf("ct")
    ca = fw.sbuf([128, 16, 4]); B_ca = Buf("ca")
    wts = [fw.sbuf([128, 16, 512], name=f"w{i}") for i in range(2)]; B_w = [Buf("w0"), Buf("w1")]
    bt = fw.sbuf([4, DEPTH, 1536]); B_bt = Buf("bt")
    ots = [fw.sbuf([4, 512], name=f"o{i}") for i in range(2)]; B_o = [Buf("o0"), Buf("o1")]
    pss = [fw.psum([128, 512], name=f"p{i}") for i in range(2)]; B_p = [Buf("p0"), Buf("p1")]
    fw.dma("sp", lambda e: e.dma_start(out=ct[:], in_=cT[:, :, :]), writes=[B_ct])
    for l in range(DEPTH):
        fw.dma("sp", lambda e, l=l: e.dma_start(out=bt[:, l, :], in_=adab[l, :, :]), writes=[B_bt])
    fw.op("act", lambda e: e.activation(out=ca[:], in_=ct[:], func=AF.Silu), reads=[B_ct], writes=[B_ca])
    it = 0
    for l in range(DEPTH):
        for n in range(3):
            wt, Bw = wts[it % 2], B_w[it % 2]
            ps, Bp = pss[it % 2], B_p[it % 2]
            ot, Bo = ots[it % 2], B_o[it % 2]
            fw.dma("sp" if it % 2 == 0 else "act", lambda e, l=l, n=n, wt=wt: e.dma_start(out=wt[:], in_=adaw[l, :, :, n * 512:(n + 1) * 512]), writes=[Bw])
            for k in range(16):
                fw.op("pe", lambda e, k=k, wt=wt, ps=ps: e.matmul(ps[0:4, :], lhsT=ca[:, k, :], rhs=wt[:, k, :], start=(k == 0), stop=(k == 15)),
                      reads=[B_ca, Bw], writes=[Bp] if k in (0, 15) else [])
            fw.op("dve", lambda e, l=l, n=n, ps=ps, ot=ot: e.tensor_tensor(out=ot[:], in0=ps[0:4, :], in1=bt[:, l, n * 512:(n + 1) * 512], op=ALU.add),
                  reads=[Bp, B_bt], writes=[Bo])
            fw.dma("sp", lambda e, l=l, n=n, ot=ot: e.dma_start(out=mod[l, :, n * 512:(n + 1) * 512], in_=ot[:]), reads=[Bo], writes=[B_mod])
            it += 1
    return fw.finish([B_mod])


def build_post(mode, nparts=1, want_h=True):
    fw = FW(); nc = fw.nc
    NT = TOK // 128
    x = nc.dram_tensor("x", [TOK, D], F32, kind="ExternalInput")
    rows = nc.dram_tensor("rows", [5, D], F32, kind="ExternalInput")
    finals = []
    if want_h:
        ho = nc.dram_tensor("ho", [TOK, D], F32, kind="ExternalOutput"); B_ho = Buf("ho")
        finals.append(B_ho)
    if mode != "mod":
        xo = nc.dram_tensor("xo", [TOK, D], F32, kind="ExternalOutput"); B_xo = Buf("xo")
        finals.append(B_xo)
    if mode == "wo":
        fT = nc.dram_tensor("fT", [128, 16, TOK], F32, kind="ExternalInput")
        wo = nc.dram_tensor("wo", [128, 16, D], F32, kind="ExternalInput")
        wot = fw.sbuf([128, 16, D]); B_wo = Buf("wo")
        fw.dma("sp", lambda e: e.dma_start(out=wot[:, 0:8, :], in_=wo[:, 0:8, :]), writes=[B_wo])
        fw.dma("act", lambda e: e.dma_start(out=wot[:, 8:16, :], in_=wo[:, 8:16, :]), writes=[B_wo])
        ft = fw.sbuf([128, 16, 128]); B_ft = Buf("ft")
        pss = [fw.psum([128, 512], name=f"p{i}") for i in range(4)]; B_p = [Buf(f"p{i}") for i in range(4)]
    if mode == "y":
        yp = nc.dram_tensor("yp", [nparts, TOK, D], F32, kind="ExternalInput")
        ypt = fw.sbuf([128, D]); B_ypt = Buf("ypt")
    rt = fw.sbuf([128, 5, D]); B_rt = Buf("rt")
    for j in range(5):
        fw.dma("pool", lambda e, j=j: e.dma_start(out=rt[:, j, :], in_=rows[j:j + 1, :].partition_broadcast(128)), writes=[B_rt])
    fw.op("pool", lambda e: e.tensor_scalar_add(out=rt[:, 0, :], in0=rt[:, 0, :], scalar1=1.0), reads=[B_rt], writes=[B_rt])
    fw.op("pool", lambda e: e.tensor_scalar_add(out=rt[:, 3, :], in0=rt[:, 3, :], scalar1=1.0), reads=[B_rt], writes=[B_rt])
    xt = fw.sbuf([128, D]); B_xt = Buf("xt")
    yt = fw.sbuf([128, D]); B_yt = Buf("yt")
    ht = fw.sbuf([128, D]); B_ht = Buf("ht")
    st = fw.sbuf([128, 4, 6]); B_st = Buf("st")
    mv = fw.sbuf([128, 2]); B_mv = Buf("mv")
    rs = fw.sbuf([128, 1]); B_rs = Buf("rs")
    for t in range(NT):
        ts_ = slice(t * 128, (t + 1) * 128)
        fw.dma("sp", lambda e, ts_=ts_: e.dma_start(out=xt[:], in_=x[ts_, :]), writes=[B_xt])
        if mode == "mod":
            src, B_src = xt, B_xt
        else:
            if mode == "wo":
                fw.dma("act", lambda e, ts_=ts_: e.dma_start(out=ft[:], in_=fT[:, :, ts_]), writes=[B_ft])
                for c in range(4):
                    for k in range(16):
                        fw.op("pe", lambda e, c=c, k=k: e.matmul(pss[c][:, :], lhsT=ft[:, k, :], rhs=wot[:, k, c * 512:(c + 1) * 512], start=(k == 0), stop=(k == 15)),
                              reads=[B_ft, B_wo], writes=[B_p[c]] if k in (0, 15) else [])
                    fw.op("dve", lambda e, c=c: e.tensor_tensor(out=yt[:, c * 512:(c + 1) * 512], in0=pss[c][:, :], in1=rt[:, 0, c * 512:(c + 1) * 512], op=ALU.mult),
                          reads=[B_p[c], B_rt], writes=[B_yt])
            else:
                fw.dma("act", lambda e, ts_=ts_: e.dma_start(out=yt[:], in_=yp[0, ts_, :]), writes=[B_yt])
                for p in range(1, nparts):
                    fw.dma("act", lambda e, ts_=ts_, p=p: e.dma_start(out=ypt[:], in_=yp[p, ts_, :]), writes=[B_ypt])
                    fw.op("pool", lambda e: e.tensor_tensor(out=yt[:], in0=yt[:], in1=ypt[:], op=ALU.add), reads=[B_yt, B_ypt], writes=[B_yt])
                fw.op("dve", lambda e: e.tensor_tensor(out=yt[:], in0=yt[:], in1=rt[:, 0, :], op=ALU.mult), reads=[B_yt, B_rt], writes=[B_yt])
            fw.op("dve", lambda e: e.scalar_tensor_tensor(out=yt[:], in0=xt[:], scalar=ALPHA, in1=yt[:], op0=ALU.mult, op1=ALU.add),
                  reads=[B_xt, B_yt], writes=[B_yt])
            for c in range(4):
                fw.op("dve", lambda e, c=c: e.bn_stats(out=st[:, c, :], in_=yt[:, c * 512:(c + 1) * 512]), reads=[B_yt], writes=[B_st])
            fw.op("dve", lambda e: e.bn_aggr(out=mv[:], in_=st[:].rearrange("p a b -> p (a b)")), reads=[B_st], writes=[B_mv])
            fw.op("dve", lambda e: e.tensor_scalar_add(out=rs[:], in0=mv[:, 1:2], scalar1=1e-5), reads=[B_mv], writes=[B_rs])
            fw.op("act", lambda e: e.activation(out=rs[:], in_=rs[:], func=AF.Sqrt), reads=[B_rs], writes=[B_rs])
            fw.op("dve", lambda e: e.reciprocal(out=rs[:], in_=rs[:]), reads=[B_rs], writes=[B_rs])
            fw.op("dve", lambda e: e.tensor_scalar(out=yt[:], in0=yt[:], scalar1=mv[:, 0:1], scalar2=rs[:, 0:1], op0=ALU.subtract, op1=ALU.mult),
                  reads=[B_yt, B_mv, B_rs], writes=[B_yt])
            fw.op("pool", lambda e: e.tensor_tensor(out=yt[:], in0=yt[:], in1=rt[:, 1, :], op=ALU.mult), reads=[B_yt, B_rt], writes=[B_yt])
            fw.op("pool", lambda e: e.tensor_tensor(out=yt[:], in0=yt[:], in1=rt[:, 2, :], op=ALU.add), reads=[B_yt, B_rt], writes=[B_yt])
            fw.dma("sp", lambda e, ts_=ts_: e.dma_start(out=xo[ts_, :], in_=yt[:]), reads=[B_yt], writes=[B_xo])
            src, B_src = yt, B_yt
        if want_h:
            fw.op("dve", lambda e, src=src: e.tensor_tensor(out=ht[:], in0=src[:], in1=rt[:, 3, :], op=ALU.mult), reads=[B_src, B_rt], writes=[B_ht])
            fw.op("dve", lambda e: e.tensor_tensor(out=ht[:], in0=ht[:], in1=rt[:, 4, :], op=ALU.add), reads=[B_ht, B_rt], writes=[B_ht])
            fw.dma("sp", lambda e, ts_=ts_: e.dma_start(out=ho[ts_, :], in_=ht[:]), reads=[B_ht], writes=[B_ho])
    return fw.finish(finals)


def build_route():
    fw = FW(); nc = fw.nc
    NT = TOK // 128
    hT = nc.dram_tensor("hT", [128, 16, TOK], F32, kind="ExternalInput")
    wge = nc.dram_tensor("wge", [128, 16, 36], F32, kind="ExternalInput")
    bge = nc.dram_tensor("bge", [128, 36], F32, kind="ExternalInput")
    G = nc.dram_tensor("G", [TOK, 32], F32, kind="ExternalOutput"); B_G = Buf("G")
    MG = nc.dram_tensor("MG", [TOK, 4], F32, kind="ExternalOutput"); B_MG = Buf("MG")
    MK = nc.dram_tensor("MK", [TOK, 32], F32, kind="ExternalOutput"); B_MK = Buf("MK")
    wt = fw.sbuf([128, 16, 36]); B_wt = Buf("wt")
    bt = fw.sbuf([128, 36]); B_bt = Buf("bt")
    fw.dma("sp", lambda e: e.dma_start(out=wt[:], in_=wge[:, :, :]), writes=[B_wt])
    fw.dma("sp", lambda e: e.dma_start(out=bt[:], in_=bge[:, :]), writes=[B_bt])
    ht = fw.sbuf([128, 16, 128]); B_ht = Buf("ht")
    ps = fw.psum([128, 512]); B_ps = Buf("ps")
    def sb(shape, name):
        return fw.sbuf(shape, name=name), Buf(name)
    lg, B_lg = sb([128, 36], "lg")
    mx, B_mx = sb([128, 1], "mx"); nmx, B_nmx = sb([128, 1], "nmx")
    mg, B_mg = sb([128, 4], "mg"); eg, B_eg = sb([128, 4], "eg")
    sm, B_sm = sb([128, 1], "sm"); pt, B_pt = sb([128, 1], "pt")
    pen, B_pen = sb([128, 4], "pen")
    lem, B_lem = sb([128, 32], "lem"); lem2, B_lem2 = sb([128, 32], "lem2")
    m1, B_m1 = sb([128, 1], "m1"); m2, B_m2 = sb([128, 1], "m2")
    k1, B_k1 = sb([128, 32], "k1"); k2, B_k2 = sb([128, 32], "k2")
    dd, B_dd = sb([128, 1], "dd"); ed, B_ed = sb([128, 1], "ed")
    w1, B_w1 = sb([128, 1], "w1"); w2, B_w2 = sb([128, 1], "w2")
    gt, B_gt = sb([128, 32], "gt")
    for t in range(NT):
        ts_ = slice(t * 128, (t + 1) * 128)
        fw.dma("sp", lambda e, ts_=ts_: e.dma_start(out=ht[:], in_=hT[:, :, ts_]), writes=[B_ht])
        for k in range(16):
            fw.op("pe", lambda e, k=k: e.matmul(ps[:, 0:36], lhsT=ht[:, k, :], rhs=wt[:, k, :], start=(k == 0), stop=(k == 15)),
                  reads=[B_ht, B_wt], writes=[B_ps] if k in (0, 15) else [])
        fw.op("dve", lambda e: e.tensor_tensor(out=lg[:], in0=ps[:, 0:36], in1=bt[:], op=ALU.add), reads=[B_ps, B_bt], writes=[B_lg])
        fw.op("dve", lambda e: e.reduce_max(out=mx[:], in_=lg[:, 0:4], axis=AX.X), reads=[B_lg], writes=[B_mx])
        fw.op("dve", lambda e: e.tensor_scalar(out=mg[:], in0=lg[:, 0:4], scalar1=mx[:, 0:1], scalar2=None, op0=ALU.is_equal), reads=[B_lg, B_mx], writes=[B_mg])
        fw.op("dve", lambda e: e.tensor_scalar_mul(out=nmx[:], in0=mx[:], scalar1=-1.0), reads=[B_mx], writes=[B_nmx])
        fw.op("act", lambda e: e.activation(out=eg[:], in_=lg[:, 0:4], func=AF.Exp, bias=nmx[:, 0:1], scale=1.0), reads=[B_lg, B_nmx], writes=[B_eg])
        fw.op("dve", lambda e: e.reduce_sum(out=sm[:], in_=eg[:], axis=AX.X), reads=[B_eg], writes=[B_sm])
        fw.op("dve", lambda e: e.reciprocal(out=pt[:], in_=sm[:]), reads=[B_sm], writes=[B_pt])
        fw.op("dve", lambda e: e.tensor_scalar(out=pen[:], in0=mg[:], scalar1=-1.0, scalar2=-NEG, op0=ALU.add, op1=ALU.mult), reads=[B_mg], writes=[B_pen])
        fw.op("dve", lambda e: e.tensor_tensor(out=lem[:].rearrange("p (g e) -> p g e", g=4), in0=lg[:, 4:36].rearrange("p (g e) -> p g e", g=4),
                                               in1=pen[:].unsqueeze(2).to_broadcast([128, 4, 8]), op=ALU.add), reads=[B_lg, B_pen], writes=[B_lem])
        fw.op("dve", lambda e: e.reduce_max(out=m1[:], in_=lem[:], axis=AX.X), reads=[B_lem], writes=[B_m1])
        fw.op("dve", lambda e: e.tensor_scalar(out=k1[:], in0=lem[:], scalar1=m1[:, 0:1], scalar2=None, op0=ALU.is_equal), reads=[B_lem, B_m1], writes=[B_k1])
        fw.op("dve", lambda e: e.scalar_tensor_tensor(out=lem2[:], in0=k1[:], scalar=NEG, in1=lem[:], op0=ALU.mult, op1=ALU.add), reads=[B_k1, B_lem], writes=[B_lem2])
        fw.op("dve", lambda e: e.reduce_max(out=m2[:], in_=lem2[:], axis=AX.X), reads=[B_lem2], writes=[B_m2])
        fw.op("dve", lambda e: e.tensor_scalar(out=k2[:], in0=lem2[:], scalar1=m2[:, 0:1], scalar2=None, op0=ALU.is_equal), reads=[B_lem2, B_m2], writes=[B_k2])
        fw.op("dve", lambda e: e.tensor_tensor(out=dd[:], in0=m2[:], in1=m1[:], op=ALU.subtract), reads=[B_m1, B_m2], writes=[B_dd])
        fw.op("act", lambda e: e.activation(out=ed[:], in_=dd[:], func=AF.Exp), reads=[B_dd], writes=[B_ed])
        fw.op("dve", lambda e: e.tensor_scalar_add(out=w1[:], in0=ed[:], scalar1=1.0), reads=[B_ed], writes=[B_w1])
        fw.op("dve", lambda e: e.reciprocal(out=w1[:], in_=w1[:]), reads=[B_w1], writes=[B_w1])
        fw.op("dve", lambda e: e.tensor_tensor(out=w2[:], in0=ed[:], in1=w1[:], op=ALU.mult), reads=[B_ed, B_w1], writes=[B_w2])
        fw.op("dve", lambda e: e.tensor_tensor(out=w1[:], in0=w1[:], in1=pt[:], op=ALU.mult), reads=[B_w1, B_pt], writes=[B_w1])
        fw.op("dve", lambda e: e.tensor_tensor(out=w2[:], in0=w2[:], in1=pt[:], op=ALU.mult), reads=[B_w2, B_pt], writes=[B_w2])
        fw.op("dve", lambda e: e.tensor_scalar(out=gt[:], in0=k1[:], scalar1=w1[:, 0:1], scalar2=None, op0=ALU.mult), reads=[B_k1, B_w1], writes=[B_gt])
        fw.op("dve", lambda e: e.scalar_tensor_tensor(out=gt[:], in0=k2[:], scalar=w2[:, 0:1], in1=gt[:], op0=ALU.mult, op1=ALU.add), reads=[B_k2, B_w2, B_gt], writes=[B_gt])
        fw.dma("sp", lambda e, ts_=ts_: e.dma_start(out=G[ts_, :], in_=gt[:]), reads=[B_gt], writes=[B_G])
        fw.dma("sp", lambda e, ts_=ts_: e.dma_start(out=MG[ts_, :], in_=mg[:]), reads=[B_mg], writes=[B_MG])
        fw.op("dve", lambda e: e.tensor_tensor(out=k1[:], in0=k1[:], in1=k2[:], op=ALU.add), reads=[B_k1, B_k2], writes=[B_k1])
        fw.dma("sp", lambda e, ts_=ts_: e.dma_start(out=MK[ts_, :], in_=k1[:]), reads=[B_k1], writes=[B_MK])
    return fw.finish([B_G, B_MG, B_MK])


def build_moe(ntok):
    fw = FW(); nc = fw.nc
    NE = 4
    TB = 512
    NB = ntok // TB
    hT = nc.dram_tensor("hT", [128, 16, ntok], F32, kind="ExternalInput")
    gb = nc.dram_tensor("gb", [NE, ntok], F32, kind="ExternalInput")
    w1 = nc.dram_tensor("w1", [NE, 128, 16, 512], F32, kind="ExternalInput")
    w3 = nc.dram_tensor("w3", [NE, 128, 16, 512], F32, kind="ExternalInput")
    w2 = nc.dram_tensor("w2", [NE, 128, 4, D], F32, kind="ExternalInput")
    y = nc.dram_tensor("y", [ntok, D], F32, kind="ExternalOutput"); B_y = Buf("y")
    def sb(shape, name):
        return fw.sbuf(shape, name=name), Buf(name)
    w1t, B_w1 = sb([128, 16, 512], "w1t"); w3t, B_w3 = sb([128, 16, 512], "w3t"); w2t, B_w2 = sb([128, 4, D], "w2t")
    ht, B_ht = sb([128, 16, TB], "ht")
    gt, B_gt = sb([128, TB], "gt")
    a1, B_a1 = sb([128, TB], "a1")
    at, B_at = sb([128, 4, TB], "at")
    ya, B_ya = sb([128, 4, D], "ya")
    pA = [fw.psum([128, 512], name=f"pa{i}") for i in range(2)]; B_pA = [Buf("pa0"), Buf("pa1")]
    pB = [fw.psum([128, 512], name=f"pb{i}") for i in range(2)]; B_pB = [Buf("pb0"), Buf("pb1")]
    pY = [fw.psum([128, 512], name=f"py{i}") for i in range(4)]; B_pY = [Buf(f"py{i}") for i in range(4)]
    for b in range(NB):
        bs = slice(b * TB, (b + 1) * TB)
        fw.dma("sp", lambda e, bs=bs: e.dma_start(out=ht[:, 0:8, :], in_=hT[:, 0:8, bs]), writes=[B_ht])
        fw.dma("act", lambda e, bs=bs: e.dma_start(out=ht[:, 8:16, :], in_=hT[:, 8:16, bs]), writes=[B_ht])
        for ex in range(NE):
            fw.dma("sp", lambda e, ex=ex: e.dma_start(out=w1t[:], in_=w1[ex, :, :, :]), writes=[B_w1])
            fw.dma("act", lambda e, ex=ex: e.dma_start(out=w3t[:], in_=w3[ex, :, :, :]), writes=[B_w3])
            fw.dma("pool", lambda e, ex=ex: e.dma_start(out=w2t[:], in_=w2[ex, :, :, :]), writes=[B_w2])
            fw.dma("pool", lambda e, ex=ex, bs=bs: e.dma_start(out=gt[:], in_=gb[ex:ex + 1, bs].partition_broadcast(128)), writes=[B_gt])
            for f in range(4):
                pa, Bpa = pA[f % 2], B_pA[f % 2]
                pb, Bpb = pB[f % 2], B_pB[f % 2]
                for k in range(16):
                    fw.op("pe", lambda e, k=k, f=f, pa=pa: e.matmul(pa[:, :], lhsT=w1t[:, k, f * 128:(f + 1) * 128], rhs=ht[:, k, :], start=(k == 0), stop=(k == 15)),
                          reads=[B_w1, B_ht], writes=[Bpa] if k in (0, 15) else [])
                for k in range(16):
                    fw.op("pe", lambda e, k=k, f=f, pb=pb: e.matmul(pb[:, :], lhsT=w3t[:, k, f * 128:(f + 1) * 128], rhs=ht[:, k, :], start=(k == 0), stop=(k == 15)),
                          reads=[B_w3, B_ht], writes=[Bpb] if k in (0, 15) else [])
                fw.op("act", lambda e, pa=pa: e.activation(out=a1[:], in_=pa[:, :], func=AF.Silu), reads=[Bpa], writes=[B_a1])
                fw.op("dve", lambda e, pb=pb, f=f: e.tensor_tensor(out=at[:, f, :], in0=pb[:, :], in1=a1[:], op=ALU.mult), reads=[Bpb, B_a1], writes=[B_at])
                fw.op("pool", lambda e, f=f: e.tensor_tensor(out=at[:, f, :], in0=at[:, f, :], in1=gt[:], op=ALU.mult), reads=[B_at, B_gt], writes=[B_at])
            for tt in range(4):
                for c in range(4):
                    for f in range(4):
                        fw.op("pe", lambda e, tt=tt, c=c, f=f: e.matmul(pY[c][:, :], lhsT=at[:, f, tt * 128:(tt + 1) * 128], rhs=w2t[:, f, c * 512:(c + 1) * 512], start=(f == 0), stop=(f == 3)),
                              reads=[B_at, B_w2], writes=[B_pY[c]] if f in (0, 3) else [])
                    if ex == 0:
                        fw.op("act", lambda e, tt=tt, c=c: e.copy(out=ya[:, tt, c * 512:(c + 1) * 512], in_=pY[c][:, :]), reads=[B_pY[c]], writes=[B_ya])
                    else:
                        fw.op("dve", lambda e, tt=tt, c=c: e.tensor_tensor(out=ya[:, tt, c * 512:(c + 1) * 512], in0=pY[c][:, :], in1=ya[:, tt, c * 512:(c + 1) * 512], op=ALU.add),
                              reads=[B_pY[c], B_ya], writes=[B_ya])
        for tt in range(4):
            fw.dma("sp", lambda e, tt=tt, b=b: e.dma_start(out=y[b * TB + tt * 128: b * TB + (tt + 1) * 128, :], in_=ya[:, tt, :]), reads=[B_ya], writes=[B_y])
    return fw.finish([B_y])


def build_moe_sparse(cap):
    fw = FW(); nc = fw.nc
    NE = 4
    TB = 256
    NB = cap // TB
    hT = nc.dram_tensor("hT", [NE, 128, 16, cap], F32, kind="ExternalInput")
    gb = nc.dram_tensor("gb", [NE, cap], F32, kind="ExternalInput")
    w1 = nc.dram_tensor("w1", [NE, 128, 16, 512], F32, kind="ExternalInput")
    w3 = nc.dram_tensor("w3", [NE, 128, 16, 512], F32, kind="ExternalInput")
    w2 = nc.dram_tensor("w2", [NE, 128, 4, D], F32, kind="ExternalInput")
    y = nc.dram_tensor("y", [NE, cap, D], F32, kind="ExternalOutput"); B_y = Buf("y")
    def sb(shape, name):
        return fw.sbuf(shape, name=name), Buf(name)
    w1t, B_w1 = sb([128, 16, 512], "w1t"); w3t, B_w3 = sb([128, 16, 512], "w3t"); w2t, B_w2 = sb([128, 4, D], "w2t")
    hts = [sb([128, 16, TB], f"ht{i}") for i in range(2)]
    gt, B_gt = sb([128, TB], "gt")
    a1, B_a1 = sb([128, TB], "a1")
    at, B_at = sb([128, 4, TB], "at")
    yas = [sb([128, D], f"ya{i}") for i in range(2)]
    pA = [fw.psum([128, 512], name=f"pa{i}") for i in range(2)]; B_pA = [Buf("pa0"), Buf("pa1")]
    pB = [fw.psum([128, 512], name=f"pb{i}") for i in range(2)]; B_pB = [Buf("pb0"), Buf("pb1")]
    pY = [fw.psum([128, 512], name=f"py{i}") for i in range(4)]; B_pY = [Buf(f"py{i}") for i in range(4)]
    it = 0
    iy = 0
    for ex in range(NE):
        fw.dma("sp", lambda e, ex=ex: e.dma_start(out=w1t[:], in_=w1[ex, :, :, :]), writes=[B_w1])
        fw.dma("act", lambda e, ex=ex: e.dma_start(out=w3t[:], in_=w3[ex, :, :, :]), writes=[B_w3])
        fw.dma("pool", lambda e, ex=ex: e.dma_start(out=w2t[:], in_=w2[ex, :, :, :]), writes=[B_w2])
        for b in range(NB):
            bs = slice(b * TB, (b + 1) * TB)
            ht, B_ht = hts[it % 2]; it += 1
            fw.dma("sp", lambda e, ex=ex, bs=bs, ht=ht: e.dma_start(out=ht[:, 0:8, :], in_=hT[ex, :, 0:8, bs]), writes=[B_ht])
            fw.dma("act", lambda e, ex=ex, bs=bs, ht=ht: e.dma_start(out=ht[:, 8:16, :], in_=hT[ex, :, 8:16, bs]), writes=[B_ht])
            fw.dma("pool", lambda e, ex=ex, bs=bs: e.dma_start(out=gt[:], in_=gb[ex:ex + 1, bs].partition_broadcast(128)), writes=[B_gt])
            for f in range(4):
                pa, Bpa = pA[f % 2], B_pA[f % 2]
                pb, Bpb = pB[f % 2], B_pB[f % 2]
                for k in range(16):
                    fw.op("pe", lambda e, k=k, f=f, pa=pa, ht=ht: e.matmul(pa[:, 0:TB], lhsT=w1t[:, k, f * 128:(f + 1) * 128], rhs=ht[:, k, :], start=(k == 0), stop=(k == 15)),
                          reads=[B_w1, B_ht], writes=[Bpa] if k in (0, 15) else [])
                for k in range(16):
                    fw.op("pe", lambda e, k=k, f=f, pb=pb, ht=ht: e.matmul(pb[:, 0:TB], lhsT=w3t[:, k, f * 128:(f + 1) * 128], rhs=ht[:, k, :], start=(k == 0), stop=(k == 15)),
                          reads=[B_w3, B_ht], writes=[Bpb] if k in (0, 15) else [])
                fw.op("act", lambda e, pa=pa: e.activation(out=a1[:], in_=pa[:, 0:TB], func=AF.Silu), reads=[Bpa], writes=[B_a1])
                fw.op("dve", lambda e, pb=pb, f=f: e.tensor_tensor(out=at[:, f, :], in0=pb[:, 0:TB], in1=a1[:], op=ALU.mult), reads=[Bpb, B_a1], writes=[B_at])
                fw.op("pool", lambda e, f=f: e.tensor_tensor(out=at[:, f, :], in0=at[:, f, :], in1=gt[:], op=ALU.mult), reads=[B_at, B_gt], writes=[B_at])
            for tt in range(TB // 128):
                ya, B_ya = yas[iy % 2]; iy += 1
                for c in range(4):
                    for f in range(4):
                        fw.op("pe", lambda e, tt=tt, c=c, f=f: e.matmul(pY[c][:, :], lhsT=at[:, f, tt * 128:(tt + 1) * 128], rhs=w2t[:, f, c * 512:(c + 1) * 512], start=(f == 0), stop=(f == 3)),
                              reads=[B_at, B_w2], writes=[B_pY[c]] if f in (0, 3) else [])
                    if c % 2 == 0:
                        fw.op("act", lambda e, c=c, ya=ya: e.copy(out=ya[:, c * 512:(c + 1) * 512], in_=pY[c][:, :]), reads=[B_pY[c]], writes=[B_ya])
                    else:
                        fw.op("dve", lambda e, c=c, ya=ya: e.tensor_copy(out=ya[:, c * 512:(c + 1) * 512], in_=pY[c][:, :]), reads=[B_pY[c]], writes=[B_ya])
                r0 = b * TB + tt * 128
                fw.dma("sp", lambda e, ex=ex, r0=r0, ya=ya: e.dma_start(out=y[ex, r0:r0 + 128, :], in_=ya[:]), reads=[B_ya], writes=[B_y])
    return fw.finish([B_y])

T_SEQ = 2048


def build_gla():
    fw = FW(); nc = fw.nc
    T = T_SEQ
    hT = nc.dram_tensor("hT", [128, 16, T], F32, kind="ExternalInput")
    Wfm = nc.dram_tensor("Wfm", [2, 128, 16, 512], F32, kind="ExternalInput")
    Wtm = nc.dram_tensor("Wtm", [5, 128, 16, 512], F32, kind="ExternalInput")
    Wad = nc.dram_tensor("Wad", [128, 16, 16], F32, kind="ExternalInput")
    a2 = nc.dram_tensor("a2", [16, 512], F32, kind="ExternalInput")
    rowsd = nc.dram_tensor("rowsd", [2, 1024], F32, kind="ExternalInput")
    tri = nc.dram_tensor("tri", [128, 128], F32, kind="ExternalInput")
    feat = nc.dram_tensor("feat", [T, 1024], F32, kind="ExternalOutput"); B_feat = Buf("feat")
    PT = nc.dram_tensor("PT", [1024, T], F32); B_PT = Buf("PT")
    PM = nc.dram_tensor("PM", [T, 2560], F32); B_PM = Buf("PM")

    def sb(shape, name):
        return fw.sbuf(shape, name=name), Buf(name)
    hTb, B_hTb = sb([128, 16, 512], "hTb")
    wb = [sb([128, 16, 512], f"wb{i}") for i in range(2)]
    wad, B_wad = sb([128, 16, 16], "wad")
    adT, B_adT = sb([16, T], "adT")
    a2t, B_a2t = sb([16, 512], "a2t")
    abr, B_abr = sb([128, 512], "abr")
    ngr, B_ngr = sb([128, 1024], "ngr")
    trit, B_tri = sb([128, 128], "trit")
    ev = [sb([128, 512], f"ev{i}") for i in range(2)]
    pp = [(fw.psum([128, 512], name=f"pp{i}"), Buf(f"pp{i}")) for i in range(8)]
    fw.dma("pool", lambda e: e.dma_start(out=wad[:], in_=Wad[:, :, :]), writes=[B_wad])
    fw.dma("pool", lambda e: e.dma_start(out=a2t[:], in_=a2[:, :]), writes=[B_a2t])
    fw.dma("pool", lambda e: e.dma_start(out=abr[:], in_=rowsd[0:1, 0:512].partition_broadcast(128)), writes=[B_abr])
    fw.dma("pool", lambda e: e.dma_start(out=ngr[:], in_=rowsd[1:2, :].partition_broadcast(128)), writes=[B_ngr])
    fw.dma("pool", lambda e: e.dma_start(out=trit[:], in_=tri[:, :]), writes=[B_tri])
    it = 0
    for tb in range(T // 512):
        tbs = slice(tb * 512, (tb + 1) * 512)
        fw.dma("sp", lambda e, tbs=tbs: e.dma_start(out=hTb[:, 0:8, :], in_=hT[:, 0:8, tbs]), writes=[B_hTb])
        fw.dma("act", lambda e, tbs=tbs: e.dma_start(out=hTb[:, 8:16, :], in_=hT[:, 8:16, tbs]), writes=[B_hTb])
        p, Bp = pp[it % 8]; it += 1
        for kc in range(16):
            fw.op("pe", lambda e, kc=kc, p=p: e.matmul(p[0:16, :], lhsT=wad[:, kc, :], rhs=hTb[:, kc, :], start=(kc == 0), stop=(kc == 15)),
                  reads=[B_wad, B_hTb], writes=[Bp] if kc in (0, 15) else [])
        fw.op("act", lambda e, p=p, tbs=tbs: e.copy(out=adT[:, tbs], in_=p[0:16, :]), reads=[Bp], writes=[B_adT])
        for blk in range(7):
            w, Bw = wb[blk % 2]
            src = Wfm[blk] if blk < 2 else Wtm[blk - 2]
            fw.dma("sp", lambda e, w=w, src=src: e.dma_start(out=w[:, 0:8, :], in_=src[:, 0:8, :]), writes=[Bw])
            fw.dma("act", lambda e, w=w, src=src: e.dma_start(out=w[:, 8:16, :], in_=src[:, 8:16, :]), writes=[Bw])
            for sub in range(4):
                p, Bp = pp[it % 8]
                evt, Bev = ev[it % 2]
                it += 1
                if blk < 2:
                    for kc in range(16):
                        fw.op("pe", lambda e, kc=kc, p=p, w=w, sub=sub: e.matmul(p[:, :], lhsT=w[:, kc, sub * 128:(sub + 1) * 128], rhs=hTb[:, kc, :], start=(kc == 0), stop=(kc == 15)),
                              reads=[Bw, B_hTb], writes=[Bp] if kc in (0, 15) else [])
                    sc = 0.0625 if blk == 0 else 1.0
                    fw.op("act", lambda e, p=p, evt=evt, sc=sc: e.mul(out=evt[:], in_=p[:, :], mul=sc), reads=[Bp], writes=[Bev])
                    r0 = blk * 512 + sub * 128
                    fw.dma("pool", lambda e, evt=evt, r0=r0, tbs=tbs: e.dma_start(out=PT[r0:r0 + 128, tbs], in_=evt[:]), reads=[Bev], writes=[B_PT])
                else:
                    for kc in range(16):
                        fw.op("pe", lambda e, kc=kc, p=p, w=w, sub=sub: e.matmul(p[:, :], lhsT=hTb[:, kc, sub * 128:(sub + 1) * 128], rhs=w[:, kc, :], start=(kc == 0), stop=(kc == 15)),
                              reads=[Bw, B_hTb], writes=[Bp] if kc in (0, 15) else [])
                    fw.op("act", lambda e, p=p, evt=evt: e.copy(out=evt[:], in_=p[:, :]), reads=[Bp], writes=[Bev])
                    t0 = tb * 512 + sub * 128
                    c0 = (blk - 2) * 512
                    fw.dma("pool", lambda e, evt=evt, t0=t0, c0=c0: e.dma_start(out=PM[t0:t0 + 128, c0:c0 + 512], in_=evt[:]), reads=[Bev], writes=[B_PM])
    S = [[sb([128, 512], f"S{h}{c}") for c in range(2)] for h in range(2)]
    for h in range(2):
        for c in range(2):
            fw.op("pool", lambda e, t=S[h][c][0]: e.memset(t[:], 0.0), writes=[S[h][c][1]])
    qT, B_qT = sb([128, 4, 128], "qT"); kT, B_kT = sb([128, 4, 128], "kT")
    km, B_km = sb([128, 512], "km"); vm, B_vm = sb([128, 1024], "vm"); rm, B_rm = sb([128, 1024], "rm")
    la, B_la = sb([128, 512], "la")
    eL, B_eL = sb([128, 512], "eL")
    eP, B_eP = sb([128, 4, 128], "eP"); eN, B_eN = sb([128, 4, 128], "eN")
    aT, B_aT = sb([128, 128], "aT")
    o, B_o = sb([128, 512], "o")
    st, B_st = sb([128, 6], "st"); mv, B_mv = sb([128, 2], "mv"); rs, B_rs = sb([128, 1], "rs")
    fo, B_fo = sb([128, 1024], "fo")
    pz, B_pz = pp[0]; pL, B_pL = pp[1]; pLT, B_pLT = pp[2]; pS, B_pS = pp[3]; pO, B_pO = pp[4]; pU, B_pU = pp[5]
    for n in range(T // 128):
        ts_ = slice(n * 128, (n + 1) * 128)
        fw.dma("sp", lambda e, ts_=ts_: e.dma_start(out=qT[:], in_=PT[0:512, ts_].rearrange("(c p) t -> p c t", p=128)), reads=[B_PT], writes=[B_qT])
        fw.dma("sp", lambda e, ts_=ts_: e.dma_start(out=kT[:], in_=PT[512:1024, ts_].rearrange("(c p) t -> p c t", p=128)), reads=[B_PT], writes=[B_kT])
        fw.dma("act", lambda e, ts_=ts_: e.dma_start(out=km[:], in_=PM[ts_, 0:512]), reads=[B_PM], writes=[B_km])
        fw.dma("act", lambda e, ts_=ts_: e.dma_start(out=vm[:], in_=PM[ts_, 512:1536]), reads=[B_PM], writes=[B_vm])
        fw.dma("act", lambda e, ts_=ts_: e.dma_start(out=rm[:], in_=PM[ts_, 1536:2560]), reads=[B_PM], writes=[B_rm])
        fw.op("pe", lambda e, ts_=ts_: e.matmul(pz[:, :], lhsT=adT[:, ts_], rhs=a2t[:, :], start=True, stop=True), reads=[B_adT, B_a2t], writes=[B_pz])
        fw.op("dve", lambda e: e.tensor_tensor(out=la[:], in0=pz[:, :], in1=abr[:], op=ALU.add), reads=[B_pz, B_abr], writes=[B_la])
        fw.op("act", lambda e: e.activation(out=la[:], in_=la[:], func=AF.Exp, scale=-1.0), reads=[B_la], writes=[B_la])
        fw.op("dve", lambda e: e.tensor_scalar_add(out=la[:], in0=la[:], scalar1=1.0), reads=[B_la], writes=[B_la])
        fw.op("act", lambda e: e.activation(out=la[:], in_=la[:], func=AF.Ln), reads=[B_la], writes=[B_la])
        fw.op("dve", lambda e: e.tensor_scalar_mul(out=la[:], in0=la[:], scalar1=1.0 / 16.0), reads=[B_la], writes=[B_la])
        fw.op("pe", lambda e: e.matmul(pL[:, :], lhsT=trit[:, :], rhs=la[:, :], start=True, stop=True), reads=[B_tri, B_la], writes=[B_pL])
        fw.op("act", lambda e: e.activation(out=eL[:], in_=pL[:, :], func=AF.Exp), reads=[B_pL], writes=[B_eL])
        fw.op("dve", lambda e: e.tensor_tensor(out=km[:], in0=km[:], in1=eL[:], op=ALU.mult), reads=[B_km, B_eL], writes=[B_km])
        for cc in range(4):
            fw.op("pe", lambda e, cc=cc: e.matmul(pLT[:, cc * 128:(cc + 1) * 128], lhsT=la[:, cc * 128:(cc + 1) * 128], rhs=trit[:, :], start=True, stop=True),
                  reads=[B_la, B_tri], writes=[B_pLT])
        fw.op("act", lambda e: e.activation(out=eN[:].rearrange("p c t -> p (c t)"), in_=pLT[:, :], func=AF.Exp), reads=[B_pLT], writes=[B_eN])
        fw.op("act", lambda e: e.activation(out=eP[:].rearrange("p c t -> p (c t)"), in_=pLT[:, :], func=AF.Exp, scale=-1.0), reads=[B_pLT], writes=[B_eP])
        fw.op("dve", lambda e: e.tensor_tensor(out=qT[:], in0=qT[:], in1=eP[:], op=ALU.mult), reads=[B_qT, B_eP], writes=[B_qT])
        fw.op("pool", lambda e: e.tensor_tensor(out=kT[:], in0=kT[:], in1=eN[:], op=ALU.mult), reads=[B_kT, B_eN], writes=[B_kT])
        fw.op("act", lambda e: e.activation(out=rm[:], in_=rm[:], func=AF.Silu), reads=[B_rm], writes=[B_rm])
        for h in range(2):
            for c in range(2):
                cc = 2 * h + c
                fw.op("pe", lambda e, cc=cc, c=c: e.matmul(pS[:, 0:128], lhsT=kT[:, cc, :], rhs=qT[:, cc, :], start=(c == 0), stop=(c == 1)),
                      reads=[B_kT, B_qT], writes=[B_pS] if True else [])
            fw.op("dve", lambda e: e.tensor_tensor(out=aT[:], in0=pS[:, 0:128], in1=trit[:], op=ALU.mult), reads=[B_pS, B_tri], writes=[B_aT])
            vs = slice(h * 512, (h + 1) * 512)
            fw.op("pe", lambda e, vs=vs: e.matmul(pO[:, :], lhsT=aT[:, :], rhs=vm[:, vs], start=True, stop=False), reads=[B_aT, B_vm], writes=[B_pO])
            for c in range(2):
                cc = 2 * h + c
                fw.op("pe", lambda e, cc=cc, c=c, h=h: e.matmul(pO[:, :], lhsT=qT[:, cc, :], rhs=S[h][c][0][:, :], start=False, stop=(c == 1)),
                      reads=[B_qT, S[h][c][1]], writes=[B_pO])
            fw.op("act", lambda e: e.copy(out=o[:], in_=pO[:, :]), reads=[B_pO], writes=[B_o])
            for c in range(2):
                cc = 2 * h + c
                St, BS = S[h][c]
                fw.op("pe", lambda e, cc=cc, vs=vs: e.matmul(pU[:, :], lhsT=km[:, cc * 128:(cc + 1) * 128], rhs=vm[:, vs], start=True, stop=True), reads=[B_km, B_vm], writes=[B_pU])
                fw.op("dve", lambda e, St=St: e.tensor_tensor(out=St[:], in0=pU[:, :], in1=St[:], op=ALU.add), reads=[B_pU, BS], writes=[BS])
                fw.op("dve", lambda e, St=St, cc=cc: e.tensor_scalar(out=St[:], in0=St[:], scalar1=eP[:, cc, 127:128], scalar2=None, op0=ALU.mult), reads=[BS, B_eP], writes=[BS])
            fw.op("dve", lambda e: e.bn_stats(out=st[:], in_=o[:]), reads=[B_o], writes=[B_st])
            fw.op("dve", lambda e: e.bn_aggr(out=mv[:], in_=st[:]), reads=[B_st], writes=[B_mv])
            fw.op("dve", lambda e: e.tensor_tensor(out=rs[:], in0=mv[:, 0:1], in1=mv[:, 0:1], op=ALU.mult), reads=[B_mv], writes=[B_rs])
            fw.op("dve", lambda e: e.tensor_tensor(out=rs[:], in0=rs[:], in1=mv[:, 1:2], op=ALU.add), reads=[B_rs, B_mv], writes=[B_rs])
            fw.op("dve", lambda e: e.tensor_scalar_add(out=rs[:], in0=rs[:], scalar1=1e-5), reads=[B_rs], writes=[B_rs])
            fw.op("act", lambda e: e.activation(out=rs[:], in_=rs[:], func=AF.Sqrt), reads=[B_rs], writes=[B_rs])
            fw.op("dve", lambda e: e.reciprocal(out=rs[:], in_=rs[:]), reads=[B_rs], writes=[B_rs])
            fw.op("dve", lambda e, vs=vs: e.scalar_tensor_tensor(out=fo[:, vs], in0=o[:], scalar=rs[:, 0:1], in1=ngr[:, vs], op0=ALU.mult, op1=ALU.mult),
                  reads=[B_o, B_rs, B_ngr], writes=[B_fo])
            fw.op("pool", lambda e, vs=vs: e.tensor_tensor(out=fo[:, vs], in0=fo[:, vs], in1=rm[:, vs], op=ALU.mult), reads=[B_fo, B_rm], writes=[B_fo])
        fw.dma("sp", lambda e, ts_=ts_: e.dma_start(out=feat[ts_, :], in_=fo[:]), reads=[B_fo], writes=[B_feat])
    return fw.finish([B_feat])


def gla_inputs(hT_b, g, win, a2, ab, norm_g):
    f = np.float32
    win = np.asarray(win, f)
    q = win[:, 0:1024][:, g * 512:(g + 1) * 512]
    k = win[:, 1024:2048][:, g * 512:(g + 1) * 512]
    v = win[:, 2048:4096][:, g * 1024:(g + 1) * 1024]
    r = win[:, 4096:6144][:, g * 1024:(g + 1) * 1024]
    ad = win[:, 6144:6160]
    Wfm = np.stack([_kmajor(q), _kmajor(k)])
    Wtm = np.stack([_kmajor(k), _kmajor(v[:, :512]), _kmajor(v[:, 512:]), _kmajor(r[:, :512]), _kmajor(r[:, 512:])])
    rowsd = np.zeros((2, 1024), f)
    rowsd[0, :512] = np.asarray(ab, f)[g * 512:(g + 1) * 512]
    rowsd[1] = np.asarray(norm_g, f)[g * 1024:(g + 1) * 1024]
    tri = np.triu(np.ones((128, 128), f))
    return {"hT": hT_b, "Wfm": Wfm, "Wtm": Wtm, "Wad": _kmajor(ad), "a2": np.ascontiguousarray(np.asarray(a2, f)[:, g * 512:(g + 1) * 512]),
            "rowsd": rowsd, "tri": tri}


import math
TWO_PI = 2.0 * math.pi
RW_C = 64
W_SCALE = -math.exp(-0.5)


def build_even(layer2, do_ret=True, do_rw=True):
    fw = FW(); nc = fw.nc
    T = T_SEQ
    NFM = 5 if layer2 else 3
    NTM = 5
    hT = nc.dram_tensor("hT", [128, 16, T], F32, kind="ExternalInput")
    Wfm = nc.dram_tensor("Wfm", [NFM, 128, 16, 512], F32, kind="ExternalInput")
    Wtm = nc.dram_tensor("Wtm", [NTM, 128, 16, 512], F32, kind="ExternalInput")
    mufm = nc.dram_tensor("mufm", [NFM, 512], F32, kind="ExternalInput")
    mutm = nc.dram_tensor("mutm", [NTM, 512], F32, kind="ExternalInput")
    posi = nc.dram_tensor("posi", [1, T], I32, kind="ExternalInput")
    cst = nc.dram_tensor("cst", [128, 8, 128], F32, kind="ExternalInput")
    cst2 = nc.dram_tensor("cst2", [128, 8], F32, kind="ExternalInput")
    rowsd = nc.dram_tensor("rowsd", [12, 512], F32, kind="ExternalInput")
    lw2 = nc.dram_tensor("lw2", [64, 512], F32, kind="ExternalInput")
    la2 = nc.dram_tensor("la2", [64, 512], F32, kind="ExternalInput")
    lg2 = nc.dram_tensor("lg2", [160, 512], F32, kind="ExternalInput")
    if layer2:
        lv1 = nc.dram_tensor("lv1", [128, 8, 32], F32, kind="ExternalInput")
        lv2 = nc.dram_tensor("lv2", [32, 512], F32, kind="ExternalInput")
        vfirst = nc.dram_tensor("vfirst", [T, 512], F32, kind="ExternalInput")
    feat = nc.dram_tensor("feat", [T, 1024], F32, kind="ExternalOutput"); B_feat = Buf("feat")
    finals = [B_feat]
    if not layer2:
        vout = nc.dram_tensor("vout", [T, 512], F32, kind="ExternalOutput"); B_vout = Buf("vout")
        finals.append(B_vout)
    PT = nc.dram_tensor("PT", [NFM * 512, T], F32); B_PT = Buf("PT")
    PM = nc.dram_tensor("PM", [T, NTM * 512], F32); B_PM = Buf("PM")

    ARENA = 50000
    arena = fw.sbuf([128, ARENA], name="arena")
    off = [0]
    def sb(shape, name, dtype=F32):
        n = 1
        for d_ in shape[1:]:
            n *= d_
        assert off[0] + n <= ARENA, (name, off[0], n)
        ap = arena[0:shape[0], off[0]:off[0] + n]
        if len(shape) == 3:
            ap = ap.rearrange("p (a b) -> p a b", a=shape[1])
        if dtype is not F32:
            ap = ap.bitcast(dtype)
        off[0] += n
        return ap, Buf(name)
    def psb(shape, name, dtype=F32):
        return fw.sbuf(shape, name=name, dtype=dtype), Buf(name)
    TBK = 256
    hTb, B_hTb = sb([128, 16, TBK], "hTb")
    hTp, B_hTp = sb([128, 16, TBK], "hTp")
    w1, B_w1 = sb([128, 16, 512], "w1")
    w2, B_w2 = sb([128, 16, 512], "w2")
    mur, B_mur = sb([128, 512], "mur")
    cs, B_cs = psb([128, 8, 128], "cs")
    cs2, B_cs2 = psb([128, 8], "cs2")
    ev = [sb([128, 512], f"ev{i}") for i in range(2)]
    pp = [(fw.psum([128, 512], name=f"pp{i}"), Buf(f"pp{i}")) for i in range(8)]
    fw.dma("pool", lambda e: e.dma_start(out=cs[:], in_=cst[:, :, :]), writes=[B_cs])
    fw.dma("pool", lambda e: e.dma_start(out=cs2[:], in_=cst2[:, :]), writes=[B_cs2])
    TRI, IDN, GP0, GN0, GP1, GN1, TBD, SBD = range(8)
    it = 0
    blocks = [("fm", i) for i in range(NFM)] + [("tm", i) for i in range(NTM)]
    def mixed(kind, i):
        return (kind == "fm" and i >= 2) or (kind == "tm" and i >= 2)
    for tb in range(T // TBK):
        t0 = tb * TBK
        fw.dma("sp", lambda e, t0=t0: e.dma_start(out=hTb[:, 0:8, :], in_=hT[:, 0:8, t0:t0 + TBK]), writes=[B_hTb])
        fw.dma("act", lambda e, t0=t0: e.dma_start(out=hTb[:, 8:16, :], in_=hT[:, 8:16, t0:t0 + TBK]), writes=[B_hTb])
        if tb == 0:
            fw.op("pool", lambda e: e.memset(hTp[:, :, 0:1], 0.0), writes=[B_hTp])
            fw.dma("sp", lambda e: e.dma_start(out=hTp[:, :, 1:TBK], in_=hT[:, :, 0:TBK - 1]), writes=[B_hTp])
        else:
            fw.dma("sp", lambda e, t0=t0: e.dma_start(out=hTp[:, 0:8, :], in_=hT[:, 0:8, t0 - 1:t0 + TBK - 1]), writes=[B_hTp])
            fw.dma("act", lambda e, t0=t0: e.dma_start(out=hTp[:, 8:16, :], in_=hT[:, 8:16, t0 - 1:t0 + TBK - 1]), writes=[B_hTp])
        for (kind, bi) in blocks:
            src = Wfm[bi] if kind == "fm" else Wtm[bi]
            mx = mixed(kind, bi)
            fw.dma("sp", lambda e, src=src: e.dma_start(out=w1[:, 0:8, :], in_=src[:, 0:8, :]), writes=[B_w1])
            fw.dma("act", lambda e, src=src: e.dma_start(out=w1[:, 8:16, :], in_=src[:, 8:16, :]), writes=[B_w1])
            if mx:
                mus = mufm if kind == "fm" else mutm
                fw.dma("pool", lambda e, mus=mus, bi=bi: e.dma_start(out=mur[:], in_=mus[bi:bi + 1, :].partition_broadcast(128)), writes=[B_mur])
                fw.op("dve", lambda e: e.tensor_tensor(out=w2[:], in0=w1[:], in1=mur[:].unsqueeze(1).to_broadcast([128, 16, 512]), op=ALU.mult), reads=[B_w1, B_mur], writes=[B_w2])
                fw.op("pool", lambda e: e.tensor_tensor(out=w1[:], in0=w1[:], in1=w2[:], op=ALU.subtract), reads=[B_w1, B_w2], writes=[B_w1])
            nsub = 4 if kind == "fm" else TBK // 128
            if kind == "fm" and bi == 2:
                nsub = 3
            for sub in range(nsub):
                p, Bp = pp[it % 8]
                evt, Bev = ev[it % 2]
                it += 1
                groups = [(w1, B_w1, hTb, B_hTb)] + ([(w2, B_w2, hTp, B_hTp)] if mx else [])
                nmm = 16 * len(groups)
                j = 0
                for (w, Bw, hh, Bh) in groups:
                    for kc in range(16):
                        first, last = (j == 0), (j == nmm - 1)
                        if kind == "fm":
                            fw.op("pe", lambda e, kc=kc, p=p, w=w, hh=hh, sub=sub, first=first, last=last: e.matmul(p[:, 0:TBK], lhsT=w[:, kc, sub * 128:(sub + 1) * 128], rhs=hh[:, kc, :], start=first, stop=last),
                                  reads=[Bw, Bh], writes=[Bp] if (first or last) else [])
                        else:
                            fw.op("pe", lambda e, kc=kc, p=p, w=w, hh=hh, sub=sub, first=first, last=last: e.matmul(p[:, :], lhsT=hh[:, kc, sub * 128:(sub + 1) * 128], rhs=w[:, kc, :], start=first, stop=last),
                                  reads=[Bw, Bh], writes=[Bp] if (first or last) else [])
                        j += 1
                if kind == "fm":
                    fw.op("act", lambda e, p=p, evt=evt: e.copy(out=evt[:, 0:TBK], in_=p[:, 0:TBK]), reads=[Bp], writes=[Bev])
                    r0 = bi * 512 + sub * 128
                    fw.dma("pool", lambda e, evt=evt, r0=r0, t0=t0: e.dma_start(out=PT[r0:r0 + 128, t0:t0 + TBK], in_=evt[:, 0:TBK]), reads=[Bev], writes=[B_PT])
                else:
                    fw.op("act", lambda e, p=p, evt=evt: e.copy(out=evt[:], in_=p[:, :]), reads=[Bp], writes=[Bev])
                    tt = t0 + sub * 128
                    fw.dma("pool", lambda e, evt=evt, tt=tt, bi=bi: e.dma_start(out=PM[tt:tt + 128, bi * 512:(bi + 1) * 512], in_=evt[:]), reads=[Bev], writes=[B_PM])
    fw.barrier()
    off[0] = 0
    fo, B_fo = sb([128, 1024], "fo")
    rw_, B_rw = sb([128, 12, 512], "rowsb")
    for j in range(10 if layer2 else 9):
        fw.dma("pool", lambda e, j=j: e.dma_start(out=rw_[:, j, :], in_=rowsd[j:j + 1, :].partition_broadcast(128)), writes=[B_rw])
    st, B_st = sb([128, 6], "st"); mv, B_mv = sb([128, 2], "mv"); rs, B_rs = sb([128, 1], "rs")
    if do_ret:
        pi_, B_pi = sb([128, T], "posint", I32)
        ang, B_ang = sb([128, T], "ang")
        cosT, B_cos = sb([128, T], "cosT"); sinT, B_sin = sb([128, T], "sinT")
        fw.dma("sp", lambda e: e.dma_start(out=pi_[:], in_=posi[0:1, :].partition_broadcast(128)), writes=[B_pi])
        fw.op("dve", lambda e: e.tensor_copy(out=ang[:], in_=pi_[:]), reads=[B_pi], writes=[B_ang])
        fw.op("dve", lambda e: e.tensor_scalar(out=ang[:], in0=ang[:], scalar1=cs2[:, 0:1], scalar2=None, op0=ALU.mult), reads=[B_ang, B_cs2], writes=[B_ang])
        ki, B_ki = sb([128, T], "ki", I32)
        C1, C2 = 6.28125, TWO_PI - 6.28125
        fw.op("dve", lambda e: e.tensor_scalar(out=sinT[:], in0=ang[:], scalar1=1.0 / TWO_PI, scalar2=0.5, op0=ALU.mult, op1=ALU.add), reads=[B_ang], writes=[B_sin])
        fw.op("dve", lambda e: e.tensor_copy(out=ki[:], in_=sinT[:]), reads=[B_sin], writes=[B_ki])
        fw.op("dve", lambda e: e.tensor_copy(out=cosT[:], in_=ki[:]), reads=[B_ki], writes=[B_cos])
        fw.op("dve", lambda e: e.scalar_tensor_tensor(out=sinT[:], in0=cosT[:], scalar=-C1, in1=ang[:], op0=ALU.mult, op1=ALU.add), reads=[B_cos, B_ang], writes=[B_sin])
        fw.op("dve", lambda e: e.scalar_tensor_tensor(out=sinT[:], in0=cosT[:], scalar=-C2, in1=sinT[:], op0=ALU.mult, op1=ALU.add), reads=[B_cos, B_sin], writes=[B_sin])
        def wrap(tt, Bt, tmp, Btmp):
            fw.op("dve", lambda e: e.tensor_single_scalar(out=tmp[:], in_=tt[:], scalar=math.pi, op=ALU.is_gt), reads=[Bt], writes=[Btmp])
            fw.op("dve", lambda e: e.scalar_tensor_tensor(out=tt[:], in0=tmp[:], scalar=-TWO_PI, in1=tt[:], op0=ALU.mult, op1=ALU.add), reads=[Btmp, Bt], writes=[Bt])
            fw.op("dve", lambda e: e.tensor_single_scalar(out=tmp[:], in_=tt[:], scalar=-math.pi, op=ALU.is_lt), reads=[Bt], writes=[Btmp])
            fw.op("dve", lambda e: e.scalar_tensor_tensor(out=tt[:], in0=tmp[:], scalar=TWO_PI, in1=tt[:], op0=ALU.mult, op1=ALU.add), reads=[Btmp, Bt], writes=[Bt])
        wrap(sinT, B_sin, ang, B_ang)
        fw.op("dve", lambda e: e.tensor_scalar_add(out=cosT[:], in0=sinT[:], scalar1=0.5 * math.pi), reads=[B_sin], writes=[B_cos])
        wrap(cosT, B_cos, ang, B_ang)
        fw.op("act", lambda e: e.activation(out=sinT[:], in_=sinT[:], func=AF.Sin), reads=[B_sin], writes=[B_sin])
        fw.op("act", lambda e: e.activation(out=cosT[:], in_=cosT[:], func=AF.Sin), reads=[B_cos], writes=[B_cos])
        S = [[sb([128, 256], f"S{h}{c}") for c in range(2)] for h in range(2)]
        for h in range(2):
            for c in range(2):
                fw.op("pool", lambda e, t=S[h][c][0]: e.memset(t[:], 0.0), writes=[S[h][c][1]])
        qT, B_qT = sb([128, 4, 128], "qT"); kT, B_kT = sb([128, 4, 128], "kT")
        qR, B_qR = sb([128, 4, 128], "qR"); kR, B_kR = sb([128, 4, 128], "kR")
        tA, B_tA = sb([128, 128], "tA"); tB, B_tB = sb([128, 128], "tB")
        km, B_km = sb([128, 512], "km"); vm, B_vm = sb([128, 512], "vm"); gm, B_gm = sb([128, 512], "gm")
        aT, B_aT = sb([128, 128], "aT"); o, B_o = sb([128, 256], "o")
        pS, B_pS = pp[0]; pO, B_pO = pp[1]; pU, B_pU = pp[2]; pK, B_pK = pp[3]
        for n in range(T // 128):
            ts_ = slice(n * 128, (n + 1) * 128)
            fw.dma("sp", lambda e, ts_=ts_: e.dma_start(out=qT[:], in_=PT[0:512, ts_].rearrange("(c p) t -> p c t", p=128)), reads=[B_PT], writes=[B_qT])
            fw.dma("sp", lambda e, ts_=ts_: e.dma_start(out=kT[:], in_=PT[512:1024, ts_].rearrange("(c p) t -> p c t", p=128)), reads=[B_PT], writes=[B_kT])
            fw.dma("act", lambda e, ts_=ts_: e.dma_start(out=vm[:], in_=PM[ts_, 0:512]), reads=[B_PM], writes=[B_vm])
            fw.dma("act", lambda e, ts_=ts_: e.dma_start(out=gm[:], in_=PM[ts_, 512:1024]), reads=[B_PM], writes=[B_gm])
            fw.op("act", lambda e: e.activation(out=gm[:], in_=gm[:], func=AF.Silu), reads=[B_gm], writes=[B_gm])
            for h in range(2):
                for (src, Bsrc, dst, Bdst, tab) in ((qT, B_qT, qR, B_qR, GP0 if h == 0 else GP1), (kT, B_kT, kR, B_kR, GN0 if h == 0 else GN1)):
                    c1, c2 = 2 * h, 2 * h + 1
                    eng1, eng2 = ("dve", "pool") if src is qT else ("pool", "dve")
                    fw.op(eng1, lambda e, src=src, c1=c1, ts_=ts_: e.tensor_tensor(out=tA[:], in0=src[:, c1, :], in1=cosT[:, ts_], op=ALU.mult), reads=[Bsrc, B_cos], writes=[B_tA])
                    fw.op(eng1, lambda e, src=src, c2=c2, ts_=ts_: e.tensor_tensor(out=tB[:], in0=src[:, c2, :], in1=sinT[:, ts_], op=ALU.mult), reads=[Bsrc, B_sin], writes=[B_tB])
                    fw.op(eng1, lambda e: e.tensor_tensor(out=tA[:], in0=tA[:], in1=tB[:], op=ALU.subtract), reads=[B_tA, B_tB], writes=[B_tA])
                    fw.op(eng1, lambda e, dst=dst, c1=c1, tab=tab: e.tensor_tensor(out=dst[:, c1, :], in0=tA[:], in1=cs[:, tab, :], op=ALU.mult), reads=[B_tA, B_cs], writes=[Bdst])
                    fw.op(eng1, lambda e, src=src, c1=c1, ts_=ts_: e.tensor_tensor(out=tA[:], in0=src[:, c1, :], in1=sinT[:, ts_], op=ALU.mult), reads=[Bsrc, B_sin], writes=[B_tA])
                    fw.op(eng1, lambda e, src=src, c2=c2, ts_=ts_: e.tensor_tensor(out=tB[:], in0=src[:, c2, :], in1=cosT[:, ts_], op=ALU.mult), reads=[Bsrc, B_cos], writes=[B_tB])
                    fw.op(eng1, lambda e: e.tensor_tensor(out=tA[:], in0=tA[:], in1=tB[:], op=ALU.add), reads=[B_tA, B_tB], writes=[B_tA])
                    fw.op(eng1, lambda e, dst=dst, c2=c2, tab=tab: e.tensor_tensor(out=dst[:, c2, :], in0=tA[:], in1=cs[:, tab, :], op=ALU.mult), reads=[B_tA, B_cs], writes=[Bdst])
            for cc in range(4):
                fw.op("pe", lambda e, cc=cc: e.transpose(out=pK[:, cc * 128:(cc + 1) * 128], in_=kR[:, cc, :], identity=cs[:, IDN, :]), reads=[B_kR, B_cs], writes=[B_pK])
            fw.op("act", lambda e: e.copy(out=km[:], in_=pK[:, :]), reads=[B_pK], writes=[B_km])
            for h in range(2):
                for c in range(2):
                    cc = 2 * h + c
                    fw.op("pe", lambda e, cc=cc, c=c: e.matmul(pS[:, 0:128], lhsT=kR[:, cc, :], rhs=qR[:, cc, :], start=(c == 0), stop=(c == 1)), reads=[B_kR, B_qR], writes=[B_pS])
                fw.op("dve", lambda e: e.tensor_tensor(out=aT[:], in0=pS[:, 0:128], in1=cs[:, TRI, :], op=ALU.mult), reads=[B_pS, B_cs], writes=[B_aT])
                vs = slice(h * 256, (h + 1) * 256)
                fw.op("pe", lambda e, vs=vs: e.matmul(pO[:, 0:256], lhsT=aT[:, :], rhs=vm[:, vs], start=True, stop=False), reads=[B_aT, B_vm], writes=[B_pO])
                for c in range(2):
                    cc = 2 * h + c
                    fw.op("pe", lambda e, cc=cc, c=c, h=h: e.matmul(pO[:, 0:256], lhsT=qR[:, cc, :], rhs=S[h][c][0][:, :], start=False, stop=(c == 1)), reads=[B_qR, S[h][c][1]], writes=[B_pO])
                fw.op("act", lambda e: e.copy(out=o[:], in_=pO[:, 0:256]), reads=[B_pO], writes=[B_o])
                for c in range(2):
                    cc = 2 * h + c
                    St, BS = S[h][c]
                    fw.op("pe", lambda e, cc=cc, vs=vs: e.matmul(pU[:, 0:256], lhsT=km[:, cc * 128:(cc + 1) * 128], rhs=vm[:, vs], start=True, stop=True), reads=[B_km, B_vm], writes=[B_pU])
                    fw.op("dve", lambda e, St=St: e.tensor_tensor(out=St[:], in0=pU[:, 0:256], in1=St[:], op=ALU.add), reads=[B_pU, BS], writes=[BS])
                    fw.op("dve", lambda e, St=St, h=h: e.tensor_scalar(out=St[:], in0=St[:], scalar1=cs2[:, 1 + h:2 + h], scalar2=None, op0=ALU.mult), reads=[BS, B_cs2], writes=[BS])
                fw.op("dve", lambda e: e.bn_stats(out=st[:], in_=o[:]), reads=[B_o], writes=[B_st])
                fw.op("dve", lambda e: e.bn_aggr(out=mv[:], in_=st[:]), reads=[B_st], writes=[B_mv])
                fw.op("dve", lambda e: e.tensor_scalar_add(out=rs[:], in0=mv[:, 1:2], scalar1=1e-5), reads=[B_mv], writes=[B_rs])
                fw.op("act", lambda e: e.activation(out=rs[:], in_=rs[:], func=AF.Sqrt), reads=[B_rs], writes=[B_rs])
                fw.op("dve", lambda e: e.reciprocal(out=rs[:], in_=rs[:]), reads=[B_rs], writes=[B_rs])
                fw.op("dve", lambda e: e.tensor_scalar(out=o[:], in0=o[:], scalar1=mv[:, 0:1], scalar2=rs[:, 0:1], op0=ALU.subtract, op1=ALU.mult), reads=[B_o, B_mv, B_rs], writes=[B_o])
                fw.op("pool", lambda e, vs=vs: e.tensor_tensor(out=o[:], in0=o[:], in1=rw_[:, 0, vs], op=ALU.mult), reads=[B_o, B_rw], writes=[B_o])
                fw.op("pool", lambda e, vs=vs: e.tensor_tensor(out=o[:], in0=o[:], in1=rw_[:, 1, vs], op=ALU.add), reads=[B_o, B_rw], writes=[B_o])
                fw.op("dve", lambda e, vs=vs: e.tensor_tensor(out=fo[:, vs], in0=o[:], in1=gm[:, vs], op=ALU.mult), reads=[B_o, B_gm], writes=[B_fo])
            fw.dma("sp", lambda e, ts_=ts_: e.dma_start(out=feat[ts_, 0:512], in_=fo[:, 0:512]), reads=[B_fo], writes=[B_feat])
    if do_rw:
        C = RW_C
        SLO, SUP = 6, 7
        lw2t, B_lw2 = sb([64, 512], "lw2t"); la2t, B_la2 = sb([64, 512], "la2t")
        lg2a, B_lg2a = sb([128, 512], "lg2a"); lg2b, B_lg2b = sb([32, 512], "lg2b")
        fw.dma("pool", lambda e: e.dma_start(out=lw2t[:], in_=lw2[:, :]), writes=[B_lw2])
        fw.dma("pool", lambda e: e.dma_start(out=la2t[:], in_=la2[:, :]), writes=[B_la2])
        fw.dma("pool", lambda e: e.dma_start(out=lg2a[:], in_=lg2[0:128, :]), writes=[B_lg2a])
        fw.dma("pool", lambda e: e.dma_start(out=lg2b[:], in_=lg2[128:160, :]), writes=[B_lg2b])
        if layer2:
            lv1t, B_lv1 = sb([128, 8, 32], "lv1t"); lv2t, B_lv2 = sb([32, 512], "lv2t")
            fw.dma("pool", lambda e: e.dma_start(out=lv1t[:], in_=lv1[:, :, :]), writes=[B_lv1])
            fw.dma("pool", lambda e: e.dma_start(out=lv2t[:], in_=lv2[:, :]), writes=[B_lv2])
        H = [sb([64, 64], f"H{hh}") for hh in range(8)]
        for hh in range(8):
            fw.op("pool", lambda e, t=H[hh][0]: e.memset(t[:], 0.0), writes=[H[hh][1]])
        def t64(name, w=512):
            return sb([64, w], name)
        rt, B_rt = t64("rt"); kt, B_kt = t64("kt"); vt, B_vt = t64("vt")
        wdT, B_wdT = sb([64, 64], "wdT"); adT, B_adT = sb([64, 64], "adT"); gda, B_gda = sb([128, 64], "gda"); gdb, B_gdb = sb([32, 64], "gdb")
        lw, B_lw = t64("lw"); asg, B_asg = t64("asg"); gate, B_gate = t64("gate")
        kkx, B_kkx = t64("kkx"); sq, B_sq = t64("sq"); ss, B_ss = sb([64, 8], "ss")
        kmod, B_kmod = t64("kmod"); bv, B_bv = t64("bv"); tmp, B_tmp = t64("tmp")
        eLp, B_eLp = t64("eLp"); eLn, B_eLn = t64("eLn"); eLx, B_eLx = t64("eLx")
        Rt, B_Rt = t64("Rt"); Kt, B_Kt = t64("Kt"); Bt, B_Bt = t64("Bt"); At, B_At = t64("At")
        RT, B_RT = sb([64, 8, 64], "RTT"); KT, B_KT = sb([64, 8, 64], "KTT"); BT, B_BT = sb([64, 8, 64], "BTT"); AT, B_AT = sb([64, 8, 64], "ATT")
        dec, B_dec = sb([64, 512], "dec")
        ones64, B_ones = sb([64, 64], "ones64")
        fw.op("pool", lambda e: e.memset(ones64[:], 1.0), writes=[B_ones])
        bon, B_bon = t64("bon"); rks, B_rks = sb([64, 8], "rks")
        yt, B_yt = t64("yt")
        if layer2:
            vTa, B_vTa = sb([128, 8, 64], "vTa"); vf, B_vf = t64("vf"); sT, B_sT = sb([32, 64], "sT"); sg, B_sg = t64("sg")
        def ph(name):
            return [sb([64, 64], f"{name}{hh}") for hh in range(8)]
        Na, NTa, Nb, NTb, Pm, Pak, Prb, Prk, R0, U = [ph(n) for n in ("Na", "NTa", "Nb", "NTb", "Pm", "Pak", "Prb", "Prk", "R0", "U")]
        def regs(i):
            b_ = Buf(f"pp{i}bank")
            return [b_] * 8
        PB = [regs(i) for i in range(8)]
        def pall(i):
            return PB[i]
        hsl = lambda hh: slice(hh * 64, (hh + 1) * 64)
        for j in range(T // C):
            t0 = j * C
            tsl = slice(t0, t0 + C)
            fw.dma("sp", lambda e, tsl=tsl: e.dma_start(out=rt[:], in_=PM[tsl, 1024:1536]), reads=[B_PM], writes=[B_rt])
            fw.dma("act", lambda e, tsl=tsl: e.dma_start(out=kt[:], in_=PM[tsl, 1536:2048]), reads=[B_PM], writes=[B_kt])
            fw.dma("sp", lambda e, tsl=tsl: e.dma_start(out=vt[:], in_=PM[tsl, 2048:2560]), reads=[B_PM], writes=[B_vt])
            fw.dma("act", lambda e, tsl=tsl: e.dma_start(out=wdT[:], in_=PT[1024:1088, tsl]), reads=[B_PT], writes=[B_wdT])
            fw.dma("act", lambda e, tsl=tsl: e.dma_start(out=adT[:], in_=PT[1088:1152, tsl]), reads=[B_PT], writes=[B_adT])
            fw.dma("sp", lambda e, tsl=tsl: e.dma_start(out=gda[:], in_=PT[1152:1280, tsl]), reads=[B_PT], writes=[B_gda])
            fw.dma("sp", lambda e, tsl=tsl: e.dma_start(out=gdb[:], in_=PT[1280:1312, tsl]), reads=[B_PT], writes=[B_gdb])
            if not layer2:
                fw.dma("pool", lambda e, tsl=tsl: e.dma_start(out=vout[tsl, :], in_=vt[:]), reads=[B_vt], writes=[B_vout])
            p0, p1, p2, p3, p4, p5, p6, p7 = [pp[i][0] for i in range(8)]
            fw.op("act", lambda e: e.activation(out=wdT[:], in_=wdT[:], func=AF.Tanh), reads=[B_wdT], writes=[B_wdT])
            fw.op("pe", lambda e: e.matmul(p0[0:64, :], lhsT=wdT[:, :], rhs=lw2t[:, :], start=True, stop=True), reads=[B_wdT, B_lw2], writes=pall(0))
            fw.op("dve", lambda e: e.tensor_tensor(out=lw[:], in0=p0[0:64, :], in1=rw_[0:64, 2, :], op=ALU.add), reads=pall(0) + [B_rw], writes=[B_lw])
            fw.op("act", lambda e: e.activation(out=lw[:], in_=lw[:], func=AF.Sigmoid), reads=[B_lw], writes=[B_lw])
            fw.op("dve", lambda e: e.tensor_scalar_mul(out=lw[:], in0=lw[:], scalar1=W_SCALE), reads=[B_lw], writes=[B_lw])
            fw.op("pe", lambda e: e.matmul(p1[0:64, :], lhsT=adT[:, :], rhs=la2t[:, :], start=True, stop=True), reads=[B_adT, B_la2], writes=pall(1))
            fw.op("dve", lambda e: e.tensor_tensor(out=asg[:], in0=p1[0:64, :], in1=rw_[0:64, 3, :], op=ALU.add), reads=pall(1) + [B_rw], writes=[B_asg])
            fw.op("act", lambda e: e.activation(out=asg[:], in_=asg[:], func=AF.Sigmoid), reads=[B_asg], writes=[B_asg])
            fw.op("act", lambda e: e.activation(out=gda[:], in_=gda[:], func=AF.Sigmoid), reads=[B_gda], writes=[B_gda])
            fw.op("act", lambda e: e.activation(out=gdb[:], in_=gdb[:], func=AF.Sigmoid), reads=[B_gdb], writes=[B_gdb])
            fw.op("pe", lambda e: e.matmul(p2[0:64, :], lhsT=gda[:, :], rhs=lg2a[:, :], start=True, stop=False), reads=[B_gda, B_lg2a], writes=pall(2))
            fw.op("pe", lambda e: e.matmul(p2[0:64, :], lhsT=gdb[:, :], rhs=lg2b[:, :], start=False, stop=True), reads=[B_gdb, B_lg2b], writes=pall(2))
            fw.op("act", lambda e: e.copy(out=gate[:], in_=p2[0:64, :]), reads=pall(2), writes=[B_gate])
            if layer2:
                fw.dma("sp", lambda e, tsl=tsl: e.dma_start(out=vTa[:], in_=PT[1536:2560, tsl].rearrange("(c p) t -> p c t", p=128)), reads=[B_PT], writes=[B_vTa])
                fw.dma("act", lambda e, tsl=tsl: e.dma_start(out=vf[:], in_=vfirst[tsl, :]), writes=[B_vf])
                for cc in range(8):
                    fw.op("pe", lambda e, cc=cc: e.matmul(p3[0:32, 0:64], lhsT=lv1t[:, cc, :], rhs=vTa[:, cc, :], start=(cc == 0), stop=(cc == 7)), reads=[B_lv1, B_vTa], writes=pall(3) if cc in (0, 7) else [])
                fw.op("act", lambda e: e.copy(out=sT[:], in_=p3[0:32, 0:64]), reads=pall(3), writes=[B_sT])
                fw.op("pe", lambda e: e.matmul(p3[0:64, :], lhsT=sT[:, :], rhs=lv2t[:, :], start=True, stop=True), reads=[B_sT, B_lv2], writes=pall(3))
                fw.op("dve", lambda e: e.tensor_tensor(out=sg[:], in0=p3[0:64, :], in1=rw_[0:64, 9, :], op=ALU.add), reads=pall(3) + [B_rw], writes=[B_sg])
                fw.op("act", lambda e: e.activation(out=sg[:], in_=sg[:], func=AF.Sigmoid), reads=[B_sg], writes=[B_sg])
                fw.op("dve", lambda e: e.tensor_tensor(out=vf[:], in0=vf[:], in1=vt[:], op=ALU.subtract), reads=[B_vf, B_vt], writes=[B_vf])
                fw.op("dve", lambda e: e.tensor_tensor(out=vf[:], in0=vf[:], in1=sg[:], op=ALU.mult), reads=[B_vf, B_sg], writes=[B_vf])
                fw.op("dve", lambda e: e.tensor_tensor(out=vt[:], in0=vt[:], in1=vf[:], op=ALU.add), reads=[B_vf, B_vt], writes=[B_vt])
            fw.op("pool", lambda e: e.tensor_tensor(out=kkx[:], in0=kt[:], in1=rw_[0:64, 4, :], op=ALU.mult), reads=[B_kt, B_rw], writes=[B_kkx])
            fw.op("pool", lambda e: e.tensor_tensor(out=sq[:], in0=kkx[:], in1=kkx[:], op=ALU.mult), reads=[B_kkx], writes=[B_sq])
            fw.op("dve", lambda e: e.tensor_reduce(out=ss[:], in_=sq[:].rearrange("p (h d) -> p h d", h=8), axis=AX.X, op=ALU.add), reads=[B_sq], writes=[B_ss])
            fw.op("act", lambda e: e.activation(out=ss[:], in_=ss[:], func=AF.Sqrt), reads=[B_ss], writes=[B_ss])
            fw.op("dve", lambda e: e.tensor_scalar_max(out=ss[:], in0=ss[:], scalar1=1e-12), reads=[B_ss], writes=[B_ss])
            fw.op("dve", lambda e: e.reciprocal(out=ss[:], in_=ss[:]), reads=[B_ss], writes=[B_ss])
            fw.op("dve", lambda e: e.tensor_tensor(out=kkx[:].rearrange("p (h d) -> p h d", h=8), in0=kkx[:].rearrange("p (h d) -> p h d", h=8),
                                                   in1=ss[:].unsqueeze(2).to_broadcast([64, 8, 64]), op=ALU.mult), reads=[B_kkx, B_ss], writes=[B_kkx])
            fw.op("pool", lambda e: e.tensor_tensor(out=tmp[:], in0=asg[:], in1=rw_[0:64, 5, :], op=ALU.mult), reads=[B_asg, B_rw], writes=[B_tmp])
            fw.op("pool", lambda e: e.tensor_tensor(out=tmp[:], in0=tmp[:], in1=rw_[0:64, 5, :], op=ALU.subtract), reads=[B_tmp, B_rw], writes=[B_tmp])
            fw.op("pool", lambda e: e.tensor_scalar_add(out=tmp[:], in0=tmp[:], scalar1=1.0), reads=[B_tmp], writes=[B_tmp])
            fw.op("pool", lambda e: e.tensor_tensor(out=kmod[:], in0=kt[:], in1=tmp[:], op=ALU.mult), reads=[B_kt, B_tmp], writes=[B_kmod])
            fw.op("dve", lambda e: e.tensor_tensor(out=bv[:], in0=kkx[:], in1=asg[:], op=ALU.mult), reads=[B_kkx, B_asg], writes=[B_bv])
            fw.op("pool", lambda e: e.tensor_tensor(out=tmp[:], in0=rt[:], in1=kmod[:], op=ALU.mult), reads=[B_rt, B_kmod], writes=[B_tmp])
            fw.op("pool", lambda e: e.tensor_tensor(out=tmp[:], in0=tmp[:], in1=rw_[0:64, 6, :], op=ALU.mult), reads=[B_tmp, B_rw], writes=[B_tmp])
            fw.op("dve", lambda e: e.tensor_reduce(out=rks[:], in_=tmp[:].rearrange("p (h d) -> p h d", h=8), axis=AX.X, op=ALU.add), reads=[B_tmp], writes=[B_rks])
            fw.op("dve", lambda e: e.tensor_tensor(out=bon[:].rearrange("p (h d) -> p h d", h=8), in0=vt[:].rearrange("p (h d) -> p h d", h=8),
                                                   in1=rks[:].unsqueeze(2).to_broadcast([64, 8, 64]), op=ALU.mult), reads=[B_vt, B_rks], writes=[B_bon])
            fw.op("pe", lambda e: e.matmul(p4[0:64, :], lhsT=cs[0:64, TRI, 0:64], rhs=lw[:, :], start=True, stop=True), reads=[B_cs, B_lw], writes=pall(4))
            fw.op("act", lambda e: e.activation(out=eLp[:], in_=p4[0:64, :], func=AF.Exp), reads=pall(4), writes=[B_eLp])
            fw.op("act", lambda e: e.activation(out=eLn[:], in_=p4[0:64, :], func=AF.Exp, scale=-1.0), reads=pall(4), writes=[B_eLn])
            fw.op("dve", lambda e: e.tensor_tensor(out=eLx[:], in0=p4[0:64, :], in1=lw[:], op=ALU.subtract), reads=pall(4) + [B_lw], writes=[B_eLx])
            fw.op("act", lambda e: e.activation(out=eLx[:], in_=eLx[:], func=AF.Exp), reads=[B_eLx], writes=[B_eLx])
            fw.op("dve", lambda e: e.tensor_tensor(out=Rt[:], in0=rt[:], in1=eLp[:], op=ALU.mult), reads=[B_rt, B_eLp], writes=[B_Rt])
            fw.op("pool", lambda e: e.tensor_tensor(out=Kt[:], in0=kmod[:], in1=eLn[:], op=ALU.mult), reads=[B_kmod, B_eLn], writes=[B_Kt])
            fw.op("dve", lambda e: e.tensor_tensor(out=Bt[:], in0=bv[:], in1=eLn[:], op=ALU.mult), reads=[B_bv, B_eLn], writes=[B_Bt])
            fw.op("dve", lambda e: e.scalar_tensor_tensor(out=At[:], in0=kkx[:], scalar=-1.0, in1=eLx[:], op0=ALU.mult, op1=ALU.mult), reads=[B_kkx, B_eLx], writes=[B_At])
            for hh in range(8):
                fw.op("pe", lambda e, hh=hh: e.matmul(p5[0:64, hsl(hh)], lhsT=lw[:, hsl(hh)], rhs=ones64[:, :], start=True, stop=True), reads=[B_lw, B_ones], writes=[PB[5][hh]])
            fw.op("act", lambda e: e.activation(out=dec[:], in_=p5[0:64, :], func=AF.Exp), reads=pall(5), writes=[B_dec])
            for (src, Bsrc, dst, Bdst, bank) in ((Rt, B_Rt, RT, B_RT, 0), (Kt, B_Kt, KT, B_KT, 1), (Bt, B_Bt, BT, B_BT, 2), (At, B_At, AT, B_AT, 3)):
                pb_ = pp[bank][0]
                for hh in range(8):
                    fw.op("pe", lambda e, hh=hh, src=src, pb_=pb_: e.transpose(out=pb_[0:64, hsl(hh)], in_=src[:, hsl(hh)], identity=cs[0:64, IDN, 0:64]), reads=[Bsrc, B_cs], writes=[PB[bank][hh]])
                fw.op("act" if bank % 2 == 0 else "dve", (lambda e, dst=dst, pb_=pb_: e.copy(out=dst[:].rearrange("p h t -> p (h t)"), in_=pb_[0:64, :])) if bank % 2 == 0 else
                      (lambda e, dst=dst, pb_=pb_: e.tensor_copy(out=dst[:].rearrange("p h t -> p (h t)"), in_=pb_[0:64, :])), reads=pall(bank), writes=[Bdst])
            def score(bank, lhs, Blhs, rhs, Brhs, dsts, mask, eng):
                pb_ = pp[bank][0]
                for hh in range(8):
                    fw.op("pe", lambda e, hh=hh, pb_=pb_: e.matmul(pb_[0:64, hsl(hh)], lhsT=lhs[:, hh, :], rhs=rhs[:, hh, :], start=True, stop=True), reads=[Blhs, Brhs], writes=[PB[bank][hh]])
                for hh in range(8):
                    d, Bd = dsts[hh]
                    fw.op(eng, lambda e, hh=hh, d=d, pb_=pb_: e.tensor_tensor(out=d[:], in0=pb_[0:64, hsl(hh)], in1=cs[0:64, mask, 0:64], op=ALU.mult), reads=[PB[bank][hh], B_cs], writes=[Bd])
            score(4, BT, B_BT, AT, B_AT, Na, SUP, "dve")
            score(5, AT, B_AT, BT, B_BT, NTa, SLO, "dve")
            score(6, KT, B_KT, AT, B_AT, Pak, SUP, "dve")
            score(7, BT, B_BT, RT, B_RT, Prb, TRI, "dve")
            score(0, KT, B_KT, RT, B_RT, Prk, TRI, "dve")
            for hh in range(8):
                fw.op("pool", lambda e, hh=hh: e.tensor_tensor(out=Pm[hh][0][:], in0=Na[hh][0][:], in1=cs[0:64, IDN, 0:64], op=ALU.add), reads=[Na[hh][1], B_cs], writes=[Pm[hh][1]])
            cur, curT, nxt, nxtT = Na, NTa, Nb, NTb
            for lvl in range(1, 6):
                last = (lvl == 5)
                if not last:
                    for hh in range(8):
                        fw.op("pe", lambda e, hh=hh, cur=cur, curT=curT: e.matmul(p1[0:64, hsl(hh)], lhsT=curT[hh][0][:, :], rhs=cur[hh][0][:, :], start=True, stop=True), reads=[curT[hh][1], cur[hh][1]], writes=[PB[1][hh]])
                for hh in range(8):
                    fw.op("pe", lambda e, hh=hh, cur=cur, curT=curT: e.matmul(p2[0:64, hsl(hh)], lhsT=cur[hh][0][:, :], rhs=curT[hh][0][:, :], start=True, stop=True), reads=[curT[hh][1], cur[hh][1]], writes=[PB[2][hh]])
                for hh in range(8):
                    if not last:
                        fw.op("act", lambda e, hh=hh, nxt=nxt: e.copy(out=nxt[hh][0][:], in_=p1[0:64, hsl(hh)]), reads=[PB[1][hh]], writes=[nxt[hh][1]])
                    fw.op("dve", lambda e, hh=hh, nxtT=nxtT: e.tensor_copy(out=nxtT[hh][0][:], in_=p2[0:64, hsl(hh)]), reads=[PB[2][hh]], writes=[nxtT[hh][1]])
                for hh in range(8):
                    fw.op("pe", lambda e, hh=hh, nxtT=nxtT: e.matmul(p3[0:64, hsl(hh)], lhsT=nxtT[hh][0][:, :], rhs=Pm[hh][0][:, :], start=True, stop=True), reads=[nxtT[hh][1], Pm[hh][1]], writes=[PB[3][hh]])
                for hh in range(8):
                    fw.op("dve", lambda e, hh=hh: e.tensor_tensor(out=Pm[hh][0][:], in0=p3[0:64, hsl(hh)], in1=Pm[hh][0][:], op=ALU.add), reads=[PB[3][hh], Pm[hh][1]], writes=[Pm[hh][1]])
                cur, curT, nxt, nxtT = nxt, nxtT, cur, curT
            for hh in range(8):
                fw.op("pe", lambda e, hh=hh: e.matmul(p4[0:64, hsl(hh)], lhsT=AT[:, hh, :], rhs=H[hh][0][:, :], start=True, stop=False), reads=[B_AT, H[hh][1]], writes=[PB[4][hh]])
                fw.op("pe", lambda e, hh=hh: e.matmul(p4[0:64, hsl(hh)], lhsT=Pak[hh][0][:, :], rhs=vt[:, hsl(hh)], start=False, stop=True), reads=[Pak[hh][1], B_vt], writes=[PB[4][hh]])
            for hh in range(8):
                fw.op("act", lambda e, hh=hh: e.copy(out=R0[hh][0][:], in_=p4[0:64, hsl(hh)]), reads=[PB[4][hh]], writes=[R0[hh][1]])
            for hh in range(8):
                fw.op("pe", lambda e, hh=hh: e.matmul(p5[0:64, hsl(hh)], lhsT=Pm[hh][0][:, :], rhs=R0[hh][0][:, :], start=True, stop=True), reads=[Pm[hh][1], R0[hh][1]], writes=[PB[5][hh]])
            for hh in range(8):
                fw.op("act", lambda e, hh=hh: e.copy(out=U[hh][0][:], in_=p5[0:64, hsl(hh)]), reads=[PB[5][hh]], writes=[U[hh][1]])
            for hh in range(8):
                fw.op("pe", lambda e, hh=hh: e.matmul(p6[0:64, hsl(hh)], lhsT=RT[:, hh, :], rhs=H[hh][0][:, :], start=True, stop=False), reads=[B_RT, H[hh][1]], writes=[PB[6][hh]])
                fw.op("pe", lambda e, hh=hh: e.matmul(p6[0:64, hsl(hh)], lhsT=Prk[hh][0][:, :], rhs=vt[:, hsl(hh)], start=False, stop=False), reads=[Prk[hh][1], B_vt], writes=[PB[6][hh]])
                fw.op("pe", lambda e, hh=hh: e.matmul(p6[0:64, hsl(hh)], lhsT=Prb[hh][0][:, :], rhs=U[hh][0][:, :], start=False, stop=True), reads=[Prb[hh][1], U[hh][1]], writes=[PB[6][hh]])
            fw.op("act", lambda e: e.copy(out=yt[:], in_=p6[0:64, :]), reads=pall(6), writes=[B_yt])
            for hh in range(8):
                fw.op("pe", lambda e, hh=hh: e.matmul(p7[0:64, hsl(hh)], lhsT=Kt[:, hsl(hh)], rhs=vt[:, hsl(hh)], start=True, stop=False), reads=[B_Kt, B_vt], writes=[PB[7][hh]])
                fw.op("pe", lambda e, hh=hh: e.matmul(p7[0:64, hsl(hh)], lhsT=Bt[:, hsl(hh)], rhs=U[hh][0][:, :], start=False, stop=True), reads=[B_Bt, U[hh][1]], writes=[PB[7][hh]])
            for hh in range(8):
                fw.op("dve", lambda e, hh=hh: e.tensor_tensor(out=H[hh][0][:], in0=p7[0:64, hsl(hh)], in1=H[hh][0][:], op=ALU.add), reads=[PB[7][hh], H[hh][1]], writes=[H[hh][1]])
                fw.op("pool", lambda e, hh=hh: e.tensor_scalar(out=H[hh][0][:], in0=H[hh][0][:], scalar1=dec[:, hh * 64:hh * 64 + 1], scalar2=None, op0=ALU.mult), reads=[H[hh][1], B_dec], writes=[H[hh][1]])
            y3 = yt[:].rearrange("p (h d) -> p h d", h=8)
            fw.op("dve", lambda e: e.tensor_reduce(out=ss[:], in_=yt[:].rearrange("p (h d) -> p h d", h=8), axis=AX.X, op=ALU.add), reads=[B_yt], writes=[B_ss])
            fw.op("dve", lambda e: e.tensor_scalar_mul(out=ss[:], in0=ss[:], scalar1=1.0 / 64.0), reads=[B_ss], writes=[B_ss])
            fw.op("dve", lambda e: e.tensor_tensor(out=yt[:].rearrange("p (h d) -> p h d", h=8), in0=yt[:].rearrange("p (h d) -> p h d", h=8),
                                                   in1=ss[:].unsqueeze(2).to_broadcast([64, 8, 64]), op=ALU.subtract), reads=[B_yt, B_ss], writes=[B_yt])
            fw.op("pool", lambda e: e.tensor_tensor(out=sq[:], in0=yt[:], in1=yt[:], op=ALU.mult), reads=[B_yt], writes=[B_sq])
            fw.op("dve", lambda e: e.tensor_reduce(out=rks[:], in_=sq[:].rearrange("p (h d) -> p h d", h=8), axis=AX.X, op=ALU.add), reads=[B_sq], writes=[B_rks])
            fw.op("dve", lambda e: e.tensor_scalar(out=rks[:], in0=rks[:], scalar1=1.0 / 64.0, scalar2=64e-5, op0=ALU.mult, op1=ALU.add), reads=[B_rks], writes=[B_rks])
            fw.op("act", lambda e: e.activation(out=rks[:], in_=rks[:], func=AF.Sqrt), reads=[B_rks], writes=[B_rks])
            fw.op("dve", lambda e: e.reciprocal(out=rks[:], in_=rks[:]), reads=[B_rks], writes=[B_rks])
            fw.op("dve", lambda e: e.tensor_tensor(out=yt[:].rearrange("p (h d) -> p h d", h=8), in0=yt[:].rearrange("p (h d) -> p h d", h=8),
                                                   in1=rks[:].unsqueeze(2).to_broadcast([64, 8, 64]), op=ALU.mult), reads=[B_yt, B_rks], writes=[B_yt])
            fw.op("pool", lambda e: e.tensor_tensor(out=yt[:], in0=yt[:], in1=rw_[0:64, 7, :], op=ALU.mult), reads=[B_yt, B_rw], writes=[B_yt])
            fw.op("pool", lambda e: e.tensor_tensor(out=yt[:], in0=yt[:], in1=rw_[0:64, 8, :], op=ALU.add), reads=[B_yt, B_rw], writes=[B_yt])
            fw.op("dve", lambda e: e.tensor_tensor(out=yt[:], in0=yt[:], in1=bon[:], op=ALU.add), reads=[B_yt, B_bon], writes=[B_yt])
            fw.op("dve", lambda e: e.tensor_tensor(out=yt[:], in0=yt[:], in1=gate[:], op=ALU.mult), reads=[B_yt, B_gate], writes=[B_yt])
            fw.dma("sp", lambda e, tsl=tsl: e.dma_start(out=feat[tsl, 512:1024], in_=yt[:]), reads=[B_yt], writes=[B_feat])
    return fw.finish(finals)


def even_consts(g):
    f = np.float32
    c = np.zeros((128, 8, 128), f)
    c[:, 0] = np.triu(np.ones((128, 128), f))
    c[:, 1] = np.eye(128, dtype=f)
    t = np.arange(128, dtype=np.float64)
    for hh in range(2):
        gam = 1.0 - 2.0 ** (-5.0 - (2 * g + hh))
        c[:, 2 + 2 * hh] = (gam ** (t + 1.0))[None, :]
        c[:, 3 + 2 * hh] = (gam ** (-(t + 1.0)) / 16.0)[None, :]
    s = np.arange(128)
    c[:, 6] = (s[None, :] < s[:, None]).astype(f)
    c[:, 7] = (s[:, None] < s[None, :]).astype(f)
    c2 = np.zeros((128, 8), f)
    c2[:, 0] = (10000.0 ** (-np.arange(128, dtype=np.float32) / np.float32(128))).astype(f)
    for hh in range(2):
        gam = 1.0 - 2.0 ** (-5.0 - (2 * g + hh))
        c2[:, 1 + hh] = gam ** 128.0
    c2[:64, 3] = 1.0
    c2[64:, 4] = 1.0
    return c, c2


def even_inputs(hT_b, g, pos_b, win, layer2, p, vfirst=None):
    f = np.float32
    win = np.asarray(win, f)
    rq, rk, rv, rg = [win[:, i * 1024:(i + 1) * 1024][:, g * 512:(g + 1) * 512] for i in range(4)]
    wb = win[:, 4096:]
    mu = np.asarray(p["mu"], f)
    wr, wk, wv = [wb[:, i * 1024:(i + 1) * 1024][:, g * 512:(g + 1) * 512] for i in range(3)]
    mr, mk, mvv = [mu[i * 1024:(i + 1) * 1024][g * 512:(g + 1) * 512] for i in range(3)]
    lora = np.zeros((2048, 512), f); lora[:, :288] = wb[:, 3072:3360]
    mlo = np.zeros(512, f); mlo[:288] = mu[3072:3360]
    fm = [_kmajor(rq), _kmajor(rk), _kmajor(lora)]
    mfm = [np.zeros(512, f), np.zeros(512, f), mlo]
    if layer2:
        fm += [_kmajor(wb[:, 2048:2560]), _kmajor(wb[:, 2560:3072])]
        mfm += [mu[2048:2560], mu[2560:3072]]
    tm = [_kmajor(rv), _kmajor(rg), _kmajor(wr), _kmajor(wk), _kmajor(wv)]
    mtm = [np.zeros(512, f), np.zeros(512, f), mr, mk, mvv]
    c, c2 = even_consts(g)
    rows = np.zeros((12, 512), f)
    sl = slice(g * 512, (g + 1) * 512)
    rows[0] = np.asarray(p["ret_gn_g"], f)[sl]; rows[1] = np.asarray(p["ret_gn_b"], f)[sl]
    rows[2] = np.asarray(p["w0"], f)[sl]; rows[3] = np.asarray(p["a0"], f)[sl]
    rows[4] = np.asarray(p["kk"], f)[sl]; rows[5] = np.asarray(p["ka"], f)[sl]
    rows[6] = np.asarray(p["rk"], f).reshape(-1)[sl]; rows[7] = np.asarray(p["lnx_g"], f)[sl]; rows[8] = np.asarray(p["lnx_b"], f)[sl]
    d = {"hT": hT_b, "Wfm": np.stack(fm), "Wtm": np.stack(tm), "mufm": np.stack(mfm).astype(f), "mutm": np.stack(mtm).astype(f),
         "posi": np.ascontiguousarray(np.asarray(pos_b, np.int32)[None, :]), "cst": c, "cst2": c2,
         "lw2": np.ascontiguousarray(np.asarray(p["w2"], f)[:, sl]), "la2": np.ascontiguousarray(np.asarray(p["a2"], f)[:, sl]),
         "lg2": np.ascontiguousarray(np.asarray(p["g2"], f)[:, sl])}
    if layer2:
        rows[9] = np.asarray(p["v0"], f)[sl]
        d["lv1"] = _kmajor(np.asarray(p["v1"], f))
        d["lv2"] = np.ascontiguousarray(np.asarray(p["v2"], f)[:, sl])
        d["vfirst"] = vfirst
    d["rowsd"] = rows
    return d


_CACHE = {}
def _get(name, fn, *a):
    key = (name,) + a
    if key not in _CACHE:
        _CACHE[key] = fn(*a)
    return _CACHE[key]


def _run(nc, in_maps):
    res = run_bass_kernel_spmd(nc, in_maps, core_ids=list(range(NCORE)))
    return res.results


def kernel(x, c, positions, ada_w, ada_b, ln_g, ln_b, ev_win, ev_wo, ret_gn_g, ret_gn_b,
           rw_mu, rw_w0, rw_w2, rw_a0, rw_a2, rw_g2, rw_kk, rw_ka, rw_rk, rw_lnx_g, rw_lnx_b,
           rw_v0, rw_v1, rw_v2, od_win, od_wo, gla_a2, gla_ab, gla_norm_g,
           moe_wg, moe_bg, moe_we, moe_be, moe_w1, moe_w3, moe_w2):
    f = np.float32
    x = np.asarray(x, f); c = np.asarray(c, f)
    positions = np.asarray(positions)
    vfirst = None
    T = x.shape[0] * x.shape[1]
    xs = x.reshape(NCORE, TOK, D)
    cT = _kmajor(np.ascontiguousarray(c.T))
    ada_w = np.asarray(ada_w, f); ada_b = np.asarray(ada_b, f)
    ins = []
    for i in range(NCORE):
        cs = slice(i * 1536, (i + 1) * 1536)
        aw = np.stack([_kmajor(ada_w[l][:, cs]) for l in range(DEPTH)])
        ab = np.ascontiguousarray(np.broadcast_to(ada_b[:, None, cs], (DEPTH, 4, 1536)))
        ins.append({"cT": cT, "adaw": aw, "adab": ab})
    r = _run(_get("ada", build_ada), ins)
    mod = np.concatenate([r[i]["mod"] for i in range(NCORE)], axis=-1)
    def seg(l, b, j):
        return mod[l, b, j * D:(j + 1) * D]
    zero = np.zeros(D, f)
    def rows_for(b, gt, g, bb, sc, sh):
        return np.ascontiguousarray(np.stack([np.asarray(v, np.float32) for v in (gt, g, bb, sc, sh)]))
    ln_g = np.asarray(ln_g, f); ln_b = np.asarray(ln_b, f)
    ins = [{"x": xs[i], "rows": rows_for(i // 2, zero, zero, zero, seg(0, i // 2, 1), seg(0, i // 2, 0))} for i in range(NCORE)]
    r = _run(_get("post_mod", build_post, "mod"), ins)
    h = [r[i]["ho"] for i in range(NCORE)]
    xcur = [xs[i] for i in range(NCORE)]
    for l in range(DEPTH):
        j = l // 2
        wo = np.asarray(ev_wo[j] if l % 2 == 0 else od_wo[j], f)
        hTb = [_kmajor(np.ascontiguousarray(np.concatenate([h[2 * b], h[2 * b + 1]], axis=0).T)) for b in range(4)]
        if l % 2 == 0:
            layer2 = (j == 1)
            p = {"mu": rw_mu[j], "ret_gn_g": ret_gn_g[j], "ret_gn_b": ret_gn_b[j], "w0": rw_w0[j], "w2": rw_w2[j], "a0": rw_a0[j],
                 "a2": rw_a2[j], "g2": rw_g2[j], "kk": rw_kk[j], "ka": rw_ka[j], "rk": rw_rk[j], "lnx_g": rw_lnx_g[j], "lnx_b": rw_lnx_b[j]}
            if layer2:
                p.update({"v0": rw_v0[j - 1], "v1": rw_v1[j - 1], "v2": rw_v2[j - 1]})
            ins = [even_inputs(hTb[i // 2], i % 2, positions[i // 2], ev_win[j], layer2, p, vfirst[i] if layer2 else None) for i in range(NCORE)]
            r = _run(_get("even", build_even, layer2), ins)
            if not layer2:
                vfirst = [r[i]["vout"] for i in range(NCORE)]
            featb = []
            for b in range(4):
                fb = np.empty((T_SEQ, D), f)
                for g in range(2):
                    fc = r[2 * b + g]["feat"]
                    fb[:, g * 512:(g + 1) * 512] = fc[:, 0:512]
                    fb[:, 1024 + g * 512:1024 + (g + 1) * 512] = fc[:, 512:1024]
                featb.append(fb)
        else:
            ins = [gla_inputs(hTb[i // 2], i % 2, od_win[j], gla_a2[j], gla_ab[j], gla_norm_g[j]) for i in range(NCORE)]
            r = _run(_get("gla", build_gla), ins)
            featb = [np.concatenate([r[2 * b]["feat"], r[2 * b + 1]["feat"]], axis=1) for b in range(4)]
        feat = [featb[i // 2][(i % 2) * TOK:(i % 2 + 1) * TOK] for i in range(NCORE)]
        wo_l = _kmajor(wo)
        ins = []
        for i in range(NCORE):
            b = i // 2
            ins.append({"x": xcur[i], "fT": _kmajor(np.ascontiguousarray(feat[i].T)), "wo": wo_l,
                        "rows": rows_for(b, seg(l, b, 2), ln_g[l, 0], ln_b[l, 0], seg(l, b, 4), seg(l, b, 3))})
        r = _run(_get("post_wo", build_post, "wo"), ins)
        xcur = [r[i]["xo"] for i in range(NCORE)]
        h2 = [r[i]["ho"] for i in range(NCORE)]
        wge = _kmajor(np.concatenate([np.asarray(moe_wg[l], f), np.asarray(moe_we[l], f)], axis=1))
        bge = _tile_rows(np.concatenate([np.asarray(moe_bg[l], f), np.asarray(moe_be[l], f)]))
        h2T = [_kmajor(np.ascontiguousarray(h2[i].T)) for i in range(NCORE)]
        r = _run(_get("route", build_route), [{"hT": h2T[i], "wge": wge, "bge": bge} for i in range(NCORE)])
        G = np.concatenate([r[i]["G"] for i in range(NCORE)], axis=0)
        MK = np.concatenate([r[i]["MK"] for i in range(NCORE)], axis=0)
        h2_all = np.concatenate(h2, axis=0)
        idx = [np.nonzero(MK[:, e])[0] for e in range(32)]
        cap = max(256, -(-max(len(ix) for ix in idx) // 256) * 256)
        ins = []
        for i in range(NCORE):
            hts, gbr = [], np.zeros((4, cap), f)
            for e in range(4):
                E = 4 * i + e
                hb = np.zeros((cap, D), f); hb[:len(idx[E])] = h2_all[idx[E]]
                hts.append(_kmajor(np.ascontiguousarray(hb.T)))
                gbr[e, :len(idx[E])] = G[idx[E], E]
            ins.append({"hT": np.stack(hts), "gb": gbr,
                        "w1": np.stack([_kmajor(np.asarray(moe_w1[l, e], f)) for e in range(4 * i, 4 * i + 4)]),
                        "w3": np.stack([_kmajor(np.asarray(moe_w3[l, e], f)) for e in range(4 * i, 4 * i + 4)]),
                        "w2": np.stack([_kmajor(np.asarray(moe_w2[l, e], f)) for e in range(4 * i, 4 * i + 4)])})
        r = _run(_get("moe_sparse", build_moe_sparse, cap), ins)
        ypart = np.zeros((2, T, D), f)
        seen = np.zeros(T, np.int64)
        for E in range(32):
            ix = idx[E]
            ypart[seen[ix], ix] = r[E // 4]["y"][E % 4, :len(ix)]
            seen[ix] += 1
        ins = []
        for i in range(NCORE):
            b = i // 2
            yp = np.ascontiguousarray(ypart[:, i * TOK:(i + 1) * TOK])
            if l + 1 < DEPTH:
                sc, sh = seg(l + 1, b, 1), seg(l + 1, b, 0)
            else:
                sc, sh = zero, zero
            ins.append({"x": xcur[i], "yp": yp, "rows": rows_for(b, seg(l, b, 5), ln_g[l, 1], ln_b[l, 1], sc, sh)})
        lastl = (l + 1 == DEPTH)
        r = _run(_get("post_y", build_post, "y", 2, not lastl), ins)
        xcur = [r[i]["xo"] for i in range(NCORE)]
        if not lastl:
            h = [r[i]["ho"] for i in range(NCORE)]
    return np.concatenate(xcur, axis=0).reshape(x.shape).astype(np.float32)
```
